# Optimizing a Trainium2 kernel written in Bass

```python
import jax
import jax.numpy as jnp
from jax import lax
import numpy as np

D_MODEL = 4096
BATCH = 2
SEQ = 8192
DEPTH = 4

GRID_W = 64
CTX_LEN = 256
N_MIXERS = 2
N_NA_LAYERS = (DEPTH + 1) // 2
N_GLA_LAYERS = DEPTH // 2

NA_HEADS = 32
NA_HEAD_DIM = D_MODEL // NA_HEADS
NA_KH = 8
NA_KW = 16

GLA_HEADS = 8
GLA_DK = D_MODEL // 2 // GLA_HEADS
GLA_DV = D_MODEL // GLA_HEADS
GLA_DK_TOTAL = GLA_HEADS * GLA_DK
GLA_GATE_RANK = 16
GLA_GATE_NORMALIZER = 16.0
GLA_CHUNK = 64
ROPE_BASE = 10000.0

N_EXPERTS = 64
EXPERT_DIM = 192
TOP_K = 8
N_GROUPS = 8
TOPK_GROUPS = 4
ROUTED_SCALE = 2.5

ADA_RANK = 256
LN_EPS = 1e-6
DEEPNORM_ALPHA = (2 * DEPTH) ** 0.25
DEEPNORM_BETA = (8 * DEPTH) ** -0.25
NEG_INF = -1e30

kernel_name = "hybrid_na_gla_moe_diffusion_trunk"


def layernorm(x, g, b):
    xf = x.astype(jnp.float32)
    mu = jnp.mean(xf, axis=-1, keepdims=True)
    var = jnp.mean(jnp.square(xf - mu), axis=-1, keepdims=True)
    y = (xf - mu) * lax.rsqrt(var + LN_EPS)
    return (y * g + b).astype(x.dtype)


def adaln(cond, w1, w2, b):
    m = (jax.nn.silu(cond) @ w1) @ w2 + b
    return jnp.split(m, 6, axis=-1)


def modulate(x, shift, scale):
    return x * (1.0 + scale) + shift


def axial_rope(t, n_tokens):
    half = t.shape[-1] // 2
    nf = half // 2
    pos = jnp.arange(n_tokens)
    inv = ROPE_BASE ** (-jnp.arange(nf, dtype=jnp.float32) / nf)

    def rot(u, p):
        ang = p.astype(jnp.float32)[:, None] * inv
        cos = jnp.cos(ang)[None, :, None, :]
        sin = jnp.sin(ang)[None, :, None, :]
        u1 = u[..., :nf].astype(jnp.float32)
        u2 = u[..., nf:].astype(jnp.float32)
        return jnp.concatenate([u1 * cos - u2 * sin, u1 * sin + u2 * cos], axis=-1)

    return jnp.concatenate([rot(t[..., :half], pos // GRID_W), rot(t[..., half:], pos % GRID_W)], axis=-1)


def neighbourhood_attention(h_lat, h_ctx, w_qkv, w_o, rpb, need_ctx):
    B, S, _ = h_lat.shape
    n_ctx = h_ctx.shape[1]
    rows = S // GRID_W
    kh = min(NA_KH, rows)
    scale = NA_HEAD_DIM ** -0.5
    qkv = (h_lat @ w_qkv).reshape(B, rows, GRID_W, 3, NA_HEADS, NA_HEAD_DIM)
    q, k, v = qkv[..., 0, :, :], qkv[..., 1, :, :], qkv[..., 2, :, :]
    qkv_c = (h_ctx @ w_qkv).reshape(B, n_ctx, 3, NA_HEADS, NA_HEAD_DIM)
    qc, kc, vc = qkv_c[:, :, 0], qkv_c[:, :, 1], qkv_c[:, :, 2]

    col = jnp.arange(GRID_W)
    c_start = jnp.clip(col - NA_KW // 2, 0, GRID_W - NA_KW)
    col_ok = (col[None, :] >= c_start[:, None]) & (col[None, :] < c_start[:, None] + NA_KW)
    dc = jnp.clip(col[None, :] - col[:, None], 1 - NA_KW, NA_KW - 1) + NA_KW - 1
    band_mask = jnp.broadcast_to(col_ok[:, None, :], (GRID_W, kh, GRID_W)).reshape(GRID_W, kh * GRID_W)
    n_band = kh * GRID_W

    def row_block(r):
        r_start = jnp.clip(r - kh // 2, 0, rows - kh)
        dr = r_start + jnp.arange(kh) - r + NA_KH - 1
        bias = rpb[:, dr[None, :, None], dc[:, None, :]].reshape(NA_HEADS, GRID_W, n_band)
        q_r = q[:, r]
        k_b = lax.dynamic_slice_in_dim(k, r_start, kh, axis=1).reshape(B, n_band, NA_HEADS, NA_HEAD_DIM)
        v_b = lax.dynamic_slice_in_dim(v, r_start, kh, axis=1).reshape(B, n_band, NA_HEADS, NA_HEAD_DIM)
        s_band = jnp.einsum('bqhd,bkhd->bhqk', q_r, k_b).astype(jnp.float32) * scale + bias
        s_band = jnp.where(band_mask, s_band, NEG_INF)
        s_ctx = jnp.einsum('bqhd,bchd->bhqc', q_r, kc).astype(jnp.float32) * scale
        p = jax.nn.softmax(jnp.concatenate([s_band, s_ctx], axis=-1), axis=-1).astype(v.dtype)
        return (jnp.einsum('bhqk,bkhd->bqhd', p[..., :n_band], v_b)
                + jnp.einsum('bhqc,bchd->bqhd', p[..., n_band:], vc))

    o = lax.map(row_block, jnp.arange(rows))
    y_lat = o.transpose(1, 0, 2, 3, 4).reshape(B, S, D_MODEL) @ w_o
    if not need_ctx:
        return y_lat, None
    s = jnp.einsum('bqhd,bkhd->bhqk', qc, kc).astype(jnp.float32) * scale
    p = jax.nn.softmax(s, axis=-1).astype(vc.dtype)
    y_ctx = jnp.einsum('bhqk,bkhd->bqhd', p, vc).reshape(B, n_ctx, D_MODEL) @ w_o
    return y_lat, y_ctx


def gla_scan(q, k, v, log_a, s0):
    B, T, H, dk = q.shape
    dv = v.shape[-1]
    L = GLA_CHUNK
    n = T // L

    def chunks(a):
        return a.astype(jnp.float32).reshape(B, n, L, H, a.shape[-1]).transpose(1, 0, 3, 2, 4)

    tril = jnp.tril(jnp.ones((L, L), dtype=bool))

    def step(S, inp):
        qc, kc, vc, lac = inp
        b = jnp.cumsum(lac, axis=-2)
        m = b[..., L // 2:L // 2 + 1, :]
        bl = b[..., -1:, :]
        attn = jnp.einsum('bhtd,bhsd->bhts', qc * jnp.exp(b - m), kc * jnp.exp(m - b))
        attn = jnp.where(tril, attn, 0.0)
        o = (jnp.einsum('bhts,bhsv->bhtv', attn, vc)
             + jnp.einsum('bhtd,bhdv->bhtv', qc * jnp.exp(b), S))
        S = jnp.exp(bl[..., 0, :])[..., None] * S + jnp.einsum('bhsd,bhsv->bhdv', kc * jnp.exp(bl - b), vc)
        return S, o

    S_fin, o = lax.scan(step, s0, (chunks(q), chunks(k), chunks(v), chunks(log_a)))
    o = o.transpose(1, 0, 3, 2, 4).reshape(B, T, H, dv)
    return o, S_fin


def gla_mixer(h_lat, h_ctx, w_in, w_a1, w_a2, b_a, norm_w, w_o, need_ctx):
    def project(h):
        B, T, _ = h.shape
        z = h @ w_in
        q, k, v, g = jnp.split(z, [GLA_DK_TOTAL, 2 * GLA_DK_TOTAL, 2 * GLA_DK_TOTAL + D_MODEL], axis=-1)
        q = q.reshape(B, T, GLA_HEADS, GLA_DK) * (GLA_DK ** -0.5)
        k = k.reshape(B, T, GLA_HEADS, GLA_DK)
        v = v.reshape(B, T, GLA_HEADS, GLA_DV)
        la = [(jax.nn.log_sigmoid(((h @ w_a1[d]) @ w_a2[d] + b_a[d]).astype(jnp.float32))
               / GLA_GATE_NORMALIZER).reshape(B, T, GLA_HEADS, GLA_DK) for d in range(2)]
        return q, k, v, g, la[0], la[1]

    def output(o_f, o_b_rev, g):
        B, T = g.shape[:2]
        o = o_f + o_b_rev[:, ::-1]
        o = o * lax.rsqrt(jnp.mean(jnp.square(o), axis=-1, keepdims=True) + LN_EPS) * norm_w
        o = o.astype(g.dtype) * jax.nn.silu(g).reshape(B, T, GLA_HEADS, GLA_DV)
        return o.reshape(B, T, D_MODEL) @ w_o

    B, S, _ = h_lat.shape
    qc, kc, vc, gc, lac_f, lac_b = project(h_ctx)
    ql, kl, vl, gl, lal_f, lal_b = project(h_lat)
    ql = axial_rope(ql, S)
    kl = axial_rope(kl, S)
    s0 = jnp.zeros((B, GLA_HEADS, GLA_DK, GLA_DV), jnp.float32)
    oc_f, sc_f = gla_scan(qc, kc, vc, lac_f, s0)
    oc_b, sc_b = gla_scan(qc[:, ::-1], kc[:, ::-1], vc[:, ::-1], lac_b[:, ::-1], s0)
    ol_f, _ = gla_scan(ql, kl, vl, lal_f, sc_f)
    ol_b, _ = gla_scan(ql[:, ::-1], kl[:, ::-1], vl[:, ::-1], lal_b[:, ::-1], sc_b)
    y_lat = output(ol_f, ol_b, gl)
    if not need_ctx:
        return y_lat, None
    return y_lat, output(oc_f, oc_b, gc)


def moe_ffn(h, w_router, e_bias, w_gate, w_up, w_down, s_gate, s_up, s_down):
    shape = h.shape
    t = h.reshape(-1, D_MODEL)
    n = t.shape[0]
    scores = jax.nn.sigmoid((t @ w_router).astype(jnp.float32))
    biased = scores + e_bias.astype(jnp.float32)
    grp = biased.reshape(n, N_GROUPS, N_EXPERTS // N_GROUPS)
    grp_score = jnp.sum(lax.top_k(grp, 2)[0], axis=-1)
    _, top_g = lax.top_k(grp_score, TOPK_GROUPS)
    g_mask = jnp.sum(jax.nn.one_hot(top_g, N_GROUPS, dtype=jnp.float32), axis=1) > 0
    e_mask = jnp.repeat(g_mask, N_EXPERTS // N_GROUPS, axis=1)
    _, idx = lax.top_k(jnp.where(e_mask, biased, NEG_INF), TOP_K)
    w = jnp.take_along_axis(scores, idx, axis=1)
    w = w / jnp.sum(w, axis=-1, keepdims=True) * ROUTED_SCALE
    gates = jnp.sum(jax.nn.one_hot(idx, N_EXPERTS, dtype=jnp.float32) * w[..., None], axis=1).astype(t.dtype)
    shared = (jax.nn.silu(t @ s_gate) * (t @ s_up)) @ s_down

    def expert(acc, p):
        wg, wu, wd, g = p
        hid = jax.nn.silu(t @ wg) * (t @ wu) * g[:, None]
        return acc + hid @ wd, None

    out, _ = lax.scan(expert, shared, (w_gate, w_up, w_down, gates.T))
    return out.reshape(shape)


def setup_inputs(seed: int = 0) -> dict:
    key = jax.random.key(seed)
    ks = jax.random.split(key, 32)
    D = D_MODEL

    def nrm(k, shape, scale):
        return jax.random.normal(k, shape, jnp.float32) * scale

    return {
        "x": nrm(ks[0], (BATCH, SEQ, D), 1.0),
        "c": nrm(ks[1], (BATCH, D), 1.0),
        "ctx": nrm(ks[2], (BATCH, CTX_LEN, D), 1.0),
        "c_ctx": nrm(ks[3], (D,), 1.0),
        "ada_w1": nrm(ks[4], (DEPTH, D, ADA_RANK), D ** -0.5),
        "ada_w2": nrm(ks[5], (DEPTH, ADA_RANK, 6 * D), 0.5 * ADA_RANK ** -0.5),
        "ada_b": nrm(ks[6], (DEPTH, 6 * D), 0.01),
        "ln_g": 1.0 + nrm(ks[7], (DEPTH, 2, D), 0.01),
        "ln_b": nrm(ks[8], (DEPTH, 2, D), 0.01),
        "na_w_qkv": nrm(ks[9], (N_NA_LAYERS, D, 3 * D), D ** -0.5),
        "na_w_o": nrm(ks[10], (N_NA_LAYERS, D, D), DEEPNORM_BETA * D ** -0.5),
        "na_rpb": nrm(ks[11], (N_NA_LAYERS, NA_HEADS, 2 * NA_KH - 1, 2 * NA_KW - 1), 0.1),
        "gla_w_in": nrm(ks[12], (N_GLA_LAYERS, D, 2 * GLA_DK_TOTAL + 2 * D), D ** -0.5),
        "gla_w_a1": nrm(ks[13], (N_GLA_LAYERS, 2, D, GLA_GATE_RANK), D ** -0.5),
        "gla_w_a2": nrm(ks[14], (N_GLA_LAYERS, 2, GLA_GATE_RANK, GLA_DK_TOTAL), GLA_GATE_RANK ** -0.5),
        "gla_b_a": 1.0 + nrm(ks[15], (N_GLA_LAYERS, 2, GLA_DK_TOTAL), 0.5),
        "gla_norm": 1.0 + nrm(ks[16], (N_GLA_LAYERS, GLA_DV), 0.01),
        "gla_w_o": nrm(ks[17], (N_GLA_LAYERS, D, D), DEEPNORM_BETA * D ** -0.5),
        "moe_router": nrm(ks[18], (DEPTH, D, N_EXPERTS), D ** -0.5),
        "moe_bias": nrm(ks[19], (DEPTH, N_EXPERTS), 0.01),
        "moe_w_gate": nrm(ks[20], (DEPTH, N_EXPERTS, D, EXPERT_DIM), D ** -0.5),
        "moe_w_up": nrm(ks[21], (DEPTH, N_EXPERTS, D, EXPERT_DIM), D ** -0.5),
        "moe_w_down": nrm(ks[22], (DEPTH, N_EXPERTS, EXPERT_DIM, D), DEEPNORM_BETA * EXPERT_DIM ** -0.5),
        "sh_w_gate": nrm(ks[23], (DEPTH, D, EXPERT_DIM), D ** -0.5),
        "sh_w_up": nrm(ks[24], (DEPTH, D, EXPERT_DIM), D ** -0.5),
        "sh_w_down": nrm(ks[25], (DEPTH, EXPERT_DIM, D), DEEPNORM_BETA * EXPERT_DIM ** -0.5),
    }


def reference(x, c, ctx, c_ctx, ada_w1, ada_w2, ada_b, ln_g, ln_b, na_w_qkv, na_w_o, na_rpb,
              gla_w_in, gla_w_a1, gla_w_a2, gla_b_a, gla_norm, gla_w_o, moe_router, moe_bias,
              moe_w_gate, moe_w_up, moe_w_down, sh_w_gate, sh_w_up, sh_w_down):
    x_lat, x_ctx = x, ctx
    n_ctx = ctx.shape[1]
    for i in range(DEPTH):
        last = i == DEPTH - 1
        sh_m, sc_m, g_m, sh_f, sc_f, g_f = [m[:, None, :] for m in adaln(c, ada_w1[i], ada_w2[i], ada_b[i])]
        csh_m, csc_m, cg_m, csh_f, csc_f, cg_f = adaln(c_ctx, ada_w1[i], ada_w2[i], ada_b[i])
        h_lat = modulate(x_lat, sh_m, sc_m)
        h_ctx = modulate(x_ctx, csh_m, csc_m)
        j = i // N_MIXERS
        if i % N_MIXERS == 0:
            y_lat, y_ctx = neighbourhood_attention(h_lat, h_ctx, na_w_qkv[j], na_w_o[j], na_rpb[j], not last)
        else:
            y_lat, y_ctx = gla_mixer(h_lat, h_ctx, gla_w_in[j], gla_w_a1[j], gla_w_a2[j], gla_b_a[j],
                                     gla_norm[j], gla_w_o[j], not last)
        x_lat = layernorm(DEEPNORM_ALPHA * x_lat + g_m * y_lat, ln_g[i, 0], ln_b[i, 0])
        moe_args = (moe_router[i], moe_bias[i], moe_w_gate[i], moe_w_up[i], moe_w_down[i],
                    sh_w_gate[i], sh_w_up[i], sh_w_down[i])
        if last:
            y_lat = moe_ffn(modulate(x_lat, sh_f, sc_f), *moe_args)
        else:
            x_ctx = layernorm(DEEPNORM_ALPHA * x_ctx + cg_m * y_ctx, ln_g[i, 0], ln_b[i, 0])
            h = jnp.concatenate([modulate(x_ctx, csh_f, csc_f), modulate(x_lat, sh_f, sc_f)], axis=1)
            y = moe_ffn(h, *moe_args)
            y_ctx, y_lat = y[:, :n_ctx], y[:, n_ctx:]
            x_ctx = layernorm(DEEPNORM_ALPHA * x_ctx + cg_f * y_ctx, ln_g[i, 1], ln_b[i, 1])
        x_lat = layernorm(DEEPNORM_ALPHA * x_lat + g_f * y_lat, ln_g[i, 1], ln_b[i, 1])
    return x_lat
```

```python
import numpy as np
import ml_dtypes
from contextlib import ExitStack
import concourse.bass as bass
import concourse.mybir as mybir
from concourse.bass_utils import run_bass_kernel_spmd

F32 = mybir.dt.float32
BF16 = mybir.dt.bfloat16
AF = mybir.ActivationFunctionType
ALU = mybir.AluOpType
AX = mybir.AxisListType
NPBF = ml_dtypes.bfloat16


class CFG:
    D = 4096
    B = 2
    SEQ = 8192
    DEPTH = 4
    GRID_W = 64
    CTX = 256
    NA_H = 32
    NA_KH = 8
    NA_KW = 16
    GLA_H = 8
    GLA_RANK = 16
    GLA_L = 64
    NE = 64
    HD = 192
    TOPK = 8
    NG = 8
    TOPG = 4
    RSCALE = 2.5
    ADA_R = 256
    EPS = 1e-6


ALPHA = (2 * CFG.DEPTH) ** 0.25


class Res:
    __slots__ = ("w", "rs", "name", "sem", "dcnt", "key")
    _n = 0

    def __init__(self, name=""):
        self.w = None
        self.rs = {}
        self.name = name
        self.sem = None
        self.dcnt = 0
        Res._n += 1
        self.key = ("r", Res._n)


class Sched:
    def __init__(self, nc, es):
        self.nc = nc
        self.es = es
        self.eng = {"pe": nc.tensor, "act": nc.scalar, "dve": nc.vector, "pool": nc.gpsimd, "sp": nc.sync}
        self.sem = {}
        self.cnt = {}
        for k in ("pe", "act", "dve", "pool"):
            self.sem[k] = es.enter_context(nc.semaphore("s_" + k))
            self.cnt[k] = 0
        self.waited = {k: {} for k in self.eng}
        self.dres = []
        self.nsem = 4
        self.tes = es

    def _need(self, E, dep):
        key, val = dep
        if isinstance(key, str):
            if key == E and val > self.cnt[E]:
                return
            semh = self.sem[key]
        else:
            r = key
            val = r.dcnt
            semh = r.sem
            key = r.key
        if self.waited[E].get(key, 0) >= val:
            return
        self.eng[E].wait_ge(semh, val)
        self.waited[E][key] = val

    def _deps(self, E, reads, writes):
        for r in reads:
            if r.w is not None:
                self._need(E, r.w)
        for w in writes:
            if w.w is not None:
                self._need(E, w.w)
            for k, v in w.rs.items():
                self._need(E, (k, v))

    def _tag(self, tag, reads, writes):
        k, v = tag
        for r in reads:
            if r.rs.get(k, 0) < v:
                r.rs[k] = v
        for w in writes:
            w.w = tag
            w.rs = {}

    def op(self, E, fn, reads=(), writes=(), inc=True):
        self._deps(E, reads, writes)
        ins = fn(self.eng[E])
        if inc:
            self.cnt[E] += 1
            ins.then_inc(self.sem[E], 1)
            tag = (E, self.cnt[E])
        else:
            tag = (E, self.cnt[E] + 1)
        self._tag(tag, reads, writes)
        return ins

    def dma(self, Q, out, in_, reads=(), writes=(), prim=None):
        self._deps(Q, reads, writes)
        p = prim if prim is not None else (writes[0] if writes else reads[0])
        if p.sem is None:
            p.sem = self.es.enter_context(self.nc.semaphore("d%d" % len(self.dres)))
            self.dres.append(p)
            self.nsem += 1
            assert self.nsem < 95, "too many semaphores"
        ins = self.eng[Q].dma_start(out=out, in_=in_)
        p.dcnt += 16
        ins.then_inc(p.sem, 16)
        self._tag((p, p.dcnt), reads, writes)
        return ins

    def barrier(self):
        for E in ("pe", "act", "dve", "pool", "sp"):
            for k in ("pe", "act", "dve", "pool"):
                if k != E and self.cnt[k] > 0:
                    self._need(E, (k, self.cnt[k]))
            for r in self.dres:
                self._need(E, (r, r.dcnt))

    def finish(self):
        for k in ("pe", "act", "dve", "pool"):
            if self.cnt[k] > 0:
                self._need("sp", (k, self.cnt[k]))
        for r in self.dres:
            self._need("sp", (r, r.dcnt))


class T:
    def __init__(self, S, shape, dt, name, psum=False):
        nc = S.nc
        self.t = S.tes.enter_context(nc.psum_tensor(name, shape, dt) if psum else nc.sbuf_tensor(name, shape, dt))
        self.r = Res(name)

    def __getitem__(self, k):
        return self.t[k]


class Rot:
    def __init__(self, tiles):
        self.tiles = tiles
        self.i = 0

    def next(self):
        t = self.tiles[self.i % len(self.tiles)]
        self.i += 1
        return t


def build_ffn(tiles, D=CFG.D, NE=CFG.NE, HD=CFG.HD, last_layer_out=True):
    KC = D // 128
    Ttot = sum(t[1] for t in tiles)
    G = NE // CFG.NG
    NPAIR = NE // 2
    NCH = 3 * NPAIR + 2
    NTM = 512
    nc = bass.Bass("TRN2", target_bir_lowering=False)
    dr = lambda n, s, d, k="ExternalInput": nc.dram_tensor(n, s, d, kind=k).ap()
    xT = dr("xT", [D, Ttot], F32)
    oT = dr("oT", [D, Ttot], BF16)
    wo = dr("wo", [D, D], F32)
    cols = dr("cols", [128, 12 * KC], F32)
    wr = dr("wr", [D, NE], F32)
    eb = dr("eb", [1, NE], F32)
    wg = dr("wg", [NE, D, HD], F32)
    wu = dr("wu", [NE, D, HD], F32)
    wd = dr("wd", [NE * HD, D], F32)
    sg = dr("sg", [D, HD], F32)
    su = dr("su", [D, HD], F32)
    sd = dr("sd", [HD, D], F32)
    ident_in = dr("ident_in", [128, 128], F32)
    x2T = dr("x2T", [D, Ttot], F32, "ExternalOutput")
    r_scr = dr("r_scr", [D, NTM], F32, "Internal")
    x1_scr = dr("x1_scr", [D, NTM], F32, "Internal")

    with ExitStack() as es:
        S = Sched(nc, es)
        hT = T(S, [128, KC, NTM], BF16, "hT")
        hid = T(S, [128, NCH, NTM], BF16, "hid")
        hid_rs = [Res("hid%d" % i) for i in range(NCH)]
        USZ = max(16 * 384, (NCH + 1) // 2 * 128, KC * 128)
        arena = Rot([T(S, [128, USZ], BF16, "ar%d" % i) for i in range(3)])
        f32t = Rot([T(S, [128, NTM], F32, "f%d" % i) for i in range(8)])
        mean = T(S, [128, NTM], F32, "mean")
        rstd = T(S, [128, NTM], F32, "rstd")
        colt = T(S, [128, 12 * KC], F32, "colt")
        ident = T(S, [128, 128], BF16, "ident")
        ones32 = T(S, [128, 128], F32, "ones32")
        wrt = T(S, [128, KC, NE], BF16, "wrt")
        ebt = T(S, [128, NE], F32, "ebt")
        gT = T(S, [64, NTM], BF16, "gT")
        sm = Rot([T(S, [128, NE], F32, "sm%d" % i) for i in range(6)])
        sm8 = Rot([T(S, [128, 8], F32, "sm8_%d" % i) for i in range(6)])
        gbf = T(S, [128, NE], BF16, "gbf")
        pb = [T(S, [128, 512], F32, "pb%d" % i, psum=True) for i in range(7)]
        pbt = T(S, [128, 1024], BF16, "pbt", psum=True)

        S.dma("sp", colt[:], cols, writes=[colt.r])
        S.dma("pool", ident[:], ident_in, writes=[ident.r])
        S.op("dve", lambda e: e.memset(ones32[:], 1.0), writes=[ones32.r])
        S.dma("pool", wrt[:], wr.rearrange("(kc p) n -> p kc n", p=128), writes=[wrt.r])
        S.dma("sp", ebt[:], eb.partition_broadcast(128), writes=[ebt.r])
        for c in (1, 5):
            S.op("dve", lambda e, c=c: e.tensor_scalar_add(colt[:, c * KC:(c + 1) * KC], colt[:, c * KC:(c + 1) * KC], 1.0),
                 reads=[colt.r], writes=[colt.r])
        col = lambda idx, kc: colt[:, idx * KC + kc: idx * KC + kc + 1]

        def ln_stats(nt, s1, s2):
            m_ = mean[:, :nt]
            r_ = rstd[:, :nt]
            S.op("dve", lambda e: e.tensor_scalar_mul(m_, s1[:, :nt], 1.0 / D), reads=[s1.r], writes=[mean.r])
            tmp = f32t.next()
            S.op("dve", lambda e: e.tensor_tensor(out=tmp[:, :nt], in0=m_, in1=m_, op=ALU.mult), reads=[mean.r], writes=[tmp.r])
            S.op("dve", lambda e: e.scalar_tensor_tensor(out=tmp[:, :nt], in0=s2[:, :nt], scalar=1.0 / D, in1=tmp[:, :nt],
                                                         op0=ALU.mult, op1=ALU.subtract), reads=[s2.r, tmp.r], writes=[tmp.r])
            S.op("dve", lambda e: e.tensor_scalar_add(tmp[:, :nt], tmp[:, :nt], float(CFG.EPS)), reads=[tmp.r], writes=[tmp.r])
            S.op("act", lambda e: e.activation(out=tmp[:, :nt], in_=tmp[:, :nt], func=AF.Sqrt), reads=[tmp.r], writes=[tmp.r])
            S.op("dve", lambda e: e.reciprocal(out=r_, in_=tmp[:, :nt]), reads=[tmp.r], writes=[rstd.r])

        def stats_acc(rc, nt, mc, s1, s2):
            S.op("pe", lambda e: e.matmul(s1[:, :nt], ones32[:], rc[:, :nt], start=(mc == 0), stop=(mc == KC - 1)),
                 reads=[ones32.r, rc.r], writes=[s1.r], inc=(mc == KC - 1))
            sq = f32t.next()
            S.op("act", lambda e: e.activation(out=sq[:, :nt], in_=rc[:, :nt], func=AF.Square), reads=[rc.r], writes=[sq.r])
            S.op("pe", lambda e: e.matmul(s2[:, :nt], ones32[:], sq[:, :nt], start=(mc == 0), stop=(mc == KC - 1)),
                 reads=[ones32.r, sq.r], writes=[s2.r], inc=True)

        for (t0, nt, cond) in tiles:
            cb = 4 * cond
            oTt = hid.t[:, 0:KC, :]
            S.dma("sp", oTt[:, :, :nt], oT.rearrange("(kc p) t -> p kc t", p=128)[:, :, t0:t0 + nt], writes=hid_rs[:KC])
            s1, s2 = pb[2], pb[3]
            for mc in range(KC):
                u = arena.next()
                uv = u.t[:, 0:KC * 128].rearrange("p (kc n) -> p kc n", n=128)
                S.dma("pool", uv, wo.rearrange("(kc p) n -> p kc n", p=128)[:, :, mc * 128:(mc + 1) * 128], writes=[u.r])
                yp = pb[mc % 2]
                for kc in range(KC):
                    S.op("pe", lambda e: e.matmul(yp[:, :nt], uv[:, kc, :], oTt[:, kc, :nt], start=(kc == 0), stop=(kc == KC - 1)),
                         reads=[u.r] + hid_rs[:KC], writes=[yp.r], inc=(kc == KC - 1))
                xc = f32t.next()
                S.dma("sp", xc[:, :nt], xT[mc * 128:(mc + 1) * 128, t0:t0 + nt], writes=[xc.r])
                S.op("act", lambda e: e.activation(out=xc[:, :nt], in_=xc[:, :nt], func=AF.Copy, scale=float(ALPHA)), reads=[xc.r], writes=[xc.r])
                rc = f32t.next()
                S.op("dve", lambda e: e.scalar_tensor_tensor(out=rc[:, :nt], in0=yp[:, :nt], scalar=col(cb + 0, mc), in1=xc[:, :nt],
                                                             op0=ALU.mult, op1=ALU.add), reads=[yp.r, xc.r, colt.r], writes=[rc.r])
                stats_acc(rc, nt, mc, s1, s2)
                S.dma("sp", r_scr[mc * 128:(mc + 1) * 128, 0:nt], rc[:, :nt], reads=[rc.r], prim=rc.r)
            ln_stats(nt, s1, s2)
            S.barrier()
            for mc in range(KC):
                rc = f32t.next()
                S.dma("sp", rc[:, :nt], r_scr[mc * 128:(mc + 1) * 128, 0:nt], writes=[rc.r])
                S.op("dve", lambda e: e.tensor_tensor(out=rc[:, :nt], in0=rc[:, :nt], in1=mean[:, :nt], op=ALU.subtract),
                     reads=[rc.r, mean.r], writes=[rc.r])
                S.op("pool", lambda e: e.tensor_tensor(out=rc[:, :nt], in0=rc[:, :nt], in1=rstd[:, :nt], op=ALU.mult),
                     reads=[rc.r, rstd.r], writes=[rc.r])
                x1c = f32t.next()
                S.op("act", lambda e: e.activation(out=x1c[:, :nt], in_=rc[:, :nt], func=AF.Identity, scale=col(8, mc), bias=col(9, mc)),
                     reads=[rc.r, colt.r], writes=[x1c.r])
                S.dma("sp", x1_scr[mc * 128:(mc + 1) * 128, 0:nt], x1c[:, :nt], reads=[x1c.r], prim=x1c.r)
                S.op("dve", lambda e: e.tensor_scalar(out=hT[:, mc, :nt], in0=x1c[:, :nt], scalar1=col(cb + 1, mc), scalar2=col(cb + 2, mc),
                                                      op0=ALU.mult, op1=ALU.add), reads=[x1c.r, colt.r], writes=[hT.r])
            S.barrier()
            ntc = (nt + 127) // 128
            for tc in range(ntc):
                tn = min(128, nt - tc * 128)
                lp = pb[4 + tc % 2]
                for kc in range(KC):
                    S.op("pe", lambda e: e.matmul(lp[:tn, :NE], hT[:, kc, tc * 128:tc * 128 + tn], wrt[:, kc, :], start=(kc == 0), stop=(kc == KC - 1)),
                         reads=[hT.r, wrt.r], writes=[lp.r], inc=(kc == KC - 1))
                sc = sm.next()
                S.op("act", lambda e: e.activation(out=sc[:tn, :], in_=lp[:tn, :NE], func=AF.Sigmoid), reads=[lp.r], writes=[sc.r])
                bi = sm.next()
                S.op("dve", lambda e: e.tensor_tensor(out=bi[:tn, :], in0=sc[:tn, :], in1=ebt[:tn, :], op=ALU.add), reads=[sc.r, ebt.r], writes=[bi.r])
                bi3 = bi[:tn, :].rearrange("p (g j) -> p g j", j=G)
                m1 = sm8.next()
                S.op("dve", lambda e: e.tensor_reduce(out=m1[:tn, :], in_=bi3, axis=AX.X, op=ALU.max), reads=[bi.r], writes=[m1.r])
                eq = sm.next()
                eq3 = eq[:tn, :].rearrange("p (g j) -> p g j", j=G)
                S.op("dve", lambda e: e.tensor_tensor(out=eq3, in0=bi3, in1=m1[:tn, :].unsqueeze(2).broadcast_to([tn, CFG.NG, G]), op=ALU.is_equal),
                     reads=[bi.r, m1.r], writes=[eq.r])
                S.op("dve", lambda e: e.scalar_tensor_tensor(out=eq[:tn, :], in0=eq[:tn, :], scalar=-100.0, in1=bi[:tn, :], op0=ALU.mult, op1=ALU.add),
                     reads=[eq.r, bi.r], writes=[eq.r])
                m2 = sm8.next()
                S.op("dve", lambda e: e.tensor_reduce(out=m2[:tn, :], in_=eq3, axis=AX.X, op=ALU.max), reads=[eq.r], writes=[m2.r])
                S.op("dve", lambda e: e.tensor_tensor(out=m2[:tn, :], in0=m2[:tn, :], in1=m1[:tn, :], op=ALU.add), reads=[m1.r, m2.r], writes=[m2.r])
                t8 = sm8.next()
                S.op("dve", lambda e: e.max(out=t8[:tn, :], in_=m2[:tn, :]), reads=[m2.r], writes=[t8.r])
                gm_ = sm8.next()
                S.op("dve", lambda e: e.tensor_scalar(out=gm_[:tn, :], in0=m2[:tn, :], scalar1=t8[:tn, CFG.TOPG - 1:CFG.TOPG], scalar2=None, op0=ALU.is_ge),
                     reads=[m2.r, t8.r], writes=[gm_.r])
                mk = sm.next()
                mk3 = mk[:tn, :].rearrange("p (g j) -> p g j", j=G)
                gmb = gm_[:tn, :].unsqueeze(2).broadcast_to([tn, CFG.NG, G])
                S.op("dve", lambda e: e.tensor_tensor(out=mk3, in0=bi3, in1=gmb, op=ALU.mult), reads=[bi.r, gm_.r], writes=[mk.r])
                pen = sm8.next()
                S.op("dve", lambda e: e.tensor_scalar(out=pen[:tn, :], in0=gm_[:tn, :], scalar1=100.0, scalar2=-100.0, op0=ALU.mult, op1=ALU.add),
                     reads=[gm_.r], writes=[pen.r])
                S.op("dve", lambda e: e.tensor_tensor(out=mk3, in0=mk3, in1=pen[:tn, :].unsqueeze(2).broadcast_to([tn, CFG.NG, G]), op=ALU.add),
                     reads=[mk.r, pen.r], writes=[mk.r])
                t8b = sm8.next()
                S.op("dve", lambda e: e.max(out=t8b[:tn, :], in_=mk[:tn, :]), reads=[mk.r], writes=[t8b.r])
                sel = sm.next()
                S.op("dve", lambda e: e.tensor_scalar(out=sel[:tn, :], in0=mk[:tn, :], scalar1=t8b[:tn, CFG.TOPK - 1:CFG.TOPK], scalar2=None, op0=ALU.is_ge),
                     reads=[mk.r, t8b.r], writes=[sel.r])
                S.op("dve", lambda e: e.tensor_tensor(out=sel[:tn, :], in0=sel[:tn, :], in1=sc[:tn, :], op=ALU.mult), reads=[sel.r, sc.r], writes=[sel.r])
                ws = sm8.next()
                S.op("dve", lambda e: e.tensor_reduce(out=ws[:tn, 0:1], in_=sel[:tn, :], axis=AX.X, op=ALU.add), reads=[sel.r], writes=[ws.r])
                S.op("dve", lambda e: e.reciprocal(out=ws[:tn, 1:2], in_=ws[:tn, 0:1]), reads=[ws.r], writes=[ws.r])
                S.op("dve", lambda e: e.tensor_scalar(out=gbf[:tn, :], in0=sel[:tn, :], scalar1=ws[:tn, 1:2], scalar2=float(CFG.RSCALE), op0=ALU.mult, op1=ALU.mult),
                     reads=[sel.r, ws.r], writes=[gbf.r])
                S.op("pe", lambda e: e.transpose(out=pbt[:NE, :tn], in_=gbf[:tn, :], identity=ident[:tn, :tn]), reads=[gbf.r, ident.r], writes=[pbt.r])
                S.op("dve", lambda e: e.tensor_copy(out=gT[:NE, tc * 128:tc * 128 + tn], in_=pbt[:NE, :tn]), reads=[pbt.r], writes=[gT.r])
            for p in range(NPAIR + 1):
                shared = (p == NPAIR)
                nm = 2 if shared else 3
                gps = [pb[0], pb[1], pb[2]]
                ups = [pb[3], pb[4], pb[5]]
                for which, wsrc, ssrc, pss in ((0, wg, sg, gps), (1, wu, su, ups)):
                    KH = max(1, KC // 2)
                    for k0 in range(0, KC, KH):
                        u = arena.next()
                        nk = min(KH, KC - k0)
                        ncol = HD if shared else 2 * HD
                        uv = u.t[:, 0:nk * ncol].rearrange("p (kc n) -> p kc n", n=ncol)
                        if shared:
                            S.dma("pool", uv, ssrc.rearrange("(kc p) n -> p kc n", p=128)[:, k0:k0 + nk, :], writes=[u.r])
                        else:
                            for j in range(2):
                                S.dma("pool", uv[:, :, j * HD:(j + 1) * HD], wsrc[2 * p + j].rearrange("(kc p) n -> p kc n", p=128)[:, k0:k0 + nk, :], writes=[u.r])
                        for m in range(nm):
                            mw = min(128, ncol - m * 128)
                            for kk in range(nk):
                                kc = k0 + kk
                                S.op("pe", lambda e: e.matmul(pss[m][:mw, :nt], uv[:, kk, m * 128:m * 128 + mw], hT[:, kc, :nt], start=(kc == 0), stop=(kc == KC - 1)),
                                     reads=[u.r, hT.r], writes=[pss[m].r], inc=(kk == nk - 1))
                gbs = []
                if not shared:
                    for j in range(2):
                        gb = pb[6]
                        e_ = 2 * p + j
                        selT = ident[0:NE, e_:e_ + 1].broadcast_to([NE, 128])
                        S.op("pe", lambda e: e.matmul(gb[:, :nt], selT, gT[0:NE, :nt], start=True, stop=True), reads=[ident.r, gT.r], writes=[gb.r])
                        gs_ = f32t.next()
                        S.op("act", lambda e: e.activation(out=gs_[:, :nt], in_=gb[:, :nt], func=AF.Copy), reads=[gb.r], writes=[gs_.r])
                        gbs.append(gs_)
                for m in range(nm):
                    c = 3 * p + m
                    mw = 128 if (not shared or m == 0) else HD - 128
                    sgl = f32t.next()
                    S.op("act", lambda e: e.activation(out=sgl[:mw, :nt], in_=gps[m][:mw, :nt], func=AF.Silu), reads=[gps[m].r], writes=[sgl.r])
                    if shared:
                        S.op("dve", lambda e: e.tensor_tensor(out=hid[:mw, c, :nt], in0=sgl[:mw, :nt], in1=ups[m][:mw, :nt], op=ALU.mult),
                             reads=[sgl.r, ups[m].r], writes=[hid_rs[c]])
                        continue
                    S.op("dve", lambda e: e.tensor_tensor(out=sgl[:, :nt], in0=sgl[:, :nt], in1=ups[m][:, :nt], op=ALU.mult),
                         reads=[sgl.r, ups[m].r], writes=[sgl.r])
                    if m == 1:
                        for j in range(2):
                            S.op("pool", lambda e: e.tensor_tensor(out=hid[64 * j:64 * j + 64, c, :nt], in0=sgl[64 * j:64 * j + 64, :nt],
                                                                   in1=gbs[j][64 * j:64 * j + 64, :nt], op=ALU.mult),
                                 reads=[sgl.r, gbs[j].r], writes=[hid_rs[c]])
                    else:
                        g_ = gbs[0 if m == 0 else 1]
                        S.op("pool", lambda e: e.tensor_tensor(out=hid[:, c, :nt], in0=sgl[:, :nt], in1=g_[:, :nt], op=ALU.mult),
                             reads=[sgl.r, g_.r], writes=[hid_rs[c]])
            s1, s2 = pb[2], pb[3]
            NR = 3 * NPAIR
            H1 = (NCH + 1) // 2
            for mc in range(KC):
                yp = pb[mc % 2]
                for half in range(2):
                    c0 = half * H1
                    c1 = min(NCH, c0 + H1)
                    u = arena.next()
                    uv = u.t[:, 0:(c1 - c0) * 128].rearrange("p (c n) -> p c n", n=128)
                    rc1 = min(c1, NR)
                    if rc1 > c0:
                        S.dma("pool", uv[:, 0:rc1 - c0, :], wd.rearrange("(c p) n -> p c n", p=128)[:, c0:rc1, mc * 128:(mc + 1) * 128], writes=[u.r])
                    if c1 > NR:
                        S.dma("pool", uv[:, NR - c0, :], sd[0:128, mc * 128:(mc + 1) * 128], writes=[u.r])
                        S.dma("pool", uv[0:HD - 128, NR + 1 - c0, :], sd[128:HD, mc * 128:(mc + 1) * 128], writes=[u.r])
                    for c in range(c0, c1):
                        kr = 128 if c != NCH - 1 else HD - 128
                        S.op("pe", lambda e: e.matmul(yp[:, :nt], uv[:kr, c - c0, :], hid[:kr, c, :nt], start=(c == 0), stop=(c == NCH - 1)),
                             reads=[u.r, hid_rs[c]], writes=[yp.r], inc=(c == c1 - 1))
                xc = f32t.next()
                S.dma("sp", xc[:, :nt], x1_scr[mc * 128:(mc + 1) * 128, 0:nt], writes=[xc.r])
                S.op("act", lambda e: e.activation(out=xc[:, :nt], in_=xc[:, :nt], func=AF.Copy, scale=float(ALPHA)), reads=[xc.r], writes=[xc.r])
                rc = f32t.next()
                S.op("dve", lambda e: e.scalar_tensor_tensor(out=rc[:, :nt], in0=yp[:, :nt], scalar=col(cb + 3, mc), in1=xc[:, :nt],
                                                             op0=ALU.mult, op1=ALU.add), reads=[yp.r, xc.r, colt.r], writes=[rc.r])
                stats_acc(rc, nt, mc, s1, s2)
                S.dma("sp", r_scr[mc * 128:(mc + 1) * 128, 0:nt], rc[:, :nt], reads=[rc.r], prim=rc.r)
            ln_stats(nt, s1, s2)
            S.barrier()
            for mc in range(KC):
                rc = f32t.next()
                S.dma("sp", rc[:, :nt], r_scr[mc * 128:(mc + 1) * 128, 0:nt], writes=[rc.r])
                S.op("dve", lambda e: e.tensor_tensor(out=rc[:, :nt], in0=rc[:, :nt], in1=mean[:, :nt], op=ALU.subtract),
                     reads=[rc.r, mean.r], writes=[rc.r])
                S.op("pool", lambda e: e.tensor_tensor(out=rc[:, :nt], in0=rc[:, :nt], in1=rstd[:, :nt], op=ALU.mult),
                     reads=[rc.r, rstd.r], writes=[rc.r])
                x2c = f32t.next()
                S.op("act", lambda e: e.activation(out=x2c[:, :nt], in_=rc[:, :nt], func=AF.Identity, scale=col(10, mc), bias=col(11, mc)),
                     reads=[rc.r, colt.r], writes=[x2c.r])
                S.dma("sp", x2T[mc * 128:(mc + 1) * 128, t0:t0 + nt], x2c[:, :nt], reads=[x2c.r], prim=x2c.r)
            S.barrier()
        S.finish()
    return nc


def na_geometry(ROWS, KH=CFG.NA_KH):
    geo, keys, reps = [], {}, []
    for rp in range(ROWS // 2):
        s0 = min(max(2 * rp - KH // 2, 0), ROWS - KH)
        s1 = min(max(2 * rp + 1 - KH // 2, 0), ROWS - KH)
        cl, ch = s0 // 2, (s1 + KH - 1) // 2
        key = (s0 - 2 * rp, s1 - 2 * rp, cl - rp, ch - cl)
        if key not in keys:
            keys[key] = len(reps)
            reps.append(rp)
        geo.append((cl, ch - cl + 1, keys[key]))
    return geo, reps


def na_bias_tables(rpb, ROWS, W=CFG.GRID_W, KH=CFG.NA_KH, KW=CFG.NA_KW, NEG=-30000.0):
    geo, reps = na_geometry(ROWS, KH)
    H = rpb.shape[0]
    out = np.full((H, 128, len(reps), 5, 128), NEG, np.float32)
    kk = np.arange(128)
    qq = np.arange(128)
    for v, rp in enumerate(reps):
        cl, nb, _ = geo[rp]
        qrow = 2 * rp + qq // W
        qcol = qq % W
        rs = np.clip(qrow - KH // 2, 0, ROWS - KH)
        cs = np.clip(qcol - KW // 2, 0, W - KW)
        for ci in range(nb):
            krow = 2 * (cl + ci) + kk // W
            kcol = kk % W
            ok = ((krow[:, None] >= rs[None, :]) & (krow[:, None] < rs[None, :] + KH)
                  & (kcol[:, None] >= cs[None, :]) & (kcol[:, None] < cs[None, :] + KW))
            dr = np.clip(krow[:, None] - qrow[None, :] + KH - 1, 0, 2 * KH - 2)
            dc = np.clip(kcol[:, None] - qcol[None, :], 1 - KW, KW - 1) + KW - 1
            g = rpb[:, dr, dc]
            out[:, :, v, ci, :] = np.where(ok[None], g, np.float32(NEG))
    return out


def build_na(D=CFG.D, NHC=4, ROWS=128, W=CFG.GRID_W, CTXN=CFG.CTX, B=CFG.B, debug=False):
    KC = D // 128
    L = ROWS * W
    TT = L + CTXN
    NCHK = TT // 128
    LCH = L // 128
    NCC = CTXN // 128
    geo, reps = na_geometry(ROWS)
    NV = len(reps)
    scale = float(128 ** -0.5)
    nc = bass.Bass("TRN2", target_bir_lowering=False)
    dr = lambda n, s, d, k="ExternalInput": nc.dram_tensor(n, s, d, kind=k).ap()
    xT = dr("xT", [B, D, TT], F32)
    cols = dr("cols", [128, 6 * KC], F32)
    wqkv = dr("wqkv", [D, 3 * NHC * 128], F32)
    bias = dr("bias", [NHC, 128, NV * 5 * 128], F32)
    oT = dr("oT", [B, NHC * 128, TT], BF16, "ExternalOutput")
    qk_scr = dr("qk_scr", [B, 2 * NHC, 128, TT], BF16, "ExternalOutput" if debug else "Internal")
    v_scr = dr("v_scr", [B, NHC, 128, NCHK, 128], BF16, "ExternalOutput" if debug else "Internal")
    tiles = [(t0, min(512, L - t0), 0) for t0 in range(0, L, 512)] + [(L, CTXN, 1)]
    with ExitStack() as es:
        S = Sched(nc, es)
        pb = [T(S, [128, 512], F32, "pb%d" % i, psum=True) for i in range(4)]
        st2 = [T(S, [128, 1024], F32, "st%d" % i, psum=True) for i in range(2)]
        colt = T(S, [128, 6 * KC], F32, "colt")
        ones_bf = T(S, [128, 128], BF16, "ones_bf")
        S.dma("sp", colt[:], cols, writes=[colt.r])
        S.op("dve", lambda e: e.memset(ones_bf[:], 1.0), writes=[ones_bf.r])
        for c in range(3):
            S.op("dve", lambda e, c=c: e.tensor_scalar_add(colt[:, (2 * c + 1) * KC:(2 * c + 2) * KC], colt[:, (2 * c + 1) * KC:(2 * c + 2) * KC], 1.0),
                 reads=[colt.r], writes=[colt.r])
        col = lambda idx, kc: colt[:, idx * KC + kc: idx * KC + kc + 1]
        with ExitStack() as es1:
            S.tes = es1
            Wt = T(S, [128, KC, 3 * NHC * 128], BF16, "Wt")
            hT = T(S, [128, KC, 512], BF16, "hT")
            xcs = Rot([T(S, [128, 512], F32, "xc%d" % i) for i in range(4)])
            obs = Rot([T(S, [128, 512], BF16, "ob%d" % i) for i in range(4)])
            for j in range(3):
                S.dma("pool", Wt[:, :, j * NHC * 128:(j + 1) * NHC * 128],
                      wqkv.rearrange("(kc p) n -> p kc n", p=128)[:, :, j * NHC * 128:(j + 1) * NHC * 128], writes=[Wt.r])
            for b in range(B):
                for (t0, nt, isctx) in tiles:
                    cnd = 2 if isctx else b
                    for kc in range(KC):
                        xc = xcs.next()
                        S.dma("sp", xc[:, :nt], xT[b, kc * 128:(kc + 1) * 128, t0:t0 + nt], writes=[xc.r])
                        S.op("dve", lambda e: e.tensor_scalar(out=hT[:, kc, :nt], in0=xc[:, :nt], scalar1=col(2 * cnd + 1, kc), scalar2=col(2 * cnd, kc),
                                                              op0=ALU.mult, op1=ALU.add), reads=[xc.r, colt.r], writes=[hT.r])
                    for j in range(2 * NHC):
                        ps = pb[j % 2]
                        for kc in range(KC):
                            S.op("pe", lambda e: e.matmul(ps[:, :nt], Wt[:, kc, j * 128:(j + 1) * 128], hT[:, kc, :nt], start=(kc == 0), stop=(kc == KC - 1)),
                                 reads=[Wt.r, hT.r], writes=[ps.r], inc=(kc == KC - 1))
                        ob = obs.next()
                        S.op("act", lambda e: e.activation(out=ob[:, :nt], in_=ps[:, :nt], func=AF.Copy), reads=[ps.r], writes=[ob.r])
                        S.dma("sp", qk_scr[b, j, :, t0:t0 + nt], ob[:, :nt], reads=[ob.r], prim=ob.r)
                    for tc in range(nt // 128):
                        ps = pb[2 + tc % 2]
                        for kc in range(KC):
                            S.op("pe", lambda e: e.matmul(ps[:, :NHC * 128], hT[:, kc, tc * 128:(tc + 1) * 128], Wt[:, kc, 2 * NHC * 128:3 * NHC * 128],
                                                          start=(kc == 0), stop=(kc == KC - 1)), reads=[Wt.r, hT.r], writes=[ps.r], inc=(kc == KC - 1))
                        ob = obs.next()
                        S.op("act", lambda e: e.activation(out=ob[:, :NHC * 128], in_=ps[:, :NHC * 128], func=AF.Copy), reads=[ps.r], writes=[ob.r])
                        chk = (t0 + tc * 128) // 128
                        S.dma("sp", v_scr[b, :, :, chk, :].rearrange("h p n -> p h n"), ob[:, :NHC * 128].rearrange("p (h n) -> p h n", n=128),
                              reads=[ob.r], prim=ob.r)
            S.barrier()
        with ExitStack() as es2:
            S.tes = es2
            qTs = Rot([T(S, [128, TT], BF16, "qT%d" % i) for i in range(2)])
            kTs = Rot([T(S, [128, TT], BF16, "kT%d" % i) for i in range(2)])
            Vs = Rot([T(S, [128, NCHK, 128], BF16, "V%d" % i) for i in range(2)])
            oTs = Rot([T(S, [128, TT], BF16, "oTh%d" % i) for i in range(2)])
            bts = Rot([T(S, [128, NV * 5 * 128], F32, "bt%d" % i) for i in range(2)])
            tbs = Rot([T(S, [128, 640], F32, "tb%d" % i) for i in range(2)])
            pTs = Rot([T(S, [128, 1024], BF16, "pT%d" % i) for i in range(2)])
            recs = Rot([T(S, [128, 256], F32, "rec%d" % i) for i in range(2)])
            pos = Rot([pb[0], pb[1]])
            pds = Rot([pb[2], pb[3]])
            sts = Rot(st2)
            for b in range(B):
                for h in range(NHC):
                    qT, kT, V, oTh, bt = qTs.next(), kTs.next(), Vs.next(), oTs.next(), bts.next()
                    S.dma("sp", qT[:], qk_scr[b, h], writes=[qT.r])
                    S.dma("sp", kT[:], qk_scr[b, NHC + h], writes=[kT.r])
                    S.dma("sp", V[:], v_scr[b, h], writes=[V.r])
                    S.dma("sp", bt[:], bias[h], writes=[bt.r])
                    for rp in range(ROWS // 2):
                        cl, nb, var = geo[rp]
                        st, po, pd, tb, pT, rec = sts.next(), pos.next(), pds.next(), tbs.next(), pTs.next(), recs.next()
                        qsl = qT[:, rp * 128:(rp + 1) * 128]
                        for ci in range(nb):
                            S.op("pe", lambda e: e.matmul(st[:, ci * 128:(ci + 1) * 128], kT[:, (cl + ci) * 128:(cl + ci + 1) * 128], qsl, start=True, stop=True),
                                 reads=[kT.r, qT.r], writes=[st.r], inc=False)
                        for cj in range(NCC):
                            S.op("pe", lambda e: e.matmul(st[:, (5 + cj) * 128:(6 + cj) * 128], kT[:, L + cj * 128:L + (cj + 1) * 128], qsl, start=True, stop=True),
                                 reads=[kT.r, qT.r], writes=[st.r], inc=(cj == NCC - 1))
                        S.op("dve", lambda e: e.scalar_tensor_tensor(out=tb[:, :nb * 128], in0=st[:, :nb * 128], scalar=scale,
                                                                     in1=bt[:, var * 640:var * 640 + nb * 128], op0=ALU.mult, op1=ALU.add),
                             reads=[st.r, bt.r], writes=[tb.r])
                        S.op("act", lambda e: e.activation(out=pT[:, :nb * 128], in_=tb[:, :nb * 128], func=AF.Exp), reads=[tb.r], writes=[pT.r])
                        S.op("act", lambda e: e.activation(out=pT[:, 640:640 + CTXN], in_=st[:, 640:640 + CTXN], func=AF.Exp, scale=scale), reads=[st.r], writes=[pT.r])
                        slots = [(cl + ci, ci) for ci in range(nb)] + [(LCH + cj, 5 + cj) for cj in range(NCC)]
                        for i, (kchunk, slot) in enumerate(slots):
                            S.op("pe", lambda e: e.matmul(po[:, 0:128], V[:, kchunk, :], pT[:, slot * 128:(slot + 1) * 128], start=(i == 0), stop=(i == len(slots) - 1)),
                                 reads=[V.r, pT.r], writes=[po.r], inc=False)
                            S.op("pe", lambda e: e.matmul(pd[:, 0:128], ones_bf[:], pT[:, slot * 128:(slot + 1) * 128], start=(i == 0), stop=(i == len(slots) - 1)),
                                 reads=[ones_bf.r, pT.r], writes=[pd.r], inc=(i == len(slots) - 1))
                        S.op("dve", lambda e: e.reciprocal(out=rec[:, 0:128], in_=pd[:, 0:128]), reads=[pd.r], writes=[rec.r])
                        S.op("dve", lambda e: e.tensor_tensor(out=oTh[:, rp * 128:(rp + 1) * 128], in0=po[:, 0:128], in1=rec[:, 0:128], op=ALU.mult),
                             reads=[po.r, rec.r], writes=[oTh.r])
                    st, po, pd, pT, rec = sts.next(), pos.next(), pds.next(), pTs.next(), recs.next()
                    for cj in range(NCC):
                        S.op("pe", lambda e: e.matmul(st[:, cj * CTXN:(cj + 1) * CTXN], kT[:, L + cj * 128:L + (cj + 1) * 128], qT[:, L:L + CTXN], start=True, stop=True),
                             reads=[kT.r, qT.r], writes=[st.r], inc=(cj == NCC - 1))
                    S.op("act", lambda e: e.activation(out=pT[:, :NCC * CTXN], in_=st[:, :NCC * CTXN], func=AF.Exp, scale=scale), reads=[st.r], writes=[pT.r])
                    for cj in range(NCC):
                        S.op("pe", lambda e: e.matmul(po[:, 0:CTXN], V[:, LCH + cj, :], pT[:, cj * CTXN:(cj + 1) * CTXN], start=(cj == 0), stop=(cj == NCC - 1)),
                             reads=[V.r, pT.r], writes=[po.r], inc=False)
                        S.op("pe", lambda e: e.matmul(pd[:, 0:CTXN], ones_bf[:], pT[:, cj * CTXN:(cj + 1) * CTXN], start=(cj == 0), stop=(cj == NCC - 1)),
                             reads=[ones_bf.r, pT.r], writes=[pd.r], inc=(cj == NCC - 1))
                    S.op("dve", lambda e: e.reciprocal(out=rec[:, 0:CTXN], in_=pd[:, 0:CTXN]), reads=[pd.r], writes=[rec.r])
                    S.op("dve", lambda e: e.tensor_tensor(out=oTh[:, L:L + CTXN], in0=po[:, 0:CTXN], in1=rec[:, 0:CTXN], op=ALU.mult),
                         reads=[po.r, rec.r], writes=[oTh.r])
                    S.dma("sp", oT[b, h * 128:(h + 1) * 128, :], oTh[:], reads=[oTh.r], prim=oTh.r)
            S.barrier()
        S.tes = es
        S.finish()
    return nc


def gla_consts(ROWS, W=CFG.GRID_W):
    L = ROWS * W
    nf = 64
    inv = (np.float32(10000.0) ** (-np.arange(nf, dtype=np.float32) / np.float32(nf))).astype(np.float32)
    pos = np.arange(L)
    p = np.arange(128)
    cos = np.zeros((128, 2, L), np.float32)
    sin = np.zeros((128, 2, L), np.float32)
    for ch, pp in enumerate((pos // W, pos % W)):
        ang = pp.astype(np.float32)[None, :] * inv[p % 64][:, None]
        cos[:, ch, :] = np.cos(ang)
        sin[:, ch, :] = np.sin(ang) * np.where(p < 64, -1.0, 1.0)[:, None].astype(np.float32)
    perm = np.zeros((128, 128), np.float32)
    perm[(p + 64) % 128, p] = 1.0
    si = np.arange(64)
    msk = np.zeros((64, 2, 64), np.float32)
    msk[:, 0, :] = (si[:, None] <= si[None, :])
    msk[:, 1, :] = (si[:, None] >= si[None, :])
    rmask = np.ones((128, 512), np.float32)
    rmask[:, ::64] = 0.0
    return cos, sin, perm, msk, rmask


def build_gla(D=CFG.D, ROWS=128, W=CFG.GRID_W, CTXN=CFG.CTX, B=CFG.B, dbg=0):
    KC = D // 128
    L = ROWS * W
    TT = L + CTXN
    NCK = TT // 64
    DK, DV = 256, 512
    nc = bass.Bass("TRN2", target_bir_lowering=False)
    dr = lambda n, s, d, k="ExternalInput": nc.dram_tensor(n, s, d, kind=k).ap()
    xT = dr("xT", [B, D, TT], F32)
    cols = dr("cols", [128, 6 * KC], F32)
    win = dr("win", [D, 1536], F32)
    wa1 = dr("wa1", [D, 32], F32)
    wa2p = dr("wa2p", [32, 2, 256], F32)
    ba = dr("ba", [128, 4], F32)
    nw = dr("nw", [128, 4], F32)
    cos_d = dr("cos", [128, 2, L], F32)
    sin_d = dr("sin", [128, 2, L], F32)
    perm_d = dr("perm_in", [128, 128], F32)
    ident_d = dr("ident_in", [128, 128], F32)
    msk_d = dr("msk_in", [64, 2, 64], F32)
    rmask_d = dr("rmask_in", [128, 512], F32)
    oT = dr("oT", [B, DV, TT], BF16, "ExternalOutput")
    F_scr = dr("F_scr", [B, 2, 3, 2, 128, TT], BF16, "Internal")
    ksl_scr = dr("ksl_scr", [B, 2, TT, DK], BF16, "Internal")
    v_scr = dr("v_scr", [B, TT, DV], BF16, "Internal")
    g_scr = dr("g_scr", [B, 4, 128, TT], BF16, "Internal")
    of_scr = dr("of_scr", [B, TT, DV], F32, "Internal")
    tiles = [(t0, min(512, L - t0), 0) for t0 in range(0, L, 512)] + [(L, CTXN, 1)]
    with ExitStack() as es:
        S = Sched(nc, es)
        pb = [T(S, [128, 512], F32, "pb%d" % i, psum=True) for i in range(6)]
        pbt = [T(S, [128, 1024], BF16, "pbt%d" % i, psum=True) for i in range(2)]
        colt = T(S, [128, 6 * KC], F32, "colt")
        ident = T(S, [128, 128], BF16, "ident")
        bat = T(S, [128, 4], F32, "bat")
        nwt = T(S, [128, 4], F32, "nwt")
        ebl = T(S, [128, B * 4, NCK], F32, "ebl")
        msk = T(S, [64, 2, 64], F32, "msk")
        S.dma("sp", colt[:], cols, writes=[colt.r])
        S.dma("pool", ident[:], ident_d, writes=[ident.r])
        S.dma("sp", bat[:], ba, writes=[bat.r])
        S.dma("sp", nwt[:], nw, writes=[nwt.r])
        S.dma("sp", msk[:], msk_d, writes=[msk.r])
        S.op("dve", lambda e: e.tensor_scalar_mul(bat[:], bat[:], -1.0), reads=[bat.r], writes=[bat.r])
        for c in range(3):
            S.op("dve", lambda e, c=c: e.tensor_scalar_add(colt[:, (2 * c + 1) * KC:(2 * c + 2) * KC], colt[:, (2 * c + 1) * KC:(2 * c + 2) * KC], 1.0),
                 reads=[colt.r], writes=[colt.r])
        col = lambda idx, kc: colt[:, idx * KC + kc: idx * KC + kc + 1]
        with ExitStack() as es1:
            S.tes = es1
            Wt = T(S, [128, KC, 1536], BF16, "Wt")
            Wa = T(S, [128, KC, 32], BF16, "Wa")
            Wa2 = T(S, [32, 2, 256], BF16, "Wa2")
            perm = T(S, [128, 128], F32, "perm")
            rmask = T(S, [128, 512], F32, "rmask")
            hT = T(S, [128, KC, 512], BF16, "hT")
            a_bf = T(S, [32, 512], BF16, "a_bf")
            xcs = Rot([T(S, [128, 512], F32, "xc%d" % i) for i in range(3)])
            ft = Rot([T(S, [128, 512], F32, "ft%d" % i) for i in range(10)])
            qk32 = [T(S, [128, 512], F32, "qk32_%d" % i) for i in range(4)]
            cum = [T(S, [128, 512], F32, "cum%d" % i) for i in range(4)]
            cst = T(S, [128, 2, 512], F32, "cst")
            snt = T(S, [128, 2, 512], F32, "snt")
            obs = Rot([T(S, [128, 512], BF16, "ob%d" % i) for i in range(6)])
            kslS = Rot([T(S, [128, 4, 256], BF16, "kslS%d" % i) for i in range(2)])
            for j in range(3):
                S.dma("pool", Wt[:, :, j * 512:(j + 1) * 512], win.rearrange("(kc p) n -> p kc n", p=128)[:, :, j * 512:(j + 1) * 512], writes=[Wt.r])
            S.dma("pool", Wa[:], wa1.rearrange("(kc p) n -> p kc n", p=128), writes=[Wa.r])
            S.dma("pool", Wa2[:], wa2p, writes=[Wa2.r])
            S.dma("sp", perm[:], perm_d, writes=[perm.r])
            S.dma("sp", rmask[:], rmask_d, writes=[rmask.r])
            for b in range(B):
                for (t0, nt, isctx) in tiles:
                    cnd = 2 if isctx else b
                    nck = nt // 64
                    ck0 = t0 // 64
                    for kc in range(KC):
                        xc = xcs.next()
                        S.dma("sp", xc[:, :nt], xT[b, kc * 128:(kc + 1) * 128, t0:t0 + nt], writes=[xc.r])
                        S.op("dve", lambda e: e.tensor_scalar(out=hT[:, kc, :nt], in0=xc[:, :nt], scalar1=col(2 * cnd + 1, kc), scalar2=col(2 * cnd, kc),
                                                              op0=ALU.mult, op1=ALU.add), reads=[xc.r, colt.r], writes=[hT.r])
                    if not isctx:
                        S.dma("sp", cst[:, :, :nt], cos_d[:, :, t0:t0 + nt], writes=[cst.r])
                        S.dma("sp", snt[:, :, :nt], sin_d[:, :, t0:t0 + nt], writes=[snt.r])

                    def proj(j, ps, n=nt):
                        for kc in range(KC):
                            S.op("pe", lambda e: e.matmul(ps[:, :n], Wt[:, kc, j * 128:(j + 1) * 128], hT[:, kc, :n], start=(kc == 0), stop=(kc == KC - 1)),
                                 reads=[Wt.r, hT.r], writes=[ps.r], inc=(kc == KC - 1))
                    for j in range(4):
                        ps = pb[j % 2]
                        proj(j, ps)
                        dst = qk32[j]
                        if isctx:
                            S.op("act", lambda e: e.activation(out=dst[:, :nt], in_=ps[:, :nt], func=AF.Copy, scale=(1.0 / 16.0 if j < 2 else 1.0)),
                                 reads=[ps.r], writes=[dst.r])
                        else:
                            raw = ft.next()
                            S.op("act", lambda e: e.activation(out=raw[:, :nt], in_=ps[:, :nt], func=AF.Copy, scale=(1.0 / 16.0 if j < 2 else 1.0)),
                                 reads=[ps.r], writes=[raw.r])
                            sw = pb[2]
                            S.op("pe", lambda e: e.matmul(sw[:, :nt], perm[:], raw[:, :nt], start=True, stop=True), reads=[perm.r, raw.r], writes=[sw.r])
                            t1 = ft.next()
                            S.op("dve", lambda e: e.tensor_tensor(out=t1[:, :nt], in0=sw[:, :nt], in1=snt[:, j % 2, :nt], op=ALU.mult), reads=[sw.r, snt.r], writes=[t1.r])
                            S.op("pool", lambda e: e.tensor_tensor(out=raw[:, :nt], in0=raw[:, :nt], in1=cst[:, j % 2, :nt], op=ALU.mult), reads=[raw.r, cst.r], writes=[raw.r])
                            S.op("pool", lambda e: e.tensor_tensor(out=dst[:, :nt], in0=raw[:, :nt], in1=t1[:, :nt], op=ALU.add), reads=[raw.r, t1.r], writes=[dst.r])
                    for j in range(4):
                        ps = pb[j % 2]
                        proj(8 + j, ps)
                        ob = obs.next()
                        S.op("act", lambda e: e.activation(out=ob[:, :nt], in_=ps[:, :nt], func=AF.Silu), reads=[ps.r], writes=[ob.r])
                        S.dma("sp", g_scr[b, j, :, t0:t0 + nt], ob[:, :nt], reads=[ob.r], prim=ob.r)
                    for tc in range(nt // 128):
                        ps = pb[tc % 2]
                        for kc in range(KC):
                            S.op("pe", lambda e: e.matmul(ps[:, :DV], hT[:, kc, tc * 128:(tc + 1) * 128], Wt[:, kc, 512:1024], start=(kc == 0), stop=(kc == KC - 1)),
                                 reads=[Wt.r, hT.r], writes=[ps.r], inc=(kc == KC - 1))
                        ob = obs.next()
                        S.op("act", lambda e: e.activation(out=ob[:, :DV], in_=ps[:, :DV], func=AF.Copy), reads=[ps.r], writes=[ob.r])
                        S.dma("sp", v_scr[b, t0 + tc * 128:t0 + (tc + 1) * 128, :], ob[:, :DV], reads=[ob.r], prim=ob.r)
                    pa = pb[3]
                    for kc in range(KC):
                        S.op("pe", lambda e: e.matmul(pa[:32, :nt], Wa[:, kc, :], hT[:, kc, :nt], start=(kc == 0), stop=(kc == KC - 1)),
                             reads=[Wa.r, hT.r], writes=[pa.r], inc=(kc == KC - 1))
                    S.op("act", lambda e: e.activation(out=a_bf[:, :nt], in_=pa[:32, :nt], func=AF.Copy), reads=[pa.r], writes=[a_bf.r])
                    for d in range(2):
                        for ch in range(2):
                            pz = pb[4 + ch]
                            S.op("pe", lambda e: e.matmul(pz[:, :nt], Wa2[:, d, ch * 128:(ch + 1) * 128], a_bf[:, :nt], start=True, stop=True),
                                 reads=[Wa2.r, a_bf.r], writes=[pz.r])
                            l_ = ft.next()
                            S.op("act", lambda e: e.activation(out=l_[:, :nt], in_=pz[:, :nt], func=AF.Exp, scale=-1.0, bias=bat[:, d * 2 + ch:d * 2 + ch + 1]),
                                 reads=[pz.r, bat.r], writes=[l_.r])
                            S.op("act", lambda e: e.activation(out=l_[:, :nt], in_=l_[:, :nt], func=AF.Ln, bias=1.0), reads=[l_.r], writes=[l_.r])
                            cm = cum[d * 2 + ch]
                            S.op("dve", lambda e: e.tensor_tensor_scan(out=cm[:, :nt], data0=rmask[:, :nt], data1=l_[:, :nt], initial=0.0, op0=ALU.mult, op1=ALU.add),
                                 reads=[rmask.r, l_.r], writes=[cm.r])
                            if d == 1:
                                c3 = cm[:, :nt].rearrange("p (c j) -> p c j", j=64)
                                l3 = l_[:, :nt].rearrange("p (c j) -> p c j", j=64)
                                S.op("pool", lambda e: e.tensor_tensor(out=l3, in0=l3, in1=c3[:, :, 63:64].broadcast_to([128, nck, 64]), op=ALU.add),
                                     reads=[l_.r, cm.r], writes=[l_.r])
                                S.op("pool", lambda e: e.tensor_tensor(out=cm[:, :nt], in0=l_[:, :nt], in1=cm[:, :nt], op=ALU.subtract), reads=[l_.r, cm.r], writes=[cm.r])
                    for d in range(2):
                        im, il = (32, 63) if d == 0 else (31, 0)
                        for ch in range(2):
                            cm = cum[d * 2 + ch]
                            c3 = cm[:, :nt].rearrange("p (c j) -> p c j", j=64)
                            qr, kr = qk32[ch], qk32[2 + ch]
                            dm = ft.next()
                            dm3 = dm[:, :nt].rearrange("p (c j) -> p c j", j=64)
                            S.op("dve", lambda e: e.tensor_tensor(out=dm3, in0=c3, in1=c3[:, :, im:im + 1].broadcast_to([128, nck, 64]), op=ALU.subtract),
                                 reads=[cm.r], writes=[dm.r])
                            specs = []
                            e1 = ft.next()
                            S.op("act", lambda e: e.activation(out=e1[:, :nt], in_=dm[:, :nt], func=AF.Exp, scale=-1.0 / 16.0), reads=[dm.r], writes=[e1.r])
                            specs.append((0, qr, e1))
                            e2 = ft.next()
                            S.op("act", lambda e: e.activation(out=e2[:, :nt], in_=dm[:, :nt], func=AF.Exp, scale=1.0 / 16.0), reads=[dm.r], writes=[e2.r])
                            specs.append((1, kr, e2))
                            e3 = ft.next()
                            S.op("act", lambda e: e.activation(out=e3[:, :nt], in_=cm[:, :nt], func=AF.Exp, scale=-1.0 / 16.0), reads=[cm.r], writes=[e3.r])
                            specs.append((2, qr, e3))
                            e33 = e3[:, :nt].rearrange("p (c j) -> p c j", j=64)
                            S.op("pool", lambda e: e.tensor_copy(out=ebl[:, b * 4 + d * 2 + ch, ck0:ck0 + nck], in_=e33[:, :, il]), reads=[e3.r], writes=[ebl.r])
                            for (which, src, ex) in specs:
                                ob = obs.next()
                                S.op("dve" if which != 1 else "pool", lambda e: e.tensor_tensor(out=ob[:, :nt], in0=src[:, :nt], in1=ex[:, :nt], op=ALU.mult),
                                     reads=[src.r, ex.r], writes=[ob.r])
                                S.dma("sp", F_scr[b, d, which, ch, :, t0:t0 + nt], ob[:, :nt], reads=[ob.r], prim=ob.r)
                            dl = ft.next()
                            dl3 = dl[:, :nt].rearrange("p (c j) -> p c j", j=64)
                            S.op("dve", lambda e: e.tensor_tensor(out=dl3, in0=c3, in1=c3[:, :, il:il + 1].broadcast_to([128, nck, 64]), op=ALU.subtract),
                                 reads=[cm.r], writes=[dl.r])
                            S.op("act", lambda e: e.activation(out=dl[:, :nt], in_=dl[:, :nt], func=AF.Exp, scale=1.0 / 16.0), reads=[dl.r], writes=[dl.r])
                            kb = obs.next()
                            S.op("dve", lambda e: e.tensor_tensor(out=kb[:, :nt], in0=kr[:, :nt], in1=dl[:, :nt], op=ALU.mult), reads=[kr.r, dl.r], writes=[kb.r])
                            if ch == 0:
                                ks_t = kslS.next()
                            pt = pbt[ch]
                            for tc in range(nt // 128):
                                S.op("pe", lambda e: e.transpose(out=pt[:, tc * 128:(tc + 1) * 128], in_=kb[:, tc * 128:(tc + 1) * 128], identity=ident[:]),
                                     reads=[kb.r, ident.r], writes=[pt.r], inc=(tc == nt // 128 - 1))
                            S.op("act", lambda e: e.activation(out=ks_t[:, :nt // 128, ch * 128:(ch + 1) * 128], in_=pt[:, :nt].rearrange("p (t n) -> p t n", n=128), func=AF.Copy),
                                 reads=[pt.r], writes=[ks_t.r])
                            if ch == 1:
                                S.dma("sp", ksl_scr[b, d, t0:t0 + nt, :].rearrange("(t p) n -> p t n", p=128), ks_t[:, :nt // 128, :], reads=[ks_t.r], prim=ks_t.r)
            S.barrier()
        with ExitStack() as es2:
            S.tes = es2
            if dbg == 1:
                B = 0
            St = T(S, [128, 2, DV], F32, "St")
            Sb = T(S, [128, 2, DV], BF16, "Sb")
            Fb = Rot([T(S, [128, 6, 512], BF16, "Fb%d" % i) for i in range(2)])
            kslb = Rot([T(S, [64, 8, DK], BF16, "kslb%d" % i) for i in range(2)])
            vb = Rot([T(S, [64, 8, DV], BF16, "vb%d" % i) for i in range(2)])
            ofb = Rot([T(S, [64, 8, DV], F32, "ofb%d" % i) for i in range(2)])
            gb = Rot([T(S, [128, 4, 512], BF16, "gb%d" % i) for i in range(2)])
            oTb = Rot([T(S, [128, 4, 512], BF16, "oTb%d" % i) for i in range(2)])
            atb = Rot([T(S, [64, 64], BF16, "atb%d" % i) for i in range(2)])
            osb = Rot([T(S, [64, DV], F32, "osb%d" % i) for i in range(2)])
            onb = Rot([T(S, [64, DV], BF16, "onb%d" % i) for i in range(2)])
            sq = T(S, [64, DV], F32, "sqj")
            msb = Rot([T(S, [64, 4], F32, "msb%d" % i) for i in range(2)])
            tmb = Rot([T(S, [128, 4, 64], F32, "tmb%d" % i) for i in range(2)])
            pat = Rot([pb[0], pb[1]])
            pos_ = Rot([pb[2], pb[3]])
            for b in range(B):
                for d in range(1 if dbg == 2 else 2):
                    S.op("dve", lambda e: e.memset(St[:], 0.0), writes=[St.r])
                    S.op("dve", lambda e: e.memset(Sb[:], 0.0), writes=[Sb.r])
                    lat = [t for t in tiles if not t[2]]
                    order = [tiles[-1]] + (lat if d == 0 else lat[::-1])
                    for (t0, nt, isctx) in order:
                        nck = nt // 64
                        F, ksl, v = Fb.next(), kslb.next(), vb.next()
                        for which in range(3):
                            for ch in range(2):
                                S.dma("sp", F[:, which * 2 + ch, :nt], F_scr[b, d, which, ch, :, t0:t0 + nt], writes=[F.r])
                        S.dma("sp", ksl[:, :nck, :], ksl_scr[b, d, t0:t0 + nt, :].rearrange("(c p) n -> p c n", p=64), writes=[ksl.r])
                        S.dma("sp", v[:, :nck, :], v_scr[b, t0:t0 + nt, :].rearrange("(c p) n -> p c n", p=64), writes=[v.r])
                        of = ofb.next()
                        if d == 1:
                            g, oTt = gb.next(), oTb.next()
                            S.dma("sp", of[:, :nck, :], of_scr[b, t0:t0 + nt, :].rearrange("(c p) n -> p c n", p=64), writes=[of.r])
                            for j in range(4):
                                S.dma("sp", g[:, j, :nt], g_scr[b, j, :, t0:t0 + nt], writes=[g.r])
                        for c in (range(nck) if d == 0 else range(nck - 1, -1, -1)):
                            cs = c * 64
                            gck = t0 // 64 + c
                            pa_ = pat.next()
                            for ch in range(2):
                                S.op("pe", lambda e: e.matmul(pa_[:64, :64], F[:, 2 + ch, cs:cs + 64], F[:, 0 + ch, cs:cs + 64], start=(ch == 0), stop=(ch == 1)),
                                     reads=[F.r], writes=[pa_.r], inc=(ch == 1))
                            at = atb.next()
                            S.op("dve", lambda e: e.tensor_tensor(out=at[:], in0=pa_[:64, :64], in1=msk[:, d, :], op=ALU.mult), reads=[pa_.r, msk.r], writes=[at.r])
                            po = pos_.next()
                            S.op("pe", lambda e: e.matmul(po[:64, :DV], at[:], v[:, c, :], start=True, stop=False), reads=[at.r, v.r], writes=[po.r], inc=False)
                            for ch in range(2):
                                S.op("pe", lambda e: e.matmul(po[:64, :DV], F[:, 4 + ch, cs:cs + 64], Sb[:, ch, :], start=False, stop=(ch == 1)),
                                     reads=[F.r, Sb.r], writes=[po.r], inc=(ch == 1))
                            for ch in range(2):
                                pd_ = pb[4 + ch]
                                S.op("pe", lambda e: e.matmul(pd_[:, :DV], ksl[:, c, ch * 128:(ch + 1) * 128], v[:, c, :], start=True, stop=True),
                                     reads=[ksl.r, v.r], writes=[pd_.r])
                                S.op("dve", lambda e: e.scalar_tensor_tensor(out=St[:, ch, :], in0=St[:, ch, :], scalar=ebl[:, b * 4 + d * 2 + ch, gck:gck + 1],
                                                                             in1=pd_[:, :DV], op0=ALU.mult, op1=ALU.add), reads=[St.r, ebl.r, pd_.r], writes=[St.r])
                                S.op("act", lambda e: e.activation(out=Sb[:, ch, :], in_=St[:, ch, :], func=AF.Copy), reads=[St.r], writes=[Sb.r])
                            if d == 0:
                                S.op("act", lambda e: e.activation(out=of[:, c, :], in_=po[:64, :DV], func=AF.Copy), reads=[po.r], writes=[of.r])
                            else:
                                os_, on_, ms = osb.next(), onb.next(), msb.next()
                                S.op("dve", lambda e: e.tensor_tensor(out=os_[:], in0=po[:64, :DV], in1=of[:, c, :], op=ALU.add), reads=[po.r, of.r], writes=[os_.r])
                                if dbg == 4:
                                    continue
                                S.op("pool", lambda e: e.tensor_tensor(out=sq[:], in0=os_[:], in1=os_[:], op=ALU.mult), reads=[os_.r], writes=[sq.r])
                                S.op("dve", lambda e: e.tensor_reduce(out=ms[:, 0:1], in_=sq[:], axis=AX.X, op=ALU.add), reads=[sq.r], writes=[ms.r])
                                S.op("dve", lambda e: e.tensor_scalar(out=ms[:, 1:2], in0=ms[:, 0:1], scalar1=1.0 / DV, scalar2=float(CFG.EPS), op0=ALU.mult, op1=ALU.add),
                                     reads=[ms.r], writes=[ms.r])
                                S.op("act", lambda e: e.activation(out=ms[:, 2:3], in_=ms[:, 1:2], func=AF.Sqrt), reads=[ms.r], writes=[ms.r])
                                S.op("dve", lambda e: e.reciprocal(out=ms[:, 3:4], in_=ms[:, 2:3]), reads=[ms.r], writes=[ms.r])
                                S.op("dve", lambda e: e.tensor_scalar_mul(on_[:], os_[:], ms[:, 3:4]), reads=[os_.r, ms.r], writes=[on_.r])
                                pt = pbt[(gck) % 2]
                                if dbg == 3:
                                    continue
                                for j in range(4):
                                    S.op("pe", lambda e: e.transpose(out=pt[:, j * 64:(j + 1) * 64], in_=on_[:, j * 128:(j + 1) * 128], identity=ident[:64, :64]),
                                         reads=[on_.r, ident.r], writes=[pt.r], inc=(j == 3))
                                tm = tmb.next()
                                for j in range(4):
                                    S.op("act", lambda e: e.activation(out=tm[:, j, :], in_=pt[:, j * 64:(j + 1) * 64], func=AF.Copy, scale=nwt[:, j:j + 1]),
                                         reads=[pt.r, nwt.r], writes=[tm.r])
                                S.op("dve", lambda e: e.tensor_tensor(out=oTt[:, :, cs:cs + 64], in0=tm[:], in1=g[:, :, cs:cs + 64], op=ALU.mult),
                                     reads=[tm.r, g.r], writes=[oTt.r])
                        if d == 0:
                            S.dma("sp", of_scr[b, t0:t0 + nt, :].rearrange("(c p) n -> p c n", p=64), of[:, :nck, :], reads=[of.r], prim=of.r)
                        else:
                            S.dma("sp", oT[b].rearrange("(j p) t -> p j t", p=128)[:, :, t0:t0 + nt], oTt[:, :, :nt], reads=[oTt.r], prim=oTt.r)
                    S.barrier()
        S.tes = es
        S.finish()
    return nc


def build_ada(D=CFG.D, R=CFG.ADA_R, NL=CFG.DEPTH):
    KC = D // 128
    NJ = 6 * D // 128
    RC = R // 128
    HJ = NJ // 2
    nc = bass.Bass("TRN2", target_bir_lowering=False)
    dr = lambda n, s, d, k="ExternalInput": nc.dram_tensor(n, s, d, kind=k).ap()
    condT = dr("condT", [128, KC, 3], F32)
    w1 = dr("w1", [NL, D, R], F32)
    w2 = dr("w2", [NL, R, 6 * D], F32)
    bT = dr("bT", [NL, 128, NJ], F32)
    mods = dr("mods", [NL, 128, NJ, 3], F32, "ExternalOutput")
    with ExitStack() as es:
        S = Sched(nc, es)
        ct = T(S, [128, KC, 3], F32, "ct")
        sT = T(S, [128, KC, 3], BF16, "sT")
        W1 = T(S, [128, KC, R], BF16, "W1")
        W2 = T(S, [128, RC, 6 * D], BF16, "W2")
        bt = T(S, [128, NJ], F32, "bt")
        ub = T(S, [128, RC, 3], BF16, "ub")
        res = T(S, [128, NJ, 3], F32, "res")
        pu = T(S, [128, 512], F32, "pu", psum=True)
        pm = [T(S, [128, 512], F32, "pm%d" % i, psum=True) for i in range(2)]
        S.dma("sp", ct[:], condT, writes=[ct.r])
        S.op("act", lambda e: e.activation(out=sT[:], in_=ct[:], func=AF.Silu), reads=[ct.r], writes=[sT.r])
        for l in range(NL):
            S.dma("pool", W1[:], w1[l].rearrange("(kc p) n -> p kc n", p=128), writes=[W1.r])
            for q in range(6):
                S.dma("pool", W2[:, :, q * D:(q + 1) * D], w2[l].rearrange("(kc p) n -> p kc n", p=128)[:, :, q * D:(q + 1) * D], writes=[W2.r])
            S.dma("sp", bt[:], bT[l], writes=[bt.r])
            for m in range(RC):
                for kc in range(KC):
                    S.op("pe", lambda e: e.matmul(pu[:, m * 4:m * 4 + 3], W1[:, kc, m * 128:(m + 1) * 128], sT[:, kc, :], start=(kc == 0), stop=(kc == KC - 1)),
                         reads=[W1.r, sT.r], writes=[pu.r], inc=(kc == KC - 1))
            for m in range(RC):
                S.op("act", lambda e: e.activation(out=ub[:, m, :], in_=pu[:, m * 4:m * 4 + 3], func=AF.Copy), reads=[pu.r], writes=[ub.r])
            for j in range(NJ):
                p_ = pm[j // HJ]
                o_ = (j % HJ) * 3
                for m in range(RC):
                    S.op("pe", lambda e: e.matmul(p_[:, o_:o_ + 3], W2[:, m, j * 128:(j + 1) * 128], ub[:, m, :], start=(m == 0), stop=(m == RC - 1)),
                         reads=[W2.r, ub.r], writes=[p_.r], inc=(m == RC - 1))
            for half in range(2):
                S.op("dve", lambda e: e.tensor_tensor(out=res[:, half * HJ:(half + 1) * HJ, :], in0=pm[half][:, :HJ * 3].rearrange("p (j c) -> p j c", c=3),
                                                      in1=bt[:, half * HJ:(half + 1) * HJ].unsqueeze(2).broadcast_to([128, HJ, 3]), op=ALU.add),
                     reads=[pm[half].r, bt.r], writes=[res.r])
            S.dma("sp", mods[l], res[:], reads=[res.r], prim=res.r)
        S.finish()
    return nc


_PROGS = {}


def _prog(name, fn):
    if name not in _PROGS:
        _PROGS[name] = fn()
    return _PROGS[name]


def _run(nc, in_maps):
    res = run_bass_kernel_spmd(nc, in_maps, core_ids=list(range(8)))
    return res.results


def _colset(v, KC):
    return np.ascontiguousarray(v.reshape(KC, 128).T)


def kernel(x, c, ctx, c_ctx, ada_w1, ada_w2, ada_b, ln_g, ln_b, na_w_qkv, na_w_o, na_rpb,
           gla_w_in, gla_w_a1, gla_w_a2, gla_b_a, gla_norm, gla_w_o, moe_router, moe_bias,
           moe_w_gate, moe_w_up, moe_w_down, sh_w_gate, sh_w_up, sh_w_down):
    f32 = lambda a: np.ascontiguousarray(np.asarray(a, dtype=np.float32))
    x, c, ctx, c_ctx = f32(x), f32(c), f32(ctx), f32(c_ctx)
    D, KC, B, L, CT = CFG.D, CFG.D // 128, CFG.B, CFG.SEQ, CFG.CTX
    TT = L + CT
    NL = CFG.DEPTH
    ROWS = L // CFG.GRID_W
    eye = np.eye(128, dtype=np.float32)
    conds = np.stack([c[0], c[1], c_ctx], axis=0)
    condT = np.ascontiguousarray(conds.reshape(3, KC, 128).transpose(2, 1, 0))
    bT = np.ascontiguousarray(f32(ada_b).reshape(NL, 6 * KC, 128).transpose(0, 2, 1))
    ada_in = dict(condT=condT, w1=f32(ada_w1), w2=f32(ada_w2), bT=bT)
    mods = _run(_prog("ada", build_ada), [ada_in] * 8)[0]["mods"]
    xT = np.empty((B, D, TT), np.float32)
    for b in range(B):
        xT[b, :, :L] = x[b].T
        xT[b, :, L:] = ctx[b].T
    ffn_tiles = [(0, 512, 0), (512, 512, 0), (1024, 512, 0), (1536, 512, 0), (2048, 64, 1)]
    LS, CS = L // 4, CT // 4
    cos, sin, perm, msk, rmask = gla_consts(ROWS)
    for l in range(NL):
        j = l // 2
        mcols = np.zeros((128, 6 * KC), np.float32)
        for cnd in range(3):
            mcols[:, (2 * cnd) * KC:(2 * cnd + 1) * KC] = mods[l][:, 0 * KC:1 * KC, cnd]
            mcols[:, (2 * cnd + 1) * KC:(2 * cnd + 2) * KC] = mods[l][:, 1 * KC:2 * KC, cnd]
        if l % 2 == 0:
            wq = f32(na_w_qkv[j])
            btab = na_bias_tables(f32(na_rpb[j]), ROWS)
            in_maps = []
            for k in range(8):
                wsel = np.ascontiguousarray(np.concatenate([wq[:, t * D + k * 512:t * D + (k + 1) * 512] for t in range(3)], axis=1))
                in_maps.append(dict(xT=xT, cols=mcols, wqkv=wsel, bias=np.ascontiguousarray(btab[4 * k:4 * k + 4].reshape(4, 128, -1))))
            outs = _run(_prog("na", build_na), in_maps)
            w_o = f32(na_w_o[j])
        else:
            wi = f32(gla_w_in[j])
            wa1 = np.ascontiguousarray(np.concatenate([f32(gla_w_a1[j][0]), f32(gla_w_a1[j][1])], axis=1))
            w_a2, b_a = f32(gla_w_a2[j]), f32(gla_b_a[j])
            nwt = np.ascontiguousarray(f32(gla_norm[j]).reshape(4, 128).T)
            in_maps = []
            for k in range(8):
                win = np.ascontiguousarray(np.concatenate([wi[:, k * 256:(k + 1) * 256], wi[:, 2048 + k * 256:2048 + (k + 1) * 256],
                                                           wi[:, 4096 + k * 512:4096 + (k + 1) * 512], wi[:, 8192 + k * 512:8192 + (k + 1) * 512]], axis=1))
                wa2p = np.zeros((32, 2, 256), np.float32)
                wa2p[0:16, 0] = w_a2[0][:, k * 256:(k + 1) * 256]
                wa2p[16:32, 1] = w_a2[1][:, k * 256:(k + 1) * 256]
                ba = np.ascontiguousarray(np.stack([b_a[d][k * 256 + ch * 128:k * 256 + (ch + 1) * 128] for d in range(2) for ch in range(2)], axis=1))
                in_maps.append(dict(xT=xT, cols=mcols, win=win, wa1=wa1, wa2p=wa2p, ba=ba, nw=nwt, cos=cos, sin=sin, perm_in=perm,
                                    ident_in=eye, msk_in=msk, rmask_in=rmask))
            outs = _run(_prog("gla", build_gla), in_maps)
            w_o = f32(gla_w_o[j])
        oT = np.concatenate([o["oT"] for o in outs], axis=1)
        del outs
        wd = f32(moe_w_down[l]).reshape(CFG.NE * CFG.HD, D)
        in_maps = []
        for k in range(8):
            b, q = k // 4, k % 4
            sl = lambda a: np.ascontiguousarray(np.concatenate([a[b][:, q * LS:(q + 1) * LS], a[b][:, L + q * CS:L + (q + 1) * CS]], axis=1))
            fc = np.zeros((128, 12 * KC), np.float32)
            for ci, cnd in enumerate((b, 2)):
                for i, ty in enumerate((2, 4, 3, 5)):
                    fc[:, (4 * ci + i) * KC:(4 * ci + i + 1) * KC] = mods[l][:, ty * KC:(ty + 1) * KC, cnd]
            fc[:, 8 * KC:9 * KC] = _colset(f32(ln_g[l, 0]), KC)
            fc[:, 9 * KC:10 * KC] = _colset(f32(ln_b[l, 0]), KC)
            fc[:, 10 * KC:11 * KC] = _colset(f32(ln_g[l, 1]), KC)
            fc[:, 11 * KC:12 * KC] = _colset(f32(ln_b[l, 1]), KC)
            in_maps.append(dict(xT=sl(xT), oT=sl(oT), wo=w_o, cols=fc, wr=f32(moe_router[l]), eb=f32(moe_bias[l]).reshape(1, CFG.NE),
                                wg=f32(moe_w_gate[l]), wu=f32(moe_w_up[l]), wd=wd, sg=f32(sh_w_gate[l]), su=f32(sh_w_up[l]), sd=f32(sh_w_down[l]),
                                ident_in=eye))
        outs = _run(_prog("ffn", lambda: build_ffn(ffn_tiles)), in_maps)
        del in_maps
        for k in range(8):
            b, q = k // 4, k % 4
            x2 = outs[k]["x2T"]
            xT[b, :, q * LS:(q + 1) * LS] = x2[:, :LS]
            xT[b, :, L + q * CS:L + (q + 1) * CS] = x2[:, LS:]
        del outs
    return np.ascontiguousarray(xT[:, :, :L].transpose(0, 2, 1))
```

```python
import numpy as np
import ml_dtypes
from contextlib import ExitStack
import concourse.bass as bass
import concourse.mybir as mybir
from concourse.bass_utils import run_bass_kernel_spmd

F32 = mybir.dt.float32
BF16 = mybir.dt.bfloat16
AF = mybir.ActivationFunctionType
ALU = mybir.AluOpType
AX = mybir.AxisListType
NPBF = ml_dtypes.bfloat16


class CFG:
    D = 4096
    B = 2
    SEQ = 8192
    DEPTH = 4
    GRID_W = 64
    CTX = 256
    NA_H = 32
    NA_KH = 8
    NA_KW = 16
    GLA_H = 8
    GLA_RANK = 16
    GLA_L = 64
    NE = 64
    HD = 192
    TOPK = 8
    NG = 8
    TOPG = 4
    RSCALE = 2.5
    ADA_R = 256
    EPS = 1e-6


ALPHA = (2 * CFG.DEPTH) ** 0.25


class Res:
    __slots__ = ("w", "rs", "name", "sem", "dcnt", "key")
    _n = 0

    def __init__(self, name=""):
        self.w = None
        self.rs = {}
        self.name = name
        self.sem = None
        self.dcnt = 0
        Res._n += 1
        self.key = ("r", Res._n)


class Sched:
    def __init__(self, nc, es):
        self.nc = nc
        self.es = es
        self.eng = {"pe": nc.tensor, "act": nc.scalar, "dve": nc.vector, "pool": nc.gpsimd, "sp": nc.sync}
        self.sem = {}
        self.cnt = {}
        for k in ("pe", "act", "dve", "pool"):
            self.sem[k] = es.enter_context(nc.semaphore("s_" + k))
            self.cnt[k] = 0
        self.waited = {k: {} for k in self.eng}
        self.dres = []
        self.nsem = 4
        self.tes = es
        self.prefix = ""
        self.sempool = []

    def _need(self, E, dep):
        key, val = dep
        if isinstance(key, str):
            if key == E and val > self.cnt[E]:
                return
            semh = self.sem[key]
        else:
            r = key
            val = r.dcnt
            semh = r.sem
            key = r.key
        if self.waited[E].get(key, 0) >= val:
            return
        self.eng[E].wait_ge(semh, val)
        self.waited[E][key] = val

    def _deps(self, E, reads, writes):
        for r in reads:
            if r.w is not None:
                self._need(E, r.w)
        for w in writes:
            if w.w is not None:
                self._need(E, w.w)
            for k, v in w.rs.items():
                self._need(E, (k, v))

    def _tag(self, tag, reads, writes):
        k, v = tag
        for r in reads:
            if r.rs.get(k, 0) < v:
                r.rs[k] = v
        for w in writes:
            w.w = tag
            w.rs = {}

    def op(self, E, fn, reads=(), writes=(), inc=True):
        self._deps(E, reads, writes)
        ins = fn(self.eng[E])
        if inc:
            self.cnt[E] += 1
            ins.then_inc(self.sem[E], 1)
            tag = (E, self.cnt[E])
        else:
            tag = (E, self.cnt[E] + 1)
        self._tag(tag, reads, writes)
        return ins

    def dma(self, Q, out, in_, reads=(), writes=(), prim=None):
        self._deps(Q, reads, writes)
        p = prim if prim is not None else (writes[0] if writes else reads[0])
        self._getsem(p)
        ins = self.eng[Q].dma_start(out=out, in_=in_)
        p.dcnt += 16
        ins.then_inc(p.sem, 16)
        self._tag((p, p.dcnt), reads, writes)
        return ins

    def _getsem(self, p):
        if p.sem is None:
            if self.sempool:
                p.sem, p.dcnt = self.sempool.pop()
            else:
                p.sem = self.es.enter_context(self.nc.semaphore("d%d" % self.nsem))
                self.nsem += 1
                assert self.nsem < 95, "too many semaphores"
            self.dres.append(p)

    def phase_end(self):
        self.barrier()
        for r in self.dres:
            self.sempool.append((r.sem, r.dcnt))
            r.sem = None
        self.dres = []

    def cc(self, kind, op, in_ap, out_ap):
        self.barrier()
        r = Res("cc")
        self._getsem(r)
        ins = self.nc.gpsimd.collective_compute(kind, op, replica_groups=[list(range(8))], ins=[in_ap.opt()], outs=[out_ap.opt()])
        ins.then_inc(r.sem)
        r.dcnt += 1
        self.barrier()

    def barrier(self):
        for E in ("pe", "act", "dve", "pool", "sp"):
            for k in ("pe", "act", "dve", "pool"):
                if k != E and self.cnt[k] > 0:
                    self._need(E, (k, self.cnt[k]))
            for r in self.dres:
                self._need(E, (r, r.dcnt))

    def finish(self):
        for k in ("pe", "act", "dve", "pool"):
            if self.cnt[k] > 0:
                self._need("sp", (k, self.cnt[k]))
        for r in self.dres:
            self._need("sp", (r, r.dcnt))


class FZ:
    def __init__(self, nc, S, pre, io=None, **hooks):
        self.nc, self.S, self.pre, self.io = nc, S, pre, (io or {})
        self.__dict__.update(hooks)


def _mk_dr(nc, fz):
    def dr(n, s, d, k="ExternalInput"):
        if fz is not None and n in fz.io:
            return fz.io[n]
        name = (fz.pre if fz is not None else "") + n
        return nc.dram_tensor(name, s, d, kind=k).ap()
    return dr


class T:
    def __init__(self, S, shape, dt, name, psum=False):
        nc = S.nc
        name = S.prefix + name
        self.t = S.tes.enter_context(nc.psum_tensor(name, shape, dt) if psum else nc.sbuf_tensor(name, shape, dt))
        self.r = Res(name)

    def __getitem__(self, k):
        return self.t[k]


class Rot:
    def __init__(self, tiles):
        self.tiles = tiles
        self.i = 0

    def next(self):
        t = self.tiles[self.i % len(self.tiles)]
        self.i += 1
        return t


def build_ffn(tiles, D=CFG.D, NE=CFG.NE, HD=CFG.HD, fz=None, prep=False):
    KC = D // 128
    Ttot = sum(t[1] for t in tiles)
    G = NE // CFG.NG
    NPAIR = NE // 2
    NCH = 3 * NPAIR + 2
    NTM = 512
    nc = fz.nc if fz else bass.Bass("TRN2", target_bir_lowering=False)
    dr = _mk_dr(nc, fz)
    xT = dr("xT", [D, Ttot], F32)
    oT = dr("oT", [D, Ttot], BF16)
    cols = dr("cols", [128, 12 * KC], F32)
    wr = dr("wr", [D, NE], F32)
    eb = dr("eb", [1, NE], F32)
    KHp = max(1, KC // 2)
    NKH = (KC + KHp - 1) // KHp
    if prep:
        wgu = dr("wgu", [NPAIR, 2, NKH, 128, KHp * 2 * HD], BF16)
        wsh = dr("wsh", [2, NKH, 128, KHp * HD], BF16)
        wdp = dr("wdp", [KC, 2, 128, ((NCH + 1) // 2) * 128], BF16)
        wop = dr("wop", [KC, 128, KC * 128], BF16)
        wo = wg = wu = wd = sg = su = sd = None
    else:
        wo = dr("wo", [D, D], F32)
        wg = dr("wg", [NE, D, HD], F32)
        wu = dr("wu", [NE, D, HD], F32)
        wd = dr("wd", [NE * HD, D], F32)
        sg = dr("sg", [D, HD], F32)
        su = dr("su", [D, HD], F32)
        sd = dr("sd", [HD, D], F32)
    ident_in = dr("ident_in", [128, 128], F32)
    x2T = dr("x2T", [D, Ttot], F32, "ExternalOutput")
    r_scr = dr("r_scr", [D, NTM], F32, "Internal")
    x1_scr = dr("x1_scr", [D, NTM], F32, "Internal")

    with ExitStack() as es:
        S = fz.S if fz else Sched(nc, es)
        S.tes = es
        S.prefix = fz.pre if fz else ""
        x2outs = fz.x2outs if fz else [x2T]
        hT = T(S, [128, KC, NTM], BF16, "hT")
        hid = T(S, [128, NCH, NTM], BF16, "hid")
        hid_rs = [Res("hid%d" % i) for i in range(NCH)]
        USZ = max(16 * 384, (NCH + 1) // 2 * 128, KC * 128)
        arena = Rot([T(S, [128, USZ], BF16, "ar%d" % i) for i in range(3)])
        f32t = Rot([T(S, [128, NTM], F32, "f%d" % i) for i in range(8)])
        mean = T(S, [128, NTM], F32, "mean")
        rstd = T(S, [128, NTM], F32, "rstd")
        colt = T(S, [128, 12 * KC], F32, "colt")
        ident = T(S, [128, 128], BF16, "ident")
        ones32 = T(S, [128, 128], F32, "ones32")
        wrt = T(S, [128, KC, NE], BF16, "wrt")
        ebt = T(S, [128, NE], F32, "ebt")
        gT = T(S, [64, NTM], BF16, "gT")
        sm = Rot([T(S, [128, NE], F32, "sm%d" % i) for i in range(6)])
        sm8 = Rot([T(S, [128, 8], F32, "sm8_%d" % i) for i in range(6)])
        gbf = T(S, [128, NE], BF16, "gbf")
        pb = [T(S, [128, 512], F32, "pb%d" % i, psum=True) for i in range(7)]
        pbt = T(S, [128, 1024], BF16, "pbt", psum=True)

        if fz:
            for (blk, src) in fz.colsrc:
                S.dma("sp", colt[:, blk * KC:(blk + 1) * KC], src, writes=[colt.r])
        else:
            S.dma("sp", colt[:], cols, writes=[colt.r])
        S.dma("pool", ident[:], ident_in, writes=[ident.r])
        S.op("dve", lambda e: e.memset(ones32[:], 1.0), writes=[ones32.r])
        S.dma("pool", wrt[:], wr.rearrange("(kc p) n -> p kc n", p=128), writes=[wrt.r])
        S.dma("sp", ebt[:], eb.partition_broadcast(128), writes=[ebt.r])
        for c in (1, 5):
            S.op("dve", lambda e, c=c: e.tensor_scalar_add(colt[:, c * KC:(c + 1) * KC], colt[:, c * KC:(c + 1) * KC], 1.0),
                 reads=[colt.r], writes=[colt.r])
        col = lambda idx, kc: colt[:, idx * KC + kc: idx * KC + kc + 1]

        def ln_stats(nt, s1, s2):
            m_ = mean[:, :nt]
            r_ = rstd[:, :nt]
            S.op("dve", lambda e: e.tensor_scalar_mul(m_, s1[:, :nt], 1.0 / D), reads=[s1.r], writes=[mean.r])
            tmp = f32t.next()
            S.op("dve", lambda e: e.tensor_tensor(out=tmp[:, :nt], in0=m_, in1=m_, op=ALU.mult), reads=[mean.r], writes=[tmp.r])
            S.op("dve", lambda e: e.scalar_tensor_tensor(out=tmp[:, :nt], in0=s2[:, :nt], scalar=1.0 / D, in1=tmp[:, :nt],
                                                         op0=ALU.mult, op1=ALU.subtract), reads=[s2.r, tmp.r], writes=[tmp.r])
            S.op("dve", lambda e: e.tensor_scalar_add(tmp[:, :nt], tmp[:, :nt], float(CFG.EPS)), reads=[tmp.r], writes=[tmp.r])
            S.op("act", lambda e: e.activation(out=tmp[:, :nt], in_=tmp[:, :nt], func=AF.Sqrt), reads=[tmp.r], writes=[tmp.r])
            S.op("dve", lambda e: e.reciprocal(out=r_, in_=tmp[:, :nt]), reads=[tmp.r], writes=[rstd.r])

        def stats_acc(rc, nt, mc, s1, s2):
            S.op("pe", lambda e: e.matmul(s1[:, :nt], ones32[:], rc[:, :nt], start=(mc == 0), stop=(mc == KC - 1)),
                 reads=[ones32.r, rc.r], writes=[s1.r], inc=(mc == KC - 1))
            sq = f32t.next()
            S.op("act", lambda e: e.activation(out=sq[:, :nt], in_=rc[:, :nt], func=AF.Square), reads=[rc.r], writes=[sq.r])
            S.op("pe", lambda e: e.matmul(s2[:, :nt], ones32[:], sq[:, :nt], start=(mc == 0), stop=(mc == KC - 1)),
                 reads=[ones32.r, sq.r], writes=[s2.r], inc=True)

        for (t0, nt, cond) in tiles:
            cb = 4 * cond
            oTt = hid.t[:, 0:KC, :]
            S.dma("sp", oTt[:, :, :nt], oT.rearrange("(kc p) t -> p kc t", p=128)[:, :, t0:t0 + nt], writes=hid_rs[:KC])
            s1, s2 = pb[2], pb[3]
            for mc in range(KC):
                u = arena.next()
                uv = u.t[:, 0:KC * 128].rearrange("p (kc n) -> p kc n", n=128)
                if prep:
                    S.dma("pool", u.t[:, 0:KC * 128], wop[mc], writes=[u.r])
                else:
                    S.dma("pool", uv, wo.rearrange("(kc p) n -> p kc n", p=128)[:, :, mc * 128:(mc + 1) * 128], writes=[u.r])
                yp = pb[mc % 2]
                for kc in range(KC):
                    S.op("pe", lambda e: e.matmul(yp[:, :nt], uv[:, kc, :], oTt[:, kc, :nt], start=(kc == 0), stop=(kc == KC - 1)),
                         reads=[u.r] + hid_rs[:KC], writes=[yp.r], inc=(kc == KC - 1))
                xc = f32t.next()
                S.dma("sp", xc[:, :nt], xT[mc * 128:(mc + 1) * 128, t0:t0 + nt], writes=[xc.r])
                S.op("act", lambda e: e.activation(out=xc[:, :nt], in_=xc[:, :nt], func=AF.Copy, scale=float(ALPHA)), reads=[xc.r], writes=[xc.r])
                rc = f32t.next()
                S.op("dve", lambda e: e.scalar_tensor_tensor(out=rc[:, :nt], in0=yp[:, :nt], scalar=col(cb + 0, mc), in1=xc[:, :nt],
                                                             op0=ALU.mult, op1=ALU.add), reads=[yp.r, xc.r, colt.r], writes=[rc.r])
                stats_acc(rc, nt, mc, s1, s2)
                S.dma("sp", r_scr[mc * 128:(mc + 1) * 128, 0:nt], rc[:, :nt], reads=[rc.r], prim=rc.r)
            ln_stats(nt, s1, s2)
            S.barrier()
            for mc in range(KC):
                rc = f32t.next()
                S.dma("sp", rc[:, :nt], r_scr[mc * 128:(mc + 1) * 128, 0:nt], writes=[rc.r])
                S.op("dve", lambda e: e.tensor_tensor(out=rc[:, :nt], in0=rc[:, :nt], in1=mean[:, :nt], op=ALU.subtract),
                     reads=[rc.r, mean.r], writes=[rc.r])
                S.op("pool", lambda e: e.tensor_tensor(out=rc[:, :nt], in0=rc[:, :nt], in1=rstd[:, :nt], op=ALU.mult),
                     reads=[rc.r, rstd.r], writes=[rc.r])
                x1c = f32t.next()
                S.op("act", lambda e: e.activation(out=x1c[:, :nt], in_=rc[:, :nt], func=AF.Identity, scale=col(8, mc), bias=col(9, mc)),
                     reads=[rc.r, colt.r], writes=[x1c.r])
                S.dma("sp", x1_scr[mc * 128:(mc + 1) * 128, 0:nt], x1c[:, :nt], reads=[x1c.r], prim=x1c.r)
                S.op("dve", lambda e: e.tensor_scalar(out=hT[:, mc, :nt], in0=x1c[:, :nt], scalar1=col(cb + 1, mc), scalar2=col(cb + 2, mc),
                                                      op0=ALU.mult, op1=ALU.add), reads=[x1c.r, colt.r], writes=[hT.r])
            S.barrier()
            ntc = (nt + 127) // 128
            for tc in range(ntc):
                tn = min(128, nt - tc * 128)
                lp = pb[4 + tc % 2]
                for kc in range(KC):
                    S.op("pe", lambda e: e.matmul(lp[:tn, :NE], hT[:, kc, tc * 128:tc * 128 + tn], wrt[:, kc, :], start=(kc == 0), stop=(kc == KC - 1)),
                         reads=[hT.r, wrt.r], writes=[lp.r], inc=(kc == KC - 1))
                sc = sm.next()
                S.op("act", lambda e: e.activation(out=sc[:tn, :], in_=lp[:tn, :NE], func=AF.Sigmoid), reads=[lp.r], writes=[sc.r])
                bi = sm.next()
                S.op("dve", lambda e: e.tensor_tensor(out=bi[:tn, :], in0=sc[:tn, :], in1=ebt[:tn, :], op=ALU.add), reads=[sc.r, ebt.r], writes=[bi.r])
                bi3 = bi[:tn, :].rearrange("p (g j) -> p g j", j=G)
                m1 = sm8.next()
                S.op("dve", lambda e: e.tensor_reduce(out=m1[:tn, :], in_=bi3, axis=AX.X, op=ALU.max), reads=[bi.r], writes=[m1.r])
                eq = sm.next()
                eq3 = eq[:tn, :].rearrange("p (g j) -> p g j", j=G)
                S.op("dve", lambda e: e.tensor_tensor(out=eq3, in0=bi3, in1=m1[:tn, :].unsqueeze(2).broadcast_to([tn, CFG.NG, G]), op=ALU.is_equal),
                     reads=[bi.r, m1.r], writes=[eq.r])
                S.op("dve", lambda e: e.scalar_tensor_tensor(out=eq[:tn, :], in0=eq[:tn, :], scalar=-100.0, in1=bi[:tn, :], op0=ALU.mult, op1=ALU.add),
                     reads=[eq.r, bi.r], writes=[eq.r])
                m2 = sm8.next()
                S.op("dve", lambda e: e.tensor_reduce(out=m2[:tn, :], in_=eq3, axis=AX.X, op=ALU.max), reads=[eq.r], writes=[m2.r])
                S.op("dve", lambda e: e.tensor_tensor(out=m2[:tn, :], in0=m2[:tn, :], in1=m1[:tn, :], op=ALU.add), reads=[m1.r, m2.r], writes=[m2.r])
                t8 = sm8.next()
                S.op("dve", lambda e: e.max(out=t8[:tn, :], in_=m2[:tn, :]), reads=[m2.r], writes=[t8.r])
                gm_ = sm8.next()
                S.op("dve", lambda e: e.tensor_scalar(out=gm_[:tn, :], in0=m2[:tn, :], scalar1=t8[:tn, CFG.TOPG - 1:CFG.TOPG], scalar2=None, op0=ALU.is_ge),
                     reads=[m2.r, t8.r], writes=[gm_.r])
                mk = sm.next()
                mk3 = mk[:tn, :].rearrange("p (g j) -> p g j", j=G)
                gmb = gm_[:tn, :].unsqueeze(2).broadcast_to([tn, CFG.NG, G])
                S.op("dve", lambda e: e.tensor_tensor(out=mk3, in0=bi3, in1=gmb, op=ALU.mult), reads=[bi.r, gm_.r], writes=[mk.r])
                pen = sm8.next()
                S.op("dve", lambda e: e.tensor_scalar(out=pen[:tn, :], in0=gm_[:tn, :], scalar1=100.0, scalar2=-100.0, op0=ALU.mult, op1=ALU.add),
                     reads=[gm_.r], writes=[pen.r])
                S.op("dve", lambda e: e.tensor_tensor(out=mk3, in0=mk3, in1=pen[:tn, :].unsqueeze(2).broadcast_to([tn, CFG.NG, G]), op=ALU.add),
                     reads=[mk.r, pen.r], writes=[mk.r])
                t8b = sm8.next()
                S.op("dve", lambda e: e.max(out=t8b[:tn, :], in_=mk[:tn, :]), reads=[mk.r], writes=[t8b.r])
                sel = sm.next()
                S.op("dve", lambda e: e.tensor_scalar(out=sel[:tn, :], in0=mk[:tn, :], scalar1=t8b[:tn, CFG.TOPK - 1:CFG.TOPK], scalar2=None, op0=ALU.is_ge),
                     reads=[mk.r, t8b.r], writes=[sel.r])
                S.op("dve", lambda e: e.tensor_tensor(out=sel[:tn, :], in0=sel[:tn, :], in1=sc[:tn, :], op=ALU.mult), reads=[sel.r, sc.r], writes=[sel.r])
                ws = sm8.next()
                S.op("dve", lambda e: e.tensor_reduce(out=ws[:tn, 0:1], in_=sel[:tn, :], axis=AX.X, op=ALU.add), reads=[sel.r], writes=[ws.r])
                S.op("dve", lambda e: e.reciprocal(out=ws[:tn, 1:2], in_=ws[:tn, 0:1]), reads=[ws.r], writes=[ws.r])
                S.op("dve", lambda e: e.tensor_scalar(out=gbf[:tn, :], in0=sel[:tn, :], scalar1=ws[:tn, 1:2], scalar2=float(CFG.RSCALE), op0=ALU.mult, op1=ALU.mult),
                     reads=[sel.r, ws.r], writes=[gbf.r])
                S.op("pe", lambda e: e.transpose(out=pbt[:NE, :tn], in_=gbf[:tn, :], identity=ident[:tn, :tn]), reads=[gbf.r, ident.r], writes=[pbt.r])
                S.op("dve", lambda e: e.tensor_copy(out=gT[:NE, tc * 128:tc * 128 + tn], in_=pbt[:NE, :tn]), reads=[pbt.r], writes=[gT.r])
            for p in range(NPAIR + 1):
                shared = (p == NPAIR)
                nm = 2 if shared else 3
                gps = [pb[0], pb[1], pb[2]]
                ups = [pb[3], pb[4], pb[5]]
                for which, wsrc, ssrc, pss in ((0, wg, sg, gps), (1, wu, su, ups)):
                    KH = max(1, KC // 2)
                    for k0 in range(0, KC, KH):
                        u = arena.next()
                        nk = min(KH, KC - k0)
                        ncol = HD if shared else 2 * HD
                        uv = u.t[:, 0:nk * ncol].rearrange("p (kc n) -> p kc n", n=ncol)
                        if prep:
                            S.dma("pool", u.t[:, 0:nk * ncol], (wsh[which, k0 // KH] if shared else wgu[p, which, k0 // KH])[:, 0:nk * ncol], writes=[u.r])
                        elif shared:
                            S.dma("pool", uv, ssrc.rearrange("(kc p) n -> p kc n", p=128)[:, k0:k0 + nk, :], writes=[u.r])
                        else:
                            for j in range(2):
                                S.dma("pool", uv[:, :, j * HD:(j + 1) * HD], wsrc[2 * p + j].rearrange("(kc p) n -> p kc n", p=128)[:, k0:k0 + nk, :], writes=[u.r])
                        for m in range(nm):
                            mw = min(128, ncol - m * 128)
                            for kk in range(nk):
                                kc = k0 + kk
                                S.op("pe", lambda e: e.matmul(pss[m][:mw, :nt], uv[:, kk, m * 128:m * 128 + mw], hT[:, kc, :nt], start=(kc == 0), stop=(kc == KC - 1)),
                                     reads=[u.r, hT.r], writes=[pss[m].r], inc=(kk == nk - 1))
                gbs = []
                if not shared:
                    for j in range(2):
                        gb = pb[6]
                        e_ = 2 * p + j
                        selT = ident[0:NE, e_:e_ + 1].broadcast_to([NE, 128])
                        S.op("pe", lambda e: e.matmul(gb[:, :nt], selT, gT[0:NE, :nt], start=True, stop=True), reads=[ident.r, gT.r], writes=[gb.r])
                        gs_ = f32t.next()
                        S.op("act", lambda e: e.activation(out=gs_[:, :nt], in_=gb[:, :nt], func=AF.Copy), reads=[gb.r], writes=[gs_.r])
                        gbs.append(gs_)
                for m in range(nm):
                    c = 3 * p + m
                    mw = 128 if (not shared or m == 0) else HD - 128
                    sgl = f32t.next()
                    S.op("act", lambda e: e.activation(out=sgl[:mw, :nt], in_=gps[m][:mw, :nt], func=AF.Silu), reads=[gps[m].r], writes=[sgl.r])
                    if shared:
                        S.op("dve", lambda e: e.tensor_tensor(out=hid[:mw, c, :nt], in0=sgl[:mw, :nt], in1=ups[m][:mw, :nt], op=ALU.mult),
                             reads=[sgl.r, ups[m].r], writes=[hid_rs[c]])
                        continue
                    S.op("dve", lambda e: e.tensor_tensor(out=sgl[:, :nt], in0=sgl[:, :nt], in1=ups[m][:, :nt], op=ALU.mult),
                         reads=[sgl.r, ups[m].r], writes=[sgl.r])
                    if m == 1:
                        for j in range(2):
                            S.op("pool", lambda e: e.tensor_tensor(out=hid[64 * j:64 * j + 64, c, :nt], in0=sgl[64 * j:64 * j + 64, :nt],
                                                                   in1=gbs[j][64 * j:64 * j + 64, :nt], op=ALU.mult),
                                 reads=[sgl.r, gbs[j].r], writes=[hid_rs[c]])
                    else:
                        g_ = gbs[0 if m == 0 else 1]
                        S.op("pool", lambda e: e.tensor_tensor(out=hid[:, c, :nt], in0=sgl[:, :nt], in1=g_[:, :nt], op=ALU.mult),
                             reads=[sgl.r, g_.r], writes=[hid_rs[c]])
            s1, s2 = pb[2], pb[3]
            NR = 3 * NPAIR
            H1 = (NCH + 1) // 2
            for mc in range(KC):
                yp = pb[mc % 2]
                for half in range(2):
                    c0 = half * H1
                    c1 = min(NCH, c0 + H1)
                    u = arena.next()
                    uv = u.t[:, 0:(c1 - c0) * 128].rearrange("p (c n) -> p c n", n=128)
                    rc1 = min(c1, NR)
                    if prep:
                        S.dma("pool", u.t[:, 0:(c1 - c0) * 128], wdp[mc, half][:, 0:(c1 - c0) * 128], writes=[u.r])
                        rc1, c1s = c0, NR + 9
                    else:
                        c1s = c1
                    if rc1 > c0:
                        S.dma("pool", uv[:, 0:rc1 - c0, :], wd.rearrange("(c p) n -> p c n", p=128)[:, c0:rc1, mc * 128:(mc + 1) * 128], writes=[u.r])
                    if c1s > NR and not prep:
                        S.dma("pool", uv[:, NR - c0, :], sd[0:128, mc * 128:(mc + 1) * 128], writes=[u.r])
                        S.dma("pool", uv[0:HD - 128, NR + 1 - c0, :], sd[128:HD, mc * 128:(mc + 1) * 128], writes=[u.r])
                    for c in range(c0, c1):
                        kr = 128 if c != NCH - 1 else HD - 128
                        S.op("pe", lambda e: e.matmul(yp[:, :nt], uv[:kr, c - c0, :], hid[:kr, c, :nt], start=(c == 0), stop=(c == NCH - 1)),
                             reads=[u.r, hid_rs[c]], writes=[yp.r], inc=(c == c1 - 1))
                xc = f32t.next()
                S.dma("sp", xc[:, :nt], x1_scr[mc * 128:(mc + 1) * 128, 0:nt], writes=[xc.r])
                S.op("act", lambda e: e.activation(out=xc[:, :nt], in_=xc[:, :nt], func=AF.Copy, scale=float(ALPHA)), reads=[xc.r], writes=[xc.r])
                rc = f32t.next()
                S.op("dve", lambda e: e.scalar_tensor_tensor(out=rc[:, :nt], in0=yp[:, :nt], scalar=col(cb + 3, mc), in1=xc[:, :nt],
                                                             op0=ALU.mult, op1=ALU.add), reads=[yp.r, xc.r, colt.r], writes=[rc.r])
                stats_acc(rc, nt, mc, s1, s2)
                S.dma("sp", r_scr[mc * 128:(mc + 1) * 128, 0:nt], rc[:, :nt], reads=[rc.r], prim=rc.r)
            ln_stats(nt, s1, s2)
            S.barrier()
            for mc in range(KC):
                rc = f32t.next()
                S.dma("sp", rc[:, :nt], r_scr[mc * 128:(mc + 1) * 128, 0:nt], writes=[rc.r])
                S.op("dve", lambda e: e.tensor_tensor(out=rc[:, :nt], in0=rc[:, :nt], in1=mean[:, :nt], op=ALU.subtract),
                     reads=[rc.r, mean.r], writes=[rc.r])
                S.op("pool", lambda e: e.tensor_tensor(out=rc[:, :nt], in0=rc[:, :nt], in1=rstd[:, :nt], op=ALU.mult),
                     reads=[rc.r, rstd.r], writes=[rc.r])
                x2c = f32t.next()
                S.op("act", lambda e: e.activation(out=x2c[:, :nt], in_=rc[:, :nt], func=AF.Identity, scale=col(10, mc), bias=col(11, mc)),
                     reads=[rc.r, colt.r], writes=[x2c.r])
                for xo in x2outs:
                    S.dma("sp", xo[mc * 128:(mc + 1) * 128, t0:t0 + nt], x2c[:, :nt], reads=[x2c.r], prim=x2c.r)
            S.barrier()
        if fz:
            S.phase_end()
        else:
            S.finish()
    return nc


def na_geometry(ROWS, KH=CFG.NA_KH):
    geo, keys, reps = [], {}, []
    for rp in range(ROWS // 2):
        s0 = min(max(2 * rp - KH // 2, 0), ROWS - KH)
        s1 = min(max(2 * rp + 1 - KH // 2, 0), ROWS - KH)
        cl, ch = s0 // 2, (s1 + KH - 1) // 2
        key = (s0 - 2 * rp, s1 - 2 * rp, cl - rp, ch - cl)
        if key not in keys:
            keys[key] = len(reps)
            reps.append(rp)
        geo.append((cl, ch - cl + 1, keys[key]))
    return geo, reps


def na_bias_tables(rpb, ROWS, W=CFG.GRID_W, KH=CFG.NA_KH, KW=CFG.NA_KW, NEG=-30000.0):
    geo, reps = na_geometry(ROWS, KH)
    H = rpb.shape[0]
    out = np.full((H, 128, len(reps), 5, 128), NEG, np.float32)
    kk = np.arange(128)
    qq = np.arange(128)
    for v, rp in enumerate(reps):
        cl, nb, _ = geo[rp]
        qrow = 2 * rp + qq // W
        qcol = qq % W
        rs = np.clip(qrow - KH // 2, 0, ROWS - KH)
        cs = np.clip(qcol - KW // 2, 0, W - KW)
        for ci in range(nb):
            krow = 2 * (cl + ci) + kk // W
            kcol = kk % W
            ok = ((krow[:, None] >= rs[None, :]) & (krow[:, None] < rs[None, :] + KH)
                  & (kcol[:, None] >= cs[None, :]) & (kcol[:, None] < cs[None, :] + KW))
            dr = np.clip(krow[:, None] - qrow[None, :] + KH - 1, 0, 2 * KH - 2)
            dc = np.clip(kcol[:, None] - qcol[None, :], 1 - KW, KW - 1) + KW - 1
            g = rpb[:, dr, dc]
            out[:, :, v, ci, :] = np.where(ok[None], g, np.float32(NEG))
    return out


def build_na(D=CFG.D, NHC=4, ROWS=128, W=CFG.GRID_W, CTXN=CFG.CTX, B=CFG.B, debug=False, fz=None):
    KC = D // 128
    L = ROWS * W
    TT = L + CTXN
    NCHK = TT // 128
    LCH = L // 128
    NCC = CTXN // 128
    geo, reps = na_geometry(ROWS)
    NV = len(reps)
    scale = float(128 ** -0.5)
    nc = fz.nc if fz else bass.Bass("TRN2", target_bir_lowering=False)
    dr = _mk_dr(nc, fz)
    xT = dr("xT", [B, D, TT], F32)
    cols = dr("cols", [128, 6 * KC], F32)
    wqkv = dr("wqkv", [D, 3 * NHC * 128], F32)
    bias = dr("bias", [NHC, 128, NV * 5 * 128], F32)
    oT = dr("oT", [B, NHC * 128, TT], BF16, "ExternalOutput")
    qk_scr = dr("qk_scr", [B, 2 * NHC, 128, TT], BF16, "ExternalOutput" if debug else "Internal")
    v_scr = dr("v_scr", [B, NHC, 128, NCHK, 128], BF16, "ExternalOutput" if debug else "Internal")
    tiles = [(t0, min(512, L - t0), 0) for t0 in range(0, L, 512)] + [(L, CTXN, 1)]
    with ExitStack() as es:
        S = fz.S if fz else Sched(nc, es)
        S.tes = es
        S.prefix = fz.pre if fz else ""
        pb = [T(S, [128, 512], F32, "pb%d" % i, psum=True) for i in range(4)]
        st2 = [T(S, [128, 1024], F32, "st%d" % i, psum=True) for i in range(2)]
        colt = T(S, [128, 6 * KC], F32, "colt")
        ones_bf = T(S, [128, 128], BF16, "ones_bf")
        if fz:
            fz.setup(S)
            for (blk, src) in fz.colsrc:
                S.dma("sp", colt[:, blk * KC:(blk + 1) * KC], src, writes=[colt.r])
        else:
            S.dma("sp", colt[:], cols, writes=[colt.r])
        S.op("dve", lambda e: e.memset(ones_bf[:], 1.0), writes=[ones_bf.r])
        for c in range(3):
            S.op("dve", lambda e, c=c: e.tensor_scalar_add(colt[:, (2 * c + 1) * KC:(2 * c + 2) * KC], colt[:, (2 * c + 1) * KC:(2 * c + 2) * KC], 1.0),
                 reads=[colt.r], writes=[colt.r])
        col = lambda idx, kc: colt[:, idx * KC + kc: idx * KC + kc + 1]
        with ExitStack() as es1:
            S.tes = es1
            Wt = T(S, [128, KC, 3 * NHC * 128], BF16, "Wt")
            hT = T(S, [128, KC, 512], BF16, "hT")
            xcs = Rot([T(S, [128, 512], F32, "xc%d" % i) for i in range(4)])
            obs = Rot([T(S, [128, 512], BF16, "ob%d" % i) for i in range(4)])
            for j in range(3):
                S.dma("pool", Wt[:, :, j * NHC * 128:(j + 1) * NHC * 128],
                      wqkv.rearrange("(kc p) n -> p kc n", p=128)[:, :, j * NHC * 128:(j + 1) * NHC * 128], writes=[Wt.r])
            for b in range(B):
                for (t0, nt, isctx) in tiles:
                    cnd = 2 if isctx else b
                    for kc in range(KC):
                        xc = xcs.next()
                        if fz:
                            fz.xload(S, b, kc, t0, nt, isctx, xc)
                        else:
                            S.dma("sp", xc[:, :nt], xT[b, kc * 128:(kc + 1) * 128, t0:t0 + nt], writes=[xc.r])
                        S.op("dve", lambda e: e.tensor_scalar(out=hT[:, kc, :nt], in0=xc[:, :nt], scalar1=col(2 * cnd + 1, kc), scalar2=col(2 * cnd, kc),
                                                              op0=ALU.mult, op1=ALU.add), reads=[xc.r, colt.r], writes=[hT.r])
                    for j in range(2 * NHC):
                        ps = pb[j % 2]
                        for kc in range(KC):
                            S.op("pe", lambda e: e.matmul(ps[:, :nt], Wt[:, kc, j * 128:(j + 1) * 128], hT[:, kc, :nt], start=(kc == 0), stop=(kc == KC - 1)),
                                 reads=[Wt.r, hT.r], writes=[ps.r], inc=(kc == KC - 1))
                        ob = obs.next()
                        S.op("act", lambda e: e.activation(out=ob[:, :nt], in_=ps[:, :nt], func=AF.Copy), reads=[ps.r], writes=[ob.r])
                        S.dma("sp", qk_scr[b, j, :, t0:t0 + nt], ob[:, :nt], reads=[ob.r], prim=ob.r)
                    for tc in range(nt // 128):
                        ps = pb[2 + tc % 2]
                        for kc in range(KC):
                            S.op("pe", lambda e: e.matmul(ps[:, :NHC * 128], hT[:, kc, tc * 128:(tc + 1) * 128], Wt[:, kc, 2 * NHC * 128:3 * NHC * 128],
                                                          start=(kc == 0), stop=(kc == KC - 1)), reads=[Wt.r, hT.r], writes=[ps.r], inc=(kc == KC - 1))
                        ob = obs.next()
                        S.op("act", lambda e: e.activation(out=ob[:, :NHC * 128], in_=ps[:, :NHC * 128], func=AF.Copy), reads=[ps.r], writes=[ob.r])
                        chk = (t0 + tc * 128) // 128
                        S.dma("sp", v_scr[b, :, :, chk, :].rearrange("h p n -> p h n"), ob[:, :NHC * 128].rearrange("p (h n) -> p h n", n=128),
                              reads=[ob.r], prim=ob.r)
            S.barrier()
        with ExitStack() as es2:
            S.tes = es2
            qTs = Rot([T(S, [128, TT], BF16, "qT%d" % i) for i in range(2)])
            kTs = Rot([T(S, [128, TT], BF16, "kT%d" % i) for i in range(2)])
            Vs = Rot([T(S, [128, NCHK, 128], BF16, "V%d" % i) for i in range(2)])
            oTs = Rot([T(S, [128, TT], BF16, "oTh%d" % i) for i in range(2)])
            bts = Rot([T(S, [128, NV * 5 * 128], F32, "bt%d" % i) for i in range(2)])
            tbs = Rot([T(S, [128, 640], F32, "tb%d" % i) for i in range(2)])
            pTs = Rot([T(S, [128, 1024], BF16, "pT%d" % i) for i in range(2)])
            recs = Rot([T(S, [128, 256], F32, "rec%d" % i) for i in range(2)])
            pos = Rot([pb[0], pb[1]])
            pds = Rot([pb[2], pb[3]])
            sts = Rot(st2)
            for b in range(B):
                for h in range(NHC):
                    qT, kT, V, oTh, bt = qTs.next(), kTs.next(), Vs.next(), oTs.next(), bts.next()
                    S.dma("sp", qT[:], qk_scr[b, h], writes=[qT.r])
                    S.dma("sp", kT[:], qk_scr[b, NHC + h], writes=[kT.r])
                    S.dma("sp", V[:], v_scr[b, h], writes=[V.r])
                    S.dma("sp", bt[:], bias[h], writes=[bt.r])
                    for rp in range(ROWS // 2):
                        cl, nb, var = geo[rp]
                        st, po, pd, tb, pT, rec = sts.next(), pos.next(), pds.next(), tbs.next(), pTs.next(), recs.next()
                        qsl = qT[:, rp * 128:(rp + 1) * 128]
                        for ci in range(nb):
                            S.op("pe", lambda e: e.matmul(st[:, ci * 128:(ci + 1) * 128], kT[:, (cl + ci) * 128:(cl + ci + 1) * 128], qsl, start=True, stop=True),
                                 reads=[kT.r, qT.r], writes=[st.r], inc=False)
                        for cj in range(NCC):
                            S.op("pe", lambda e: e.matmul(st[:, (5 + cj) * 128:(6 + cj) * 128], kT[:, L + cj * 128:L + (cj + 1) * 128], qsl, start=True, stop=True),
                                 reads=[kT.r, qT.r], writes=[st.r], inc=(cj == NCC - 1))
                        S.op("dve", lambda e: e.scalar_tensor_tensor(out=tb[:, :nb * 128], in0=st[:, :nb * 128], scalar=scale,
                                                                     in1=bt[:, var * 640:var * 640 + nb * 128], op0=ALU.mult, op1=ALU.add),
                             reads=[st.r, bt.r], writes=[tb.r])
                        S.op("act", lambda e: e.activation(out=pT[:, :nb * 128], in_=tb[:, :nb * 128], func=AF.Exp), reads=[tb.r], writes=[pT.r])
                        S.op("act", lambda e: e.activation(out=pT[:, 640:640 + CTXN], in_=st[:, 640:640 + CTXN], func=AF.Exp, scale=scale), reads=[st.r], writes=[pT.r])
                        slots = [(cl + ci, ci) for ci in range(nb)] + [(LCH + cj, 5 + cj) for cj in range(NCC)]
                        for i, (kchunk, slot) in enumerate(slots):
                            S.op("pe", lambda e: e.matmul(po[:, 0:128], V[:, kchunk, :], pT[:, slot * 128:(slot + 1) * 128], start=(i == 0), stop=(i == len(slots) - 1)),
                                 reads=[V.r, pT.r], writes=[po.r], inc=False)
                            S.op("pe", lambda e: e.matmul(pd[:, 0:128], ones_bf[:], pT[:, slot * 128:(slot + 1) * 128], start=(i == 0), stop=(i == len(slots) - 1)),
                                 reads=[ones_bf.r, pT.r], writes=[pd.r], inc=(i == len(slots) - 1))
                        S.op("dve", lambda e: e.reciprocal(out=rec[:, 0:128], in_=pd[:, 0:128]), reads=[pd.r], writes=[rec.r])
                        S.op("dve", lambda e: e.tensor_tensor(out=oTh[:, rp * 128:(rp + 1) * 128], in0=po[:, 0:128], in1=rec[:, 0:128], op=ALU.mult),
                             reads=[po.r, rec.r], writes=[oTh.r])
                    st, po, pd, pT, rec = sts.next(), pos.next(), pds.next(), pTs.next(), recs.next()
                    for cj in range(NCC):
                        S.op("pe", lambda e: e.matmul(st[:, cj * CTXN:(cj + 1) * CTXN], kT[:, L + cj * 128:L + (cj + 1) * 128], qT[:, L:L + CTXN], start=True, stop=True),
                             reads=[kT.r, qT.r], writes=[st.r], inc=(cj == NCC - 1))
                    S.op("act", lambda e: e.activation(out=pT[:, :NCC * CTXN], in_=st[:, :NCC * CTXN], func=AF.Exp, scale=scale), reads=[st.r], writes=[pT.r])
                    for cj in range(NCC):
                        S.op("pe", lambda e: e.matmul(po[:, 0:CTXN], V[:, LCH + cj, :], pT[:, cj * CTXN:(cj + 1) * CTXN], start=(cj == 0), stop=(cj == NCC - 1)),
                             reads=[V.r, pT.r], writes=[po.r], inc=False)
                        S.op("pe", lambda e: e.matmul(pd[:, 0:CTXN], ones_bf[:], pT[:, cj * CTXN:(cj + 1) * CTXN], start=(cj == 0), stop=(cj == NCC - 1)),
                             reads=[ones_bf.r, pT.r], writes=[pd.r], inc=(cj == NCC - 1))
                    S.op("dve", lambda e: e.reciprocal(out=rec[:, 0:CTXN], in_=pd[:, 0:CTXN]), reads=[pd.r], writes=[rec.r])
                    S.op("dve", lambda e: e.tensor_tensor(out=oTh[:, L:L + CTXN], in0=po[:, 0:CTXN], in1=rec[:, 0:CTXN], op=ALU.mult),
                         reads=[po.r, rec.r], writes=[oTh.r])
                    if fz:
                        fz.owrite_na(S, b, h, oTh)
                    else:
                        S.dma("sp", oT[b, h * 128:(h + 1) * 128, :], oTh[:], reads=[oTh.r], prim=oTh.r)
            S.barrier()
        S.tes = es
        if fz:
            S.phase_end()
        else:
            S.finish()
    return nc


def gla_consts(ROWS, W=CFG.GRID_W):
    L = ROWS * W
    nf = 64
    inv = (np.float32(10000.0) ** (-np.arange(nf, dtype=np.float32) / np.float32(nf))).astype(np.float32)
    pos = np.arange(L)
    p = np.arange(128)
    cos = np.zeros((128, 2, L), np.float32)
    sin = np.zeros((128, 2, L), np.float32)
    for ch, pp in enumerate((pos // W, pos % W)):
        ang = pp.astype(np.float32)[None, :] * inv[p % 64][:, None]
        cos[:, ch, :] = np.cos(ang)
        sin[:, ch, :] = np.sin(ang) * np.where(p < 64, -1.0, 1.0)[:, None].astype(np.float32)
    perm = np.zeros((128, 128), np.float32)
    perm[(p + 64) % 128, p] = 1.0
    si = np.arange(64)
    msk = np.zeros((64, 2, 64), np.float32)
    msk[:, 0, :] = (si[:, None] <= si[None, :])
    msk[:, 1, :] = (si[:, None] >= si[None, :])
    rmask = np.ones((128, 512), np.float32)
    rmask[:, ::64] = 0.0
    return cos, sin, perm, msk, rmask


def build_gla(D=CFG.D, ROWS=128, W=CFG.GRID_W, CTXN=CFG.CTX, B=CFG.B, dbg=0, fz=None):
    KC = D // 128
    L = ROWS * W
    TT = L + CTXN
    NCK = TT // 64
    DK, DV = 256, 512
    nc = fz.nc if fz else bass.Bass("TRN2", target_bir_lowering=False)
    dr = _mk_dr(nc, fz)
    xT = dr("xT", [B, D, TT], F32)
    cols = dr("cols", [128, 6 * KC], F32)
    win = dr("win", [D, 1536], F32)
    wa1 = dr("wa1", [D, 32], F32)
    wa2p = dr("wa2p", [32, 2, 256], F32)
    ba = dr("ba", [128, 4], F32)
    nw = dr("nw", [128, 4], F32)
    cos_d = dr("cos", [128, 2, L], F32)
    sin_d = dr("sin", [128, 2, L], F32)
    perm_d = dr("perm_in", [128, 128], F32)
    ident_d = dr("ident_in", [128, 128], F32)
    msk_d = dr("msk_in", [64, 2, 64], F32)
    rmask_d = dr("rmask_in", [128, 512], F32)
    oT = dr("oT", [B, DV, TT], BF16, "ExternalOutput")
    F_scr = dr("F_scr", [B, 2, 3, 2, 128, TT], BF16, "Internal")
    ksl_scr = dr("ksl_scr", [B, 2, TT, DK], BF16, "Internal")
    v_scr = dr("v_scr", [B, TT, DV], BF16, "Internal")
    g_scr = dr("g_scr", [B, 4, 128, TT], BF16, "Internal")
    of_scr = dr("of_scr", [B, TT, DV], F32, "Internal")
    tiles = [(t0, min(512, L - t0), 0) for t0 in range(0, L, 512)] + [(L, CTXN, 1)]
    with ExitStack() as es:
        S = fz.S if fz else Sched(nc, es)
        S.tes = es
        S.prefix = fz.pre if fz else ""
        pb = [T(S, [128, 512], F32, "pb%d" % i, psum=True) for i in range(6)]
        pbt = [T(S, [128, 1024], BF16, "pbt%d" % i, psum=True) for i in range(2)]
        colt = T(S, [128, 6 * KC], F32, "colt")
        ident = T(S, [128, 128], BF16, "ident")
        bat = T(S, [128, 4], F32, "bat")
        nwt = T(S, [128, 4], F32, "nwt")
        ebl = T(S, [128, B * 4, NCK], F32, "ebl")
        msk = T(S, [64, 2, 64], F32, "msk")
        if fz:
            fz.setup(S)
            for (blk, src) in fz.colsrc:
                S.dma("sp", colt[:, blk * KC:(blk + 1) * KC], src, writes=[colt.r])
        else:
            S.dma("sp", colt[:], cols, writes=[colt.r])
        S.dma("pool", ident[:], ident_d, writes=[ident.r])
        S.dma("sp", bat[:], ba, writes=[bat.r])
        S.dma("sp", nwt[:], nw, writes=[nwt.r])
        S.dma("sp", msk[:], msk_d, writes=[msk.r])
        S.op("dve", lambda e: e.tensor_scalar_mul(bat[:], bat[:], -1.0), reads=[bat.r], writes=[bat.r])
        for c in range(3):
            S.op("dve", lambda e, c=c: e.tensor_scalar_add(colt[:, (2 * c + 1) * KC:(2 * c + 2) * KC], colt[:, (2 * c + 1) * KC:(2 * c + 2) * KC], 1.0),
                 reads=[colt.r], writes=[colt.r])
        col = lambda idx, kc: colt[:, idx * KC + kc: idx * KC + kc + 1]
        with ExitStack() as es1:
            S.tes = es1
            Wt = T(S, [128, KC, 1536], BF16, "Wt")
            Wa = T(S, [128, KC, 32], BF16, "Wa")
            Wa2 = T(S, [32, 2, 256], BF16, "Wa2")
            perm = T(S, [128, 128], F32, "perm")
            rmask = T(S, [128, 512], F32, "rmask")
            hT = T(S, [128, KC, 512], BF16, "hT")
            a_bf = T(S, [32, 512], BF16, "a_bf")
            xcs = Rot([T(S, [128, 512], F32, "xc%d" % i) for i in range(2)])
            ft = Rot([T(S, [128, 512], F32, "ft%d" % i) for i in range(8)])
            qk32 = [T(S, [128, 512], F32, "qk32_%d" % i) for i in range(4)]
            cum = [T(S, [128, 512], F32, "cum%d" % i) for i in range(4)]
            cst = T(S, [128, 2, 512], F32, "cst")
            snt = T(S, [128, 2, 512], F32, "snt")
            obs = Rot([T(S, [128, 512], BF16, "ob%d" % i) for i in range(6)])
            kslS = Rot([T(S, [128, 4, 256], BF16, "kslS%d" % i) for i in range(2)])
            for j in range(3):
                S.dma("pool", Wt[:, :, j * 512:(j + 1) * 512], win.rearrange("(kc p) n -> p kc n", p=128)[:, :, j * 512:(j + 1) * 512], writes=[Wt.r])
            S.dma("pool", Wa[:], wa1.rearrange("(kc p) n -> p kc n", p=128), writes=[Wa.r])
            S.dma("pool", Wa2[:], wa2p, writes=[Wa2.r])
            S.dma("sp", perm[:], perm_d, writes=[perm.r])
            S.dma("sp", rmask[:], rmask_d, writes=[rmask.r])
            for b in range(B):
                for (t0, nt, isctx) in tiles:
                    cnd = 2 if isctx else b
                    nck = nt // 64
                    ck0 = t0 // 64
                    for kc in range(KC):
                        xc = xcs.next()
                        if fz:
                            fz.xload(S, b, kc, t0, nt, isctx, xc)
                        else:
                            S.dma("sp", xc[:, :nt], xT[b, kc * 128:(kc + 1) * 128, t0:t0 + nt], writes=[xc.r])
                        S.op("dve", lambda e: e.tensor_scalar(out=hT[:, kc, :nt], in0=xc[:, :nt], scalar1=col(2 * cnd + 1, kc), scalar2=col(2 * cnd, kc),
                                                              op0=ALU.mult, op1=ALU.add), reads=[xc.r, colt.r], writes=[hT.r])
                    if not isctx:
                        S.dma("sp", cst[:, :, :nt], cos_d[:, :, t0:t0 + nt], writes=[cst.r])
                        S.dma("sp", snt[:, :, :nt], sin_d[:, :, t0:t0 + nt], writes=[snt.r])

                    def proj(j, ps, n=nt):
                        for kc in range(KC):
                            S.op("pe", lambda e: e.matmul(ps[:, :n], Wt[:, kc, j * 128:(j + 1) * 128], hT[:, kc, :n], start=(kc == 0), stop=(kc == KC - 1)),
                                 reads=[Wt.r, hT.r], writes=[ps.r], inc=(kc == KC - 1))
                    for j in range(4):
                        ps = pb[j % 2]
                        proj(j, ps)
                        dst = qk32[j]
                        if isctx:
                            S.op("act", lambda e: e.activation(out=dst[:, :nt], in_=ps[:, :nt], func=AF.Copy, scale=(1.0 / 16.0 if j < 2 else 1.0)),
                                 reads=[ps.r], writes=[dst.r])
                        else:
                            raw = ft.next()
                            S.op("act", lambda e: e.activation(out=raw[:, :nt], in_=ps[:, :nt], func=AF.Copy, scale=(1.0 / 16.0 if j < 2 else 1.0)),
                                 reads=[ps.r], writes=[raw.r])
                            sw = pb[2]
                            S.op("pe", lambda e: e.matmul(sw[:, :nt], perm[:], raw[:, :nt], start=True, stop=True), reads=[perm.r, raw.r], writes=[sw.r])
                            t1 = ft.next()
                            S.op("dve", lambda e: e.tensor_tensor(out=t1[:, :nt], in0=sw[:, :nt], in1=snt[:, j % 2, :nt], op=ALU.mult), reads=[sw.r, snt.r], writes=[t1.r])
                            S.op("pool", lambda e: e.tensor_tensor(out=raw[:, :nt], in0=raw[:, :nt], in1=cst[:, j % 2, :nt], op=ALU.mult), reads=[raw.r, cst.r], writes=[raw.r])
                            S.op("pool", lambda e: e.tensor_tensor(out=dst[:, :nt], in0=raw[:, :nt], in1=t1[:, :nt], op=ALU.add), reads=[raw.r, t1.r], writes=[dst.r])
                    for j in range(4):
                        ps = pb[j % 2]
                        proj(8 + j, ps)
                        ob = obs.next()
                        S.op("act", lambda e: e.activation(out=ob[:, :nt], in_=ps[:, :nt], func=AF.Silu), reads=[ps.r], writes=[ob.r])
                        S.dma("sp", g_scr[b, j, :, t0:t0 + nt], ob[:, :nt], reads=[ob.r], prim=ob.r)
                    for tc in range(nt // 128):
                        ps = pb[tc % 2]
                        for kc in range(KC):
                            S.op("pe", lambda e: e.matmul(ps[:, :DV], hT[:, kc, tc * 128:(tc + 1) * 128], Wt[:, kc, 512:1024], start=(kc == 0), stop=(kc == KC - 1)),
                                 reads=[Wt.r, hT.r], writes=[ps.r], inc=(kc == KC - 1))
                        ob = obs.next()
                        S.op("act", lambda e: e.activation(out=ob[:, :DV], in_=ps[:, :DV], func=AF.Copy), reads=[ps.r], writes=[ob.r])
                        S.dma("sp", v_scr[b, t0 + tc * 128:t0 + (tc + 1) * 128, :], ob[:, :DV], reads=[ob.r], prim=ob.r)
                    pa = pb[3]
                    for kc in range(KC):
                        S.op("pe", lambda e: e.matmul(pa[:32, :nt], Wa[:, kc, :], hT[:, kc, :nt], start=(kc == 0), stop=(kc == KC - 1)),
                             reads=[Wa.r, hT.r], writes=[pa.r], inc=(kc == KC - 1))
                    S.op("act", lambda e: e.activation(out=a_bf[:, :nt], in_=pa[:32, :nt], func=AF.Copy), reads=[pa.r], writes=[a_bf.r])
                    for d in range(2):
                        for ch in range(2):
                            pz = pb[4 + ch]
                            S.op("pe", lambda e: e.matmul(pz[:, :nt], Wa2[:, d, ch * 128:(ch + 1) * 128], a_bf[:, :nt], start=True, stop=True),
                                 reads=[Wa2.r, a_bf.r], writes=[pz.r])
                            l_ = ft.next()
                            S.op("act", lambda e: e.activation(out=l_[:, :nt], in_=pz[:, :nt], func=AF.Exp, scale=-1.0, bias=bat[:, d * 2 + ch:d * 2 + ch + 1]),
                                 reads=[pz.r, bat.r], writes=[l_.r])
                            S.op("act", lambda e: e.activation(out=l_[:, :nt], in_=l_[:, :nt], func=AF.Ln, bias=1.0), reads=[l_.r], writes=[l_.r])
                            cm = cum[d * 2 + ch]
                            S.op("dve", lambda e: e.tensor_tensor_scan(out=cm[:, :nt], data0=rmask[:, :nt], data1=l_[:, :nt], initial=0.0, op0=ALU.mult, op1=ALU.add),
                                 reads=[rmask.r, l_.r], writes=[cm.r])
                            if d == 1:
                                c3 = cm[:, :nt].rearrange("p (c j) -> p c j", j=64)
                                l3 = l_[:, :nt].rearrange("p (c j) -> p c j", j=64)
                                S.op("pool", lambda e: e.tensor_tensor(out=l3, in0=l3, in1=c3[:, :, 63:64].broadcast_to([128, nck, 64]), op=ALU.add),
                                     reads=[l_.r, cm.r], writes=[l_.r])
                                S.op("pool", lambda e: e.tensor_tensor(out=cm[:, :nt], in0=l_[:, :nt], in1=cm[:, :nt], op=ALU.subtract), reads=[l_.r, cm.r], writes=[cm.r])
                    for d in range(2):
                        im, il = (32, 63) if d == 0 else (31, 0)
                        for ch in range(2):
                            cm = cum[d * 2 + ch]
                            c3 = cm[:, :nt].rearrange("p (c j) -> p c j", j=64)
                            qr, kr = qk32[ch], qk32[2 + ch]
                            dm = ft.next()
                            dm3 = dm[:, :nt].rearrange("p (c j) -> p c j", j=64)
                            S.op("dve", lambda e: e.tensor_tensor(out=dm3, in0=c3, in1=c3[:, :, im:im + 1].broadcast_to([128, nck, 64]), op=ALU.subtract),
                                 reads=[cm.r], writes=[dm.r])
                            specs = []
                            e1 = ft.next()
                            S.op("act", lambda e: e.activation(out=e1[:, :nt], in_=dm[:, :nt], func=AF.Exp, scale=-1.0 / 16.0), reads=[dm.r], writes=[e1.r])
                            specs.append((0, qr, e1))
                            e2 = ft.next()
                            S.op("act", lambda e: e.activation(out=e2[:, :nt], in_=dm[:, :nt], func=AF.Exp, scale=1.0 / 16.0), reads=[dm.r], writes=[e2.r])
                            specs.append((1, kr, e2))
                            e3 = ft.next()
                            S.op("act", lambda e: e.activation(out=e3[:, :nt], in_=cm[:, :nt], func=AF.Exp, scale=-1.0 / 16.0), reads=[cm.r], writes=[e3.r])
                            specs.append((2, qr, e3))
                            e33 = e3[:, :nt].rearrange("p (c j) -> p c j", j=64)
                            S.op("pool", lambda e: e.tensor_copy(out=ebl[:, b * 4 + d * 2 + ch, ck0:ck0 + nck], in_=e33[:, :, il]), reads=[e3.r], writes=[ebl.r])
                            for (which, src, ex) in specs:
                                ob = obs.next()
                                S.op("dve" if which != 1 else "pool", lambda e: e.tensor_tensor(out=ob[:, :nt], in0=src[:, :nt], in1=ex[:, :nt], op=ALU.mult),
                                     reads=[src.r, ex.r], writes=[ob.r])
                                S.dma("sp", F_scr[b, d, which, ch, :, t0:t0 + nt], ob[:, :nt], reads=[ob.r], prim=ob.r)
                            dl = ft.next()
                            dl3 = dl[:, :nt].rearrange("p (c j) -> p c j", j=64)
                            S.op("dve", lambda e: e.tensor_tensor(out=dl3, in0=c3, in1=c3[:, :, il:il + 1].broadcast_to([128, nck, 64]), op=ALU.subtract),
                                 reads=[cm.r], writes=[dl.r])
                            S.op("act", lambda e: e.activation(out=dl[:, :nt], in_=dl[:, :nt], func=AF.Exp, scale=1.0 / 16.0), reads=[dl.r], writes=[dl.r])
                            kb = obs.next()
                            S.op("dve", lambda e: e.tensor_tensor(out=kb[:, :nt], in0=kr[:, :nt], in1=dl[:, :nt], op=ALU.mult), reads=[kr.r, dl.r], writes=[kb.r])
                            if ch == 0:
                                ks_t = kslS.next()
                            pt = pbt[ch]
                            for tc in range(nt // 128):
                                S.op("pe", lambda e: e.transpose(out=pt[:, tc * 128:(tc + 1) * 128], in_=kb[:, tc * 128:(tc + 1) * 128], identity=ident[:]),
                                     reads=[kb.r, ident.r], writes=[pt.r], inc=(tc == nt // 128 - 1))
                            S.op("act", lambda e: e.activation(out=ks_t[:, :nt // 128, ch * 128:(ch + 1) * 128], in_=pt[:, :nt].rearrange("p (t n) -> p t n", n=128), func=AF.Copy),
                                 reads=[pt.r], writes=[ks_t.r])
                            if ch == 1:
                                S.dma("sp", ksl_scr[b, d, t0:t0 + nt, :].rearrange("(t p) n -> p t n", p=128), ks_t[:, :nt // 128, :], reads=[ks_t.r], prim=ks_t.r)
            S.barrier()
        with ExitStack() as es2:
            S.tes = es2
            if dbg == 1:
                B = 0
            St = T(S, [128, 2, DV], F32, "St")
            Sb = T(S, [128, 2, DV], BF16, "Sb")
            Fb = Rot([T(S, [128, 6, 512], BF16, "Fb%d" % i) for i in range(2)])
            kslb = Rot([T(S, [64, 8, DK], BF16, "kslb%d" % i) for i in range(2)])
            vb = Rot([T(S, [64, 8, DV], BF16, "vb%d" % i) for i in range(2)])
            ofb = Rot([T(S, [64, 8, DV], F32, "ofb%d" % i) for i in range(2)])
            gb = Rot([T(S, [128, 4, 512], BF16, "gb%d" % i) for i in range(2)])
            oTb = Rot([T(S, [128, 4, 512], BF16, "oTb%d" % i) for i in range(2)])
            atb = Rot([T(S, [64, 64], BF16, "atb%d" % i) for i in range(2)])
            osb = Rot([T(S, [64, DV], F32, "osb%d" % i) for i in range(2)])
            onb = Rot([T(S, [64, DV], BF16, "onb%d" % i) for i in range(2)])
            sq = T(S, [64, DV], F32, "sqj")
            msb = Rot([T(S, [64, 4], F32, "msb%d" % i) for i in range(2)])
            tmb = Rot([T(S, [128, 4, 64], F32, "tmb%d" % i) for i in range(2)])
            pat = Rot([pb[0], pb[1]])
            pos_ = Rot([pb[2], pb[3]])
            for b in range(B):
                for d in range(1 if dbg == 2 else 2):
                    S.op("dve", lambda e: e.memset(St[:], 0.0), writes=[St.r])
                    S.op("dve", lambda e: e.memset(Sb[:], 0.0), writes=[Sb.r])
                    lat = [t for t in tiles if not t[2]]
                    order = [tiles[-1]] + (lat if d == 0 else lat[::-1])
                    for (t0, nt, isctx) in order:
                        nck = nt // 64
                        F, ksl, v = Fb.next(), kslb.next(), vb.next()
                        for which in range(3):
                            for ch in range(2):
                                S.dma("sp", F[:, which * 2 + ch, :nt], F_scr[b, d, which, ch, :, t0:t0 + nt], writes=[F.r])
                        S.dma("sp", ksl[:, :nck, :], ksl_scr[b, d, t0:t0 + nt, :].rearrange("(c p) n -> p c n", p=64), writes=[ksl.r])
                        S.dma("sp", v[:, :nck, :], v_scr[b, t0:t0 + nt, :].rearrange("(c p) n -> p c n", p=64), writes=[v.r])
                        of = ofb.next()
                        if d == 1:
                            g, oTt = gb.next(), oTb.next()
                            S.dma("sp", of[:, :nck, :], of_scr[b, t0:t0 + nt, :].rearrange("(c p) n -> p c n", p=64), writes=[of.r])
                            for j in range(4):
                                S.dma("sp", g[:, j, :nt], g_scr[b, j, :, t0:t0 + nt], writes=[g.r])
                        for c in (range(nck) if d == 0 else range(nck - 1, -1, -1)):
                            cs = c * 64
                            gck = t0 // 64 + c
                            pa_ = pat.next()
                            for ch in range(2):
                                S.op("pe", lambda e: e.matmul(pa_[:64, :64], F[:, 2 + ch, cs:cs + 64], F[:, 0 + ch, cs:cs + 64], start=(ch == 0), stop=(ch == 1)),
                                     reads=[F.r], writes=[pa_.r], inc=(ch == 1))
                            at = atb.next()
                            S.op("dve", lambda e: e.tensor_tensor(out=at[:], in0=pa_[:64, :64], in1=msk[:, d, :], op=ALU.mult), reads=[pa_.r, msk.r], writes=[at.r])
                            po = pos_.next()
                            S.op("pe", lambda e: e.matmul(po[:64, :DV], at[:], v[:, c, :], start=True, stop=False), reads=[at.r, v.r], writes=[po.r], inc=False)
                            for ch in range(2):
                                S.op("pe", lambda e: e.matmul(po[:64, :DV], F[:, 4 + ch, cs:cs + 64], Sb[:, ch, :], start=False, stop=(ch == 1)),
                                     reads=[F.r, Sb.r], writes=[po.r], inc=(ch == 1))
                            for ch in range(2):
                                pd_ = pb[4 + ch]
                                S.op("pe", lambda e: e.matmul(pd_[:, :DV], ksl[:, c, ch * 128:(ch + 1) * 128], v[:, c, :], start=True, stop=True),
                                     reads=[ksl.r, v.r], writes=[pd_.r])
                                S.op("dve", lambda e: e.scalar_tensor_tensor(out=St[:, ch, :], in0=St[:, ch, :], scalar=ebl[:, b * 4 + d * 2 + ch, gck:gck + 1],
                                                                             in1=pd_[:, :DV], op0=ALU.mult, op1=ALU.add), reads=[St.r, ebl.r, pd_.r], writes=[St.r])
                                S.op("act", lambda e: e.activation(out=Sb[:, ch, :], in_=St[:, ch, :], func=AF.Copy), reads=[St.r], writes=[Sb.r])
                            if d == 0:
                                S.op("act", lambda e: e.activation(out=of[:, c, :], in_=po[:64, :DV], func=AF.Copy), reads=[po.r], writes=[of.r])
                            else:
                                os_, on_, ms = osb.next(), onb.next(), msb.next()
                                S.op("dve", lambda e: e.tensor_tensor(out=os_[:], in0=po[:64, :DV], in1=of[:, c, :], op=ALU.add), reads=[po.r, of.r], writes=[os_.r])
                                if dbg == 4:
                                    continue
                                S.op("pool", lambda e: e.tensor_tensor(out=sq[:], in0=os_[:], in1=os_[:], op=ALU.mult), reads=[os_.r], writes=[sq.r])
                                S.op("dve", lambda e: e.tensor_reduce(out=ms[:, 0:1], in_=sq[:], axis=AX.X, op=ALU.add), reads=[sq.r], writes=[ms.r])
                                S.op("dve", lambda e: e.tensor_scalar(out=ms[:, 1:2], in0=ms[:, 0:1], scalar1=1.0 / DV, scalar2=float(CFG.EPS), op0=ALU.mult, op1=ALU.add),
                                     reads=[ms.r], writes=[ms.r])
                                S.op("act", lambda e: e.activation(out=ms[:, 2:3], in_=ms[:, 1:2], func=AF.Sqrt), reads=[ms.r], writes=[ms.r])
                                S.op("dve", lambda e: e.reciprocal(out=ms[:, 3:4], in_=ms[:, 2:3]), reads=[ms.r], writes=[ms.r])
                                S.op("dve", lambda e: e.tensor_scalar_mul(on_[:], os_[:], ms[:, 3:4]), reads=[os_.r, ms.r], writes=[on_.r])
                                pt = pbt[(gck) % 2]
                                if dbg == 3:
                                    continue
                                for j in range(4):
                                    S.op("pe", lambda e: e.transpose(out=pt[:, j * 64:(j + 1) * 64], in_=on_[:, j * 128:(j + 1) * 128], identity=ident[:64, :64]),
                                         reads=[on_.r, ident.r], writes=[pt.r], inc=(j == 3))
                                tm = tmb.next()
                                for j in range(4):
                                    S.op("act", lambda e: e.activation(out=tm[:, j, :], in_=pt[:, j * 64:(j + 1) * 64], func=AF.Copy, scale=nwt[:, j:j + 1]),
                                         reads=[pt.r, nwt.r], writes=[tm.r])
                                S.op("dve", lambda e: e.tensor_tensor(out=oTt[:, :, cs:cs + 64], in0=tm[:], in1=g[:, :, cs:cs + 64], op=ALU.mult),
                                     reads=[tm.r, g.r], writes=[oTt.r])
                        if d == 0:
                            S.dma("sp", of_scr[b, t0:t0 + nt, :].rearrange("(c p) n -> p c n", p=64), of[:, :nck, :], reads=[of.r], prim=of.r)
                        elif fz:
                            fz.owrite_gla(S, b, t0, nt, isctx, oTt)
                        else:
                            S.dma("sp", oT[b].rearrange("(j p) t -> p j t", p=128)[:, :, t0:t0 + nt], oTt[:, :, :nt], reads=[oTt.r], prim=oTt.r)
                    S.barrier()
        S.tes = es
        if fz:
            S.phase_end()
        else:
            S.finish()
    return nc


def build_ada(D=CFG.D, R=CFG.ADA_R, NL=CFG.DEPTH, NCND=3, fz=None):
    KC = D // 128
    NJ = 6 * D // 128
    RC = R // 128
    HJ = NJ // 2
    nc = fz.nc if fz else bass.Bass("TRN2", target_bir_lowering=False)
    dr = _mk_dr(nc, fz)
    condT = dr("condT", [128, KC, NCND], F32)
    w1 = dr("w1", [NL, D, R], F32)
    w2 = dr("w2", [NL, R, 6 * D], F32)
    bT = dr("bT", [NL, 128, NJ], F32)
    mods = dr("mods", [NL, NCND, 128, NJ], F32, "ExternalOutput")
    with ExitStack() as es:
        S = fz.S if fz else Sched(nc, es)
        S.tes = es
        S.prefix = fz.pre if fz else ""
        ct = T(S, [128, KC, NCND], F32, "ct")
        sT = T(S, [128, KC, NCND], BF16, "sT")
        W1 = T(S, [128, KC, R], BF16, "W1")
        W2 = T(S, [128, RC, 6 * D], BF16, "W2")
        bt = T(S, [128, NJ], F32, "bt")
        ub = T(S, [128, RC, NCND], BF16, "ub")
        res = T(S, [128, NCND, NJ], F32, "res")
        pu = T(S, [128, 512], F32, "pu", psum=True)
        pm = [T(S, [128, 512], F32, "pm%d" % i, psum=True) for i in range(2)]
        S.dma("sp", ct[:], condT, writes=[ct.r])
        S.op("act", lambda e: e.activation(out=sT[:], in_=ct[:], func=AF.Silu), reads=[ct.r], writes=[sT.r])
        for l in range(NL):
            S.dma("pool", W1[:], w1[l].rearrange("(kc p) n -> p kc n", p=128), writes=[W1.r])
            for q in range(6):
                S.dma("pool", W2[:, :, q * D:(q + 1) * D], w2[l].rearrange("(kc p) n -> p kc n", p=128)[:, :, q * D:(q + 1) * D], writes=[W2.r])
            S.dma("sp", bt[:], bT[l], writes=[bt.r])
            for m in range(RC):
                for kc in range(KC):
                    S.op("pe", lambda e: e.matmul(pu[:, m * 8:m * 8 + NCND], W1[:, kc, m * 128:(m + 1) * 128], sT[:, kc, :], start=(kc == 0), stop=(kc == KC - 1)),
                         reads=[W1.r, sT.r], writes=[pu.r], inc=(kc == KC - 1))
            for m in range(RC):
                S.op("act", lambda e: e.activation(out=ub[:, m, :], in_=pu[:, m * 8:m * 8 + NCND], func=AF.Copy), reads=[pu.r], writes=[ub.r])
            for j in range(NJ):
                p_ = pm[j // HJ]
                o_ = (j % HJ) * NCND
                for m in range(RC):
                    S.op("pe", lambda e: e.matmul(p_[:, o_:o_ + NCND], W2[:, m, j * 128:(j + 1) * 128], ub[:, m, :], start=(m == 0), stop=(m == RC - 1)),
                         reads=[W2.r, ub.r], writes=[p_.r], inc=(m == RC - 1))
            for half in range(2):
                S.op("dve", lambda e: e.tensor_tensor(out=res[:, :, half * HJ:(half + 1) * HJ].rearrange("p c j -> p j c"),
                                                      in0=pm[half][:, :HJ * NCND].rearrange("p (j c) -> p j c", c=NCND),
                                                      in1=bt[:, half * HJ:(half + 1) * HJ].unsqueeze(2).broadcast_to([128, HJ, NCND]), op=ALU.add),
                     reads=[pm[half].r, bt.r], writes=[res.r])
            S.dma("sp", mods[l].rearrange("c p j -> p c j"), res[:], reads=[res.r], prim=res.r)
        if fz:
            S.phase_end()
        else:
            S.finish()
    return nc


def build_prep(NL=CFG.DEPTH, D=CFG.D, NE=CFG.NE, HD=CFG.HD):
    KC = D // 128
    KH = max(1, KC // 2)
    NKH = (KC + KH - 1) // KH
    NPAIR = NE // 2
    NPC = NPAIR // 8
    NCH = 3 * NPAIR + 2
    NR = 3 * NPAIR
    H1 = (NCH + 1) // 2
    MCC = KC // 8
    CW = MCC * 128
    nc = bass.Bass("TRN2", target_bir_lowering=False)
    dr = _mk_dr(nc, None)
    wg = dr("wg", [NL, 2 * NPC, D, HD], F32)
    wu = dr("wu", [NL, 2 * NPC, D, HD], F32)
    sg = dr("sg", [NL, D, HD], F32)
    su = dr("su", [NL, D, HD], F32)
    wd = dr("wd", [NL, NE * HD, CW], F32)
    sd = dr("sd", [NL, HD, CW], F32)
    wo = dr("wo", [NL, D, CW], F32)
    o_gu = dr("o_gu", [NL, NPC, 2, NKH, 128, KH * 2 * HD], BF16, "ExternalOutput")
    o_sh = dr("o_sh", [NL, 2, NKH, 128, KH * HD], BF16, "ExternalOutput")
    o_wd = dr("o_wd", [NL, MCC, 2, 128, H1 * 128], BF16, "ExternalOutput")
    o_wo = dr("o_wo", [NL, MCC, 128, KC * 128], BF16, "ExternalOutput")
    with ExitStack() as es:
        S = Sched(nc, es)
        USZ = max(KH * 2 * HD, H1 * 128, KC * 128)
        arena = Rot([T(S, [128, USZ], BF16, "ar%d" % i) for i in range(4)])
        zt = T(S, [128, 128], BF16, "zt")
        S.op("dve", lambda e: e.memset(zt[:], 0.0), writes=[zt.r])
        for l in range(NL):
            for which, wsrc, ssrc in ((0, wg, sg), (1, wu, su)):
                for kh in range(NKH):
                    k0 = kh * KH
                    nk = min(KH, KC - k0)
                    for p in range(NPC):
                        u = arena.next()
                        uv = u.t[:, 0:nk * 2 * HD].rearrange("p (kc n) -> p kc n", n=2 * HD)
                        for j in range(2):
                            S.dma("pool", uv[:, :, j * HD:(j + 1) * HD], wsrc[l, 2 * p + j].rearrange("(kc p) n -> p kc n", p=128)[:, k0:k0 + nk, :], writes=[u.r])
                        S.dma("sp", o_gu[l, p, which, kh][:, 0:nk * 2 * HD], u.t[:, 0:nk * 2 * HD], reads=[u.r], prim=u.r)
                    u = arena.next()
                    uv = u.t[:, 0:nk * HD].rearrange("p (kc n) -> p kc n", n=HD)
                    S.dma("pool", uv, ssrc[l].rearrange("(kc p) n -> p kc n", p=128)[:, k0:k0 + nk, :], writes=[u.r])
                    S.dma("sp", o_sh[l, which, kh][:, 0:nk * HD], u.t[:, 0:nk * HD], reads=[u.r], prim=u.r)
            for m4 in range(MCC):
                for half in range(2):
                    c0 = half * H1
                    c1 = min(NCH, c0 + H1)
                    u = arena.next()
                    uv = u.t[:, 0:(c1 - c0) * 128].rearrange("p (c n) -> p c n", n=128)
                    rc1 = min(c1, NR)
                    if rc1 > c0:
                        S.dma("pool", uv[:, 0:rc1 - c0, :], wd[l].rearrange("(c p) n -> p c n", p=128)[:, c0:rc1, m4 * 128:(m4 + 1) * 128], writes=[u.r])
                    if c1 > NR:
                        S.op("pool", lambda e: e.tensor_copy(out=uv[:, NR + 1 - c0, :], in_=zt[:]), reads=[zt.r], writes=[u.r])
                        S.dma("pool", uv[:, NR - c0, :], sd[l][0:128, m4 * 128:(m4 + 1) * 128], writes=[u.r])
                        S.dma("pool", uv[0:HD - 128, NR + 1 - c0, :], sd[l][128:HD, m4 * 128:(m4 + 1) * 128], writes=[u.r])
                    S.dma("sp", o_wd[l, m4, half][:, 0:(c1 - c0) * 128], u.t[:, 0:(c1 - c0) * 128], reads=[u.r], prim=u.r)
                u = arena.next()
                uv = u.t[:, 0:KC * 128].rearrange("p (kc n) -> p kc n", n=128)
                S.dma("pool", uv, wo[l].rearrange("(kc p) n -> p kc n", p=128)[:, :, m4 * 128:(m4 + 1) * 128], writes=[u.r])
                S.dma("sp", o_wo[l, m4], u.t[:, 0:KC * 128], reads=[u.r], prim=u.r)
        S.finish()
    return nc


def _run_prep(p, NLAY, NE):
    D = CFG.D
    KC = D // 128
    CW = (KC // 8) * 128
    NPC = NE // 2 // 8
    in_maps = []
    for k in range(8):
        wo = np.stack([(p["na_w_o"][l // 2] if l % 2 == 0 else p["gla_w_o"][l // 2])[:, k * CW:(k + 1) * CW] for l in range(NLAY)])
        in_maps.append(dict(
            wg=np.ascontiguousarray(p["moe_w_gate"][:NLAY, 2 * NPC * k:2 * NPC * (k + 1)]),
            wu=np.ascontiguousarray(p["moe_w_up"][:NLAY, 2 * NPC * k:2 * NPC * (k + 1)]),
            sg=np.ascontiguousarray(p["sh_w_gate"][:NLAY]), su=np.ascontiguousarray(p["sh_w_up"][:NLAY]),
            wd=np.ascontiguousarray(p["moe_w_down"][:NLAY, :NE, :, k * CW:(k + 1) * CW]).reshape(NLAY, NE * CFG.HD, CW),
            sd=np.ascontiguousarray(p["sh_w_down"][:NLAY, :, k * CW:(k + 1) * CW]), wo=np.ascontiguousarray(wo)))
    outs = _run(_prog(("prep", NLAY, NE), lambda: build_prep(NL=NLAY, NE=NE)), in_maps)
    res = []
    for l in range(NLAY):
        res.append(dict(wgu=np.ascontiguousarray(np.concatenate([o["o_gu"][l] for o in outs], axis=0)), wsh=np.ascontiguousarray(outs[0]["o_sh"][l]),
                        wdp=np.ascontiguousarray(np.concatenate([o["o_wd"][l] for o in outs], axis=0)),
                        wop=np.ascontiguousarray(np.concatenate([o["o_wo"][l] for o in outs], axis=0))))
    return res


def build_fused(L=CFG.SEQ, NLAY=CFG.DEPTH, NE=CFG.NE):
    D, KC, B, CT, HD = CFG.D, CFG.D // 128, CFG.B, CFG.CTX, CFG.HD
    ROWS = L // CFG.GRID_W
    LS, CS = L // 4, CT // 4
    TS, TT = LS + CS, L + CT
    NJ = 6 * KC
    nc = bass.Bass("TRN2", target_bir_lowering=False)
    din = lambda n, s, d: nc.dram_tensor(n, s, d, kind="ExternalInput").ap()
    xs = din("xs", [D, TS], F32)
    mask8_d = din("mask8", [128, 8], F32)
    ident_in = din("ident_in", [128, 128], F32)
    gconst = dict(cos=din("cos", [128, 2, L], F32), sin=din("sin", [128, 2, L], F32), perm_in=din("perm_in", [128, 128], F32),
                  msk_in=din("msk_in", [64, 2, 64], F32), rmask_in=din("rmask_in", [128, 512], F32))
    x_out = nc.dram_tensor("x_out", [D, TS], F32, kind="ExternalOutput").ap()
    xsh = nc.dram_tensor("xsh", [D, TS], F32).ap()
    DH = D // 2
    XGs = [nc.dram_tensor("XG%d" % i, [8 * DH, TS], F32, addr_space="Shared").ap() for i in range(2)]
    OSC = nc.dram_tensor("OSC", [8 * D, TS], BF16).ap()
    OSH = nc.dram_tensor("OSH", [D, TS], BF16).ap()
    modsD = nc.dram_tensor("modsD", [NLAY, 4, 128, NJ], F32).ap()
    r_scr = nc.dram_tensor("r_scr", [D, 512], F32).ap()
    x1_scr = nc.dram_tensor("x1_scr", [D, 512], F32).ap()
    ffn_tiles = [(t0, min(512, LS - t0), 0) for t0 in range(0, LS, 512)] + [(LS, CS, 1)]

    def segs_lat(b, t0, nt):
        out, t = [], t0
        while t < t0 + nt:
            r = t // LS
            ln = min(t0 + nt, (r + 1) * LS) - t
            out.append((b * 4 + r, t % LS, t - t0, ln))
            t += ln
        return out

    def segs_ctx(b, base):
        return [(b * 4 + qq, LS, base + qq * CS, CS) for qq in range(4)]

    class Hooks:
        pass
    hk = Hooks()

    def setup(S):
        hk.m8 = T(S, [128, 8], F32, "m8")
        S.dma("sp", hk.m8[:], mask8_d, writes=[hk.m8.r])
        hk.tmps = Rot([T(S, [128, 2048], BF16, "mt%d" % i) for i in range(2)])

    def xload(S, b, kc, t0, nt, isctx, xc):
        sg = segs_ctx(b, 0) if isctx else segs_lat(b, t0, nt)
        for (r, c0, off, ln) in sg:
            hf, kk = divmod(kc * 128, DH)
            S.dma("sp", xc[:, off:off + ln], XGs[hf][r * DH + kk:r * DH + kk + 128, c0:c0 + ln], writes=[xc.r])

    def owrite_na(S, b, h, oTh):
        sg = [(b * 4 + r, 0, r * LS, LS) for r in range(4)] + segs_ctx(b, L)
        for (r, c0, off, ln) in sg:
            for p0 in range(0, ln, 2048):
                pl = min(2048, ln - p0)
                for s_ in range(8):
                    tmp = hk.tmps.next()
                    S.op("pool", lambda e: e.tensor_scalar_mul(tmp[:, :pl], oTh[:, off + p0:off + p0 + pl], hk.m8[:, s_:s_ + 1]),
                         reads=[oTh.r, hk.m8.r], writes=[tmp.r])
                    row = r * D + s_ * 512 + h * 128
                    S.dma("sp", OSC[row:row + 128, c0 + p0:c0 + p0 + pl], tmp[:, :pl], reads=[tmp.r], prim=tmp.r)

    def owrite_gla(S, b, t0, nt, isctx, oTt):
        sg = segs_ctx(b, 0) if isctx else segs_lat(b, t0, nt)
        for (r, c0, off, ln) in sg:
            for s_ in range(8):
                tmp = hk.tmps.next()
                tv = tmp.t[:, 0:4 * ln].rearrange("p (j t) -> p j t", j=4)
                S.op("pool", lambda e: e.tensor_scalar_mul(tv, oTt[:, :, off:off + ln], hk.m8[:, s_:s_ + 1]), reads=[oTt.r, hk.m8.r], writes=[tmp.r])
                row = r * D + s_ * 512
                S.dma("sp", OSC[row:row + 512, c0:c0 + ln].rearrange("(j p) t -> p j t", p=128), tv, reads=[tmp.r], prim=tmp.r)

    with ExitStack() as es0:
        S = Sched(nc, es0)
        build_ada(NL=NLAY, NCND=4, fz=FZ(nc, S, "A_", io=dict(mods=modsD)))
        cp = Res("xcp")
        for i in range(8):
            S.dma("sp", xsh[i * D // 8:(i + 1) * D // 8, :], xs[i * D // 8:(i + 1) * D // 8, :], prim=cp)
        for i in range(2):
            S.cc("AllGather", ALU.bypass, xsh[i * DH:(i + 1) * DH, :], XGs[i])
        for l in range(NLAY):
            pre = "L%d_" % l
            mcolsrc = []
            for cnd in range(3):
                mcolsrc.append((2 * cnd, modsD[l, cnd, :, 0:KC]))
                mcolsrc.append((2 * cnd + 1, modsD[l, cnd, :, KC:2 * KC]))
            mio = dict(xT=None, cols=None, oT=None, ident_in=ident_in)
            if l % 2 == 0:
                build_na(ROWS=ROWS, fz=FZ(nc, S, pre + "m_", io=mio, setup=setup, xload=xload, owrite_na=owrite_na, colsrc=mcolsrc))
            else:
                mio.update(gconst)
                build_gla(ROWS=ROWS, fz=FZ(nc, S, pre + "m_", io=mio, setup=setup, xload=xload, owrite_gla=owrite_gla, colsrc=mcolsrc))
            S.cc("ReduceScatter", ALU.add, OSC, OSH)
            lncols = din(pre + "lncols", [128, 4 * KC], F32)
            fcolsrc = []
            for ci, cnd in enumerate((3, 2)):
                for i, ty in enumerate((2, 4, 3, 5)):
                    fcolsrc.append((4 * ci + i, modsD[l, cnd, :, ty * KC:(ty + 1) * KC]))
            for i in range(4):
                fcolsrc.append((8 + i, lncols[:, i * KC:(i + 1) * KC]))
            fio = dict(xT=xsh, oT=OSH, x2T=None, cols=None, r_scr=r_scr, x1_scr=x1_scr, ident_in=ident_in)
            last = (l == NLAY - 1)
            build_ffn(ffn_tiles, NE=NE, fz=FZ(nc, S, pre + "f_", io=fio, colsrc=fcolsrc, x2outs=[xsh] + ([x_out] if last else [])))
            if not last:
                for i in range(2):
                    S.cc("AllGather", ALU.bypass, xsh[i * DH:(i + 1) * DH, :], XGs[i])
        S.finish()
    return nc


_PROGS = {}


def _prog(name, fn):
    if name not in _PROGS:
        _PROGS[name] = fn()
    return _PROGS[name]


def _run(nc, in_maps):
    res = run_bass_kernel_spmd(nc, in_maps, core_ids=list(range(8)))
    return res.results


def _colset(v, KC):
    return np.ascontiguousarray(v.reshape(KC, 128).T)


_f32 = lambda a: np.ascontiguousarray(np.asarray(a, dtype=np.float32))


def _mixer_weights(l, k, p, ROWS):
    D = CFG.D
    j = l // 2
    if l % 2 == 0:
        wq = p["na_w_qkv"][j]
        wsel = np.ascontiguousarray(np.concatenate([wq[:, t * D + k * 512:t * D + (k + 1) * 512] for t in range(3)], axis=1))
        btab = na_bias_tables(_f32(p["na_rpb"][j][4 * k:4 * k + 4]), ROWS)
        return dict(wqkv=wsel, bias=np.ascontiguousarray(btab.reshape(4, 128, -1)))
    wi = p["gla_w_in"][j]
    win = np.ascontiguousarray(np.concatenate([wi[:, k * 256:(k + 1) * 256], wi[:, 2048 + k * 256:2048 + (k + 1) * 256],
                                               wi[:, 4096 + k * 512:4096 + (k + 1) * 512], wi[:, 8192 + k * 512:8192 + (k + 1) * 512]], axis=1))
    wa1 = np.ascontiguousarray(np.concatenate([p["gla_w_a1"][j][0], p["gla_w_a1"][j][1]], axis=1))
    w_a2, b_a = p["gla_w_a2"][j], p["gla_b_a"][j]
    wa2p = np.zeros((32, 2, 256), np.float32)
    wa2p[0:16, 0] = w_a2[0][:, k * 256:(k + 1) * 256]
    wa2p[16:32, 1] = w_a2[1][:, k * 256:(k + 1) * 256]
    ba = np.ascontiguousarray(np.stack([b_a[d][k * 256 + ch * 128:k * 256 + (ch + 1) * 128] for d in range(2) for ch in range(2)], axis=1))
    nwt = np.ascontiguousarray(p["gla_norm"][j].reshape(4, 128).T)
    return dict(win=win, wa1=wa1, wa2p=wa2p, ba=ba, nw=nwt)


def _ffn_weights(l, p, NE):
    D = CFG.D
    w_o = p["na_w_o"][l // 2] if l % 2 == 0 else p["gla_w_o"][l // 2]
    return dict(wo=w_o, wr=np.ascontiguousarray(p["moe_router"][l][:, :NE]), eb=np.ascontiguousarray(p["moe_bias"][l][:NE].reshape(1, NE)),
                wg=p["moe_w_gate"][l][:NE], wu=p["moe_w_up"][l][:NE], wd=np.ascontiguousarray(p["moe_w_down"][l][:NE]).reshape(NE * CFG.HD, D),
                sg=p["sh_w_gate"][l], su=p["sh_w_up"][l], sd=p["sh_w_down"][l])


def _lncols(l, p):
    KC = CFG.D // 128
    return np.ascontiguousarray(np.concatenate([_colset(p["ln_g"][l, 0], KC), _colset(p["ln_b"][l, 0], KC),
                                                _colset(p["ln_g"][l, 1], KC), _colset(p["ln_b"][l, 1], KC)], axis=1))


def _stage_x(p):
    x, ctx = p["x"], p["ctx"]
    B, L, D = x.shape
    CT = ctx.shape[1]
    xT = np.empty((B, D, L + CT), np.float32)
    for b in range(B):
        xT[b, :, :L] = x[b].T
        xT[b, :, L:] = ctx[b].T
    return xT


def kernel_fused(p, NLAY=CFG.DEPTH, NE=CFG.NE):
    D, KC = CFG.D, CFG.D // 128
    B, L, _ = p["x"].shape
    CT = p["ctx"].shape[1]
    ROWS = L // CFG.GRID_W
    LS, CS = L // 4, CT // 4
    xT = _stage_x(p)
    eye = np.eye(128, dtype=np.float32)
    cos, sin, perm, msk, rmask = gla_consts(ROWS)
    bT = np.ascontiguousarray(p["ada_b"][:NLAY].reshape(NLAY, 6 * KC, 128).transpose(0, 2, 1))
    w1, w2 = np.ascontiguousarray(p["ada_w1"][:NLAY]), np.ascontiguousarray(p["ada_w2"][:NLAY])
    in_maps = []
    for k in range(8):
        b, q = k // 4, k % 4
        conds = np.stack([p["c"][0], p["c"][1], p["c_ctx"], p["c"][b]], axis=0)
        m = dict(xs=np.ascontiguousarray(np.concatenate([xT[b][:, q * LS:(q + 1) * LS], xT[b][:, L + q * CS:L + (q + 1) * CS]], axis=1)),
                 mask8=np.ascontiguousarray(np.broadcast_to((np.arange(8) == k).astype(np.float32)[None, :], (128, 8))),
                 ident_in=eye, cos=cos, sin=sin, perm_in=perm, msk_in=msk, rmask_in=rmask,
                 A_condT=np.ascontiguousarray(conds.reshape(4, KC, 128).transpose(2, 1, 0)), A_w1=w1, A_w2=w2, A_bT=bT)
        for l in range(NLAY):
            for n, v in _mixer_weights(l, k, p, ROWS).items():
                m["L%d_m_%s" % (l, n)] = v
            for n, v in _ffn_weights(l, p, NE).items():
                m["L%d_f_%s" % (l, n)] = v
            m["L%d_lncols" % l] = _lncols(l, p)
        in_maps.append(m)
    outs = _run(_prog(("fused", L, NLAY, NE), lambda: build_fused(L, NLAY, NE)), in_maps)
    out = np.empty((B, L, D), np.float32)
    for k in range(8):
        b, q = k // 4, k % 4
        out[b, q * LS:(q + 1) * LS, :] = outs[k]["x_out"][:, :LS].T
    return out


def kernel_unfused(p, NLAY=CFG.DEPTH, NE=CFG.NE):
    D, KC = CFG.D, CFG.D // 128
    B, L, _ = p["x"].shape
    CT = p["ctx"].shape[1]
    ROWS = L // CFG.GRID_W
    LS, CS = L // 4, CT // 4
    eye = np.eye(128, dtype=np.float32)
    conds = np.stack([p["c"][0], p["c"][1], p["c_ctx"]], axis=0)
    condT = np.ascontiguousarray(conds.reshape(3, KC, 128).transpose(2, 1, 0))
    bT = np.ascontiguousarray(p["ada_b"][:NLAY].reshape(NLAY, 6 * KC, 128).transpose(0, 2, 1))
    ada_in = dict(condT=condT, w1=np.ascontiguousarray(p["ada_w1"][:NLAY]), w2=np.ascontiguousarray(p["ada_w2"][:NLAY]), bT=bT)
    mods = _run(_prog(("ada", NLAY), lambda: build_ada(NL=NLAY)), [ada_in] * 8)[0]["mods"]
    xT = _stage_x(p)
    pw = _run_prep(p, NLAY, NE)
    ffn_tiles = [(t0, min(512, LS - t0), 0) for t0 in range(0, LS, 512)] + [(LS, CS, 1)]
    cos, sin, perm, msk, rmask = gla_consts(ROWS)
    for l in range(NLAY):
        mcols = np.zeros((128, 6 * KC), np.float32)
        for cnd in range(3):
            mcols[:, (2 * cnd) * KC:(2 * cnd + 1) * KC] = mods[l][cnd][:, 0:KC]
            mcols[:, (2 * cnd + 1) * KC:(2 * cnd + 2) * KC] = mods[l][cnd][:, KC:2 * KC]
        in_maps = []
        for k in range(8):
            m = dict(xT=xT, cols=mcols)
            m.update(_mixer_weights(l, k, p, ROWS))
            if l % 2 == 1:
                m.update(cos=cos, sin=sin, perm_in=perm, ident_in=eye, msk_in=msk, rmask_in=rmask)
            in_maps.append(m)
        if l % 2 == 0:
            outs = _run(_prog(("na", ROWS), lambda: build_na(ROWS=ROWS)), in_maps)
        else:
            outs = _run(_prog(("gla", ROWS), lambda: build_gla(ROWS=ROWS)), in_maps)
        oT = np.concatenate([o["oT"] for o in outs], axis=1)
        del outs, in_maps
        fw = dict(wr=np.ascontiguousarray(p["moe_router"][l][:, :NE]), eb=np.ascontiguousarray(p["moe_bias"][l][:NE].reshape(1, NE)))
        fw.update(pw[l])
        lnc = _lncols(l, p)
        in_maps = []
        for k in range(8):
            b, q = k // 4, k % 4
            sl = lambda a: np.ascontiguousarray(np.concatenate([a[b][:, q * LS:(q + 1) * LS], a[b][:, L + q * CS:L + (q + 1) * CS]], axis=1))
            fc = np.zeros((128, 12 * KC), np.float32)
            for ci, cnd in enumerate((b, 2)):
                for i, ty in enumerate((2, 4, 3, 5)):
                    fc[:, (4 * ci + i) * KC:(4 * ci + i + 1) * KC] = mods[l][cnd][:, ty * KC:(ty + 1) * KC]
            fc[:, 8 * KC:12 * KC] = lnc
            m = dict(xT=sl(xT), oT=sl(oT), cols=fc, ident_in=eye)
            m.update(fw)
            in_maps.append(m)
        outs = _run(_prog(("ffn", LS, NE), lambda: build_ffn(ffn_tiles, NE=NE, prep=True)), in_maps)
        del in_maps
        for k in range(8):
            b, q = k // 4, k % 4
            x2 = outs[k]["x2T"]
            xT[b, :, q * LS:(q + 1) * LS] = x2[:, :LS]
            xT[b, :, L + q * CS:L + (q + 1) * CS] = x2[:, LS:]
        del outs
    return np.ascontiguousarray(xT[:, :, :L].transpose(0, 2, 1))


def kernel(x, c, ctx, c_ctx, ada_w1, ada_w2, ada_b, ln_g, ln_b, na_w_qkv, na_w_o, na_rpb,
           gla_w_in, gla_w_a1, gla_w_a2, gla_b_a, gla_norm, gla_w_o, moe_router, moe_bias,
           moe_w_gate, moe_w_up, moe_w_down, sh_w_gate, sh_w_up, sh_w_down):
    p = dict(x=x, c=c, ctx=ctx, c_ctx=c_ctx, ada_w1=ada_w1, ada_w2=ada_w2, ada_b=ada_b, ln_g=ln_g, ln_b=ln_b, na_w_qkv=na_w_qkv,
             na_w_o=na_w_o, na_rpb=na_rpb, gla_w_in=gla_w_in, gla_w_a1=gla_w_a1, gla_w_a2=gla_w_a2, gla_b_a=gla_b_a, gla_norm=gla_norm,
             gla_w_o=gla_w_o, moe_router=moe_router, moe_bias=moe_bias, moe_w_gate=moe_w_gate, moe_w_up=moe_w_up, moe_w_down=moe_w_down,
             sh_w_gate=sh_w_gate, sh_w_up=sh_w_up, sh_w_down=sh_w_down)
    p = {k: np.asarray(v, dtype=np.float32) for k, v in p.items()}
    return kernel_unfused(p)
```

```python
import numpy as np
import ml_dtypes
from contextlib import ExitStack
import concourse.bass as bass
import concourse.mybir as mybir
from concourse.bass_utils import run_bass_kernel_spmd

F32 = mybir.dt.float32
BF16 = mybir.dt.bfloat16
AF = mybir.ActivationFunctionType
ALU = mybir.AluOpType
AX = mybir.AxisListType
NPBF = ml_dtypes.bfloat16


class CFG:
    D = 4096
    B = 2
    SEQ = 8192
    DEPTH = 4
    GRID_W = 64
    CTX = 256
    NA_H = 32
    NA_KH = 8
    NA_KW = 16
    GLA_H = 8
    GLA_RANK = 16
    GLA_L = 64
    NE = 64
    HD = 192
    TOPK = 8
    NG = 8
    TOPG = 4
    RSCALE = 2.5
    ADA_R = 256
    EPS = 1e-6


ALPHA = (2 * CFG.DEPTH) ** 0.25


class Res:
    __slots__ = ("w", "rs", "name", "sem", "dcnt", "key")
    _n = 0

    def __init__(self, name=""):
        self.w = None
        self.rs = {}
        self.name = name
        self.sem = None
        self.dcnt = 0
        Res._n += 1
        self.key = ("r", Res._n)


class Sched:
    def __init__(self, nc, es):
        self.nc = nc
        self.es = es
        self.eng = {"pe": nc.tensor, "act": nc.scalar, "dve": nc.vector, "pool": nc.gpsimd, "sp": nc.sync}
        self.sem = {}
        self.cnt = {}
        for k in ("pe", "act", "dve", "pool"):
            self.sem[k] = es.enter_context(nc.semaphore("s_" + k))
            self.cnt[k] = 0
        self.waited = {k: {} for k in self.eng}
        self.dres = []
        self.nsem = 4
        self.tes = es
        self.prefix = ""
        self.sempool = []

    def _need(self, E, dep):
        key, val = dep
        if isinstance(key, str):
            if key == E and val > self.cnt[E]:
                return
            semh = self.sem[key]
        else:
            r = key
            val = r.dcnt
            semh = r.sem
            key = r.key
        if self.waited[E].get(key, 0) >= val:
            return
        self.eng[E].wait_ge(semh, val)
        self.waited[E][key] = val

    def _deps(self, E, reads, writes):
        for r in reads:
            if r.w is not None:
                self._need(E, r.w)
        for w in writes:
            if w.w is not None:
                self._need(E, w.w)
            for k, v in w.rs.items():
                self._need(E, (k, v))

    def _tag(self, tag, reads, writes):
        k, v = tag
        for r in reads:
            if r.rs.get(k, 0) < v:
                r.rs[k] = v
        for w in writes:
            w.w = tag
            w.rs = {}

    def op(self, E, fn, reads=(), writes=(), inc=True):
        self._deps(E, reads, writes)
        ins = fn(self.eng[E])
        if inc:
            self.cnt[E] += 1
            ins.then_inc(self.sem[E], 1)
            tag = (E, self.cnt[E])
        else:
            tag = (E, self.cnt[E] + 1)
        self._tag(tag, reads, writes)
        return ins

    def dma(self, Q, out, in_, reads=(), writes=(), prim=None):
        self._deps(Q, reads, writes)
        p = prim if prim is not None else (writes[0] if writes else reads[0])
        self._getsem(p)
        ins = self.eng[Q].dma_start(out=out, in_=in_)
        p.dcnt += 16
        ins.then_inc(p.sem, 16)
        self._tag((p, p.dcnt), reads, writes)
        return ins

    def _getsem(self, p):
        if p.sem is None:
            if self.sempool:
                p.sem, p.dcnt = self.sempool.pop()
            else:
                p.sem = self.es.enter_context(self.nc.semaphore("d%d" % self.nsem))
                self.nsem += 1
                assert self.nsem < 95, "too many semaphores"
            self.dres.append(p)

    def phase_end(self):
        self.barrier()
        for r in self.dres:
            self.sempool.append((r.sem, r.dcnt))
            r.sem = None
        self.dres = []

    def cc(self, kind, op, in_ap, out_ap):
        self.barrier()
        r = Res("cc")
        self._getsem(r)
        ins = self.nc.gpsimd.collective_compute(kind, op, replica_groups=[list(range(8))], ins=[in_ap.opt()], outs=[out_ap.opt()])
        ins.then_inc(r.sem)
        r.dcnt += 1
        self.barrier()

    def barrier(self):
        for E in ("pe", "act", "dve", "pool", "sp"):
            for k in ("pe", "act", "dve", "pool"):
                if k != E and self.cnt[k] > 0:
                    self._need(E, (k, self.cnt[k]))
            for r in self.dres:
                self._need(E, (r, r.dcnt))

    def finish(self):
        for k in ("pe", "act", "dve", "pool"):
            if self.cnt[k] > 0:
                self._need("sp", (k, self.cnt[k]))
        for r in self.dres:
            self._need("sp", (r, r.dcnt))


class FZ:
    def __init__(self, nc, S, pre, io=None, **hooks):
        self.nc, self.S, self.pre, self.io = nc, S, pre, (io or {})
        self.__dict__.update(hooks)


def _mk_dr(nc, fz):
    def dr(n, s, d, k="ExternalInput"):
        if fz is not None and n in fz.io:
            return fz.io[n]
        name = (fz.pre if fz is not None else "") + n
        return nc.dram_tensor(name, s, d, kind=k).ap()
    return dr


class T:
    def __init__(self, S, shape, dt, name, psum=False):
        nc = S.nc
        name = S.prefix + name
        self.t = S.tes.enter_context(nc.psum_tensor(name, shape, dt) if psum else nc.sbuf_tensor(name, shape, dt))
        self.r = Res(name)

    def __getitem__(self, k):
        return self.t[k]


class Rot:
    def __init__(self, tiles):
        self.tiles = tiles
        self.i = 0

    def next(self):
        t = self.tiles[self.i % len(self.tiles)]
        self.i += 1
        return t


def build_ffn(tiles, D=CFG.D, NE=CFG.NE, HD=CFG.HD, fz=None, prep=False):
    KC = D // 128
    Ttot = sum(t[1] for t in tiles)
    G = NE // CFG.NG
    NPAIR = NE // 2
    NCH = 3 * NPAIR + 2
    NTM = 512
    nc = fz.nc if fz else bass.Bass("TRN2", target_bir_lowering=False)
    dr = _mk_dr(nc, fz)
    xT = dr("xT", [D, Ttot], F32)
    oT = dr("oT", [D, Ttot], BF16)
    cols = dr("cols", [128, 12 * KC], F32)
    wr = dr("wr", [D, NE], F32)
    eb = dr("eb", [1, NE], F32)
    KHp = max(1, KC // 2)
    NKH = (KC + KHp - 1) // KHp
    if prep:
        wgu = dr("wgu", [NPAIR, 2, NKH, 128, KHp * 2 * HD], BF16)
        wsh = dr("wsh", [2, NKH, 128, KHp * HD], BF16)
        wdp = dr("wdp", [KC, 2, 128, ((NCH + 1) // 2) * 128], BF16)
        wop = dr("wop", [KC, 128, KC * 128], BF16)
        wo = wg = wu = wd = sg = su = sd = None
    else:
        wo = dr("wo", [D, D], F32)
        wg = dr("wg", [NE, D, HD], F32)
        wu = dr("wu", [NE, D, HD], F32)
        wd = dr("wd", [NE * HD, D], F32)
        sg = dr("sg", [D, HD], F32)
        su = dr("su", [D, HD], F32)
        sd = dr("sd", [HD, D], F32)
    ident_in = dr("ident_in", [128, 128], F32)
    x2T = dr("x2T", [D, Ttot], F32, "ExternalOutput")
    r_scr = dr("r_scr", [D, NTM], F32, "Internal")
    x1_scr = dr("x1_scr", [D, NTM], F32, "Internal")

    with ExitStack() as es:
        S = fz.S if fz else Sched(nc, es)
        S.tes = es
        S.prefix = fz.pre if fz else ""
        x2outs = fz.x2outs if fz else [x2T]
        hT = T(S, [128, KC, NTM], BF16, "hT")
        hid = T(S, [128, NCH, NTM], BF16, "hid")
        hid_rs = [Res("hid%d" % i) for i in range(NCH)]
        USZ = max(16 * 384, (NCH + 1) // 2 * 128, KC * 128)
        arena = Rot([T(S, [128, USZ], BF16, "ar%d" % i) for i in range(3)])
        f32t = Rot([T(S, [128, NTM], F32, "f%d" % i) for i in range(8)])
        mean = T(S, [128, NTM], F32, "mean")
        rstd = T(S, [128, NTM], F32, "rstd")
        colt = T(S, [128, 12 * KC], F32, "colt")
        ident = T(S, [128, 128], BF16, "ident")
        ones32 = T(S, [128, 128], F32, "ones32")
        wrt = T(S, [128, KC, NE], BF16, "wrt")
        ebt = T(S, [128, NE], F32, "ebt")
        gT = T(S, [64, NTM], BF16, "gT")
        sm = Rot([T(S, [128, NE], F32, "sm%d" % i) for i in range(6)])
        sm8 = Rot([T(S, [128, 8], F32, "sm8_%d" % i) for i in range(6)])
        gbf = T(S, [128, NE], BF16, "gbf")
        pb = [T(S, [128, 512], F32, "pb%d" % i, psum=True) for i in range(7)]
        pbt = T(S, [128, 1024], BF16, "pbt", psum=True)

        if fz:
            for (blk, src) in fz.colsrc:
                S.dma("sp", colt[:, blk * KC:(blk + 1) * KC], src, writes=[colt.r])
        else:
            S.dma("sp", colt[:], cols, writes=[colt.r])
        S.dma("pool", ident[:], ident_in, writes=[ident.r])
        S.op("dve", lambda e: e.memset(ones32[:], 1.0), writes=[ones32.r])
        S.dma("pool", wrt[:], wr.rearrange("(kc p) n -> p kc n", p=128), writes=[wrt.r])
        S.dma("sp", ebt[:], eb.partition_broadcast(128), writes=[ebt.r])
        for c in (1, 5):
            S.op("dve", lambda e, c=c: e.tensor_scalar_add(colt[:, c * KC:(c + 1) * KC], colt[:, c * KC:(c + 1) * KC], 1.0),
                 reads=[colt.r], writes=[colt.r])
        col = lambda idx, kc: colt[:, idx * KC + kc: idx * KC + kc + 1]

        def ln_stats(nt, s1, s2):
            m_ = mean[:, :nt]
            r_ = rstd[:, :nt]
            S.op("dve", lambda e: e.tensor_scalar_mul(m_, s1[:, :nt], 1.0 / D), reads=[s1.r], writes=[mean.r])
            tmp = f32t.next()
            S.op("dve", lambda e: e.tensor_tensor(out=tmp[:, :nt], in0=m_, in1=m_, op=ALU.mult), reads=[mean.r], writes=[tmp.r])
            S.op("dve", lambda e: e.scalar_tensor_tensor(out=tmp[:, :nt], in0=s2[:, :nt], scalar=1.0 / D, in1=tmp[:, :nt],
                                                         op0=ALU.mult, op1=ALU.subtract), reads=[s2.r, tmp.r], writes=[tmp.r])
            S.op("dve", lambda e: e.tensor_scalar_add(tmp[:, :nt], tmp[:, :nt], float(CFG.EPS)), reads=[tmp.r], writes=[tmp.r])
            S.op("act", lambda e: e.activation(out=tmp[:, :nt], in_=tmp[:, :nt], func=AF.Sqrt), reads=[tmp.r], writes=[tmp.r])
            S.op("dve", lambda e: e.reciprocal(out=r_, in_=tmp[:, :nt]), reads=[tmp.r], writes=[rstd.r])

        def stats_acc(rc, nt, mc, s1, s2):
            S.op("pe", lambda e: e.matmul(s1[:, :nt], ones32[:], rc[:, :nt], start=(mc == 0), stop=(mc == KC - 1)),
                 reads=[ones32.r, rc.r], writes=[s1.r], inc=(mc == KC - 1))
            sq = f32t.next()
            S.op("act", lambda e: e.activation(out=sq[:, :nt], in_=rc[:, :nt], func=AF.Square), reads=[rc.r], writes=[sq.r])
            S.op("pe", lambda e: e.matmul(s2[:, :nt], ones32[:], sq[:, :nt], start=(mc == 0), stop=(mc == KC - 1)),
                 reads=[ones32.r, sq.r], writes=[s2.r], inc=True)

        for (t0, nt, cond) in tiles:
            cb = 4 * cond
            oTt = hid.t[:, 0:KC, :]
            S.dma("sp", oTt[:, :, :nt], oT.rearrange("(kc p) t -> p kc t", p=128)[:, :, t0:t0 + nt], writes=hid_rs[:KC])
            s1, s2 = pb[2], pb[3]
            for mc in range(KC):
                u = arena.next()
                uv = u.t[:, 0:KC * 128].rearrange("p (kc n) -> p kc n", n=128)
                if prep:
                    S.dma("pool", u.t[:, 0:KC * 128], wop[mc], writes=[u.r])
                else:
                    S.dma("pool", uv, wo.rearrange("(kc p) n -> p kc n", p=128)[:, :, mc * 128:(mc + 1) * 128], writes=[u.r])
                yp = pb[mc % 2]
                for kc in range(KC):
                    S.op("pe", lambda e: e.matmul(yp[:, :nt], uv[:, kc, :], oTt[:, kc, :nt], start=(kc == 0), stop=(kc == KC - 1)),
                         reads=[u.r] + hid_rs[:KC], writes=[yp.r], inc=(kc == KC - 1))
                xc = f32t.next()
                S.dma("sp", xc[:, :nt], xT[mc * 128:(mc + 1) * 128, t0:t0 + nt], writes=[xc.r])
                S.op("act", lambda e: e.activation(out=xc[:, :nt], in_=xc[:, :nt], func=AF.Copy, scale=float(ALPHA)), reads=[xc.r], writes=[xc.r])
                rc = f32t.next()
                S.op("dve", lambda e: e.scalar_tensor_tensor(out=rc[:, :nt], in0=yp[:, :nt], scalar=col(cb + 0, mc), in1=xc[:, :nt],
                                                             op0=ALU.mult, op1=ALU.add), reads=[yp.r, xc.r, colt.r], writes=[rc.r])
                stats_acc(rc, nt, mc, s1, s2)
                S.dma("sp", r_scr[mc * 128:(mc + 1) * 128, 0:nt], rc[:, :nt], reads=[rc.r], prim=rc.r)
            ln_stats(nt, s1, s2)
            S.barrier()
            for mc in range(KC):
                rc = f32t.next()
                S.dma("sp", rc[:, :nt], r_scr[mc * 128:(mc + 1) * 128, 0:nt], writes=[rc.r])
                S.op("dve", lambda e: e.tensor_tensor(out=rc[:, :nt], in0=rc[:, :nt], in1=mean[:, :nt], op=ALU.subtract),
                     reads=[rc.r, mean.r], writes=[rc.r])
                S.op("pool", lambda e: e.tensor_tensor(out=rc[:, :nt], in0=rc[:, :nt], in1=rstd[:, :nt], op=ALU.mult),
                     reads=[rc.r, rstd.r], writes=[rc.r])
                x1c = f32t.next()
                S.op("act", lambda e: e.activation(out=x1c[:, :nt], in_=rc[:, :nt], func=AF.Identity, scale=col(8, mc), bias=col(9, mc)),
                     reads=[rc.r, colt.r], writes=[x1c.r])
                S.dma("sp", x1_scr[mc * 128:(mc + 1) * 128, 0:nt], x1c[:, :nt], reads=[x1c.r], prim=x1c.r)
                S.op("dve", lambda e: e.tensor_scalar(out=hT[:, mc, :nt], in0=x1c[:, :nt], scalar1=col(cb + 1, mc), scalar2=col(cb + 2, mc),
                                                      op0=ALU.mult, op1=ALU.add), reads=[x1c.r, colt.r], writes=[hT.r])
            S.barrier()
            ntc = (nt + 127) // 128
            for tc in range(ntc):
                tn = min(128, nt - tc * 128)
                lp = pb[4 + tc % 2]
                for kc in range(KC):
                    S.op("pe", lambda e: e.matmul(lp[:tn, :NE], hT[:, kc, tc * 128:tc * 128 + tn], wrt[:, kc, :], start=(kc == 0), stop=(kc == KC - 1)),
                         reads=[hT.r, wrt.r], writes=[lp.r], inc=(kc == KC - 1))
                sc = sm.next()
                S.op("act", lambda e: e.activation(out=sc[:tn, :], in_=lp[:tn, :NE], func=AF.Sigmoid), reads=[lp.r], writes=[sc.r])
                bi = sm.next()
                S.op("dve", lambda e: e.tensor_tensor(out=bi[:tn, :], in0=sc[:tn, :], in1=ebt[:tn, :], op=ALU.add), reads=[sc.r, ebt.r], writes=[bi.r])
                bi3 = bi[:tn, :].rearrange("p (g j) -> p g j", j=G)
                m1 = sm8.next()
                S.op("dve", lambda e: e.tensor_reduce(out=m1[:tn, :], in_=bi3, axis=AX.X, op=ALU.max), reads=[bi.r], writes=[m1.r])
                eq = sm.next()
                eq3 = eq[:tn, :].rearrange("p (g j) -> p g j", j=G)
                S.op("dve", lambda e: e.tensor_tensor(out=eq3, in0=bi3, in1=m1[:tn, :].unsqueeze(2).broadcast_to([tn, CFG.NG, G]), op=ALU.is_equal),
                     reads=[bi.r, m1.r], writes=[eq.r])
                S.op("dve", lambda e: e.scalar_tensor_tensor(out=eq[:tn, :], in0=eq[:tn, :], scalar=-100.0, in1=bi[:tn, :], op0=ALU.mult, op1=ALU.add),
                     reads=[eq.r, bi.r], writes=[eq.r])
                m2 = sm8.next()
                S.op("dve", lambda e: e.tensor_reduce(out=m2[:tn, :], in_=eq3, axis=AX.X, op=ALU.max), reads=[eq.r], writes=[m2.r])
                S.op("dve", lambda e: e.tensor_tensor(out=m2[:tn, :], in0=m2[:tn, :], in1=m1[:tn, :], op=ALU.add), reads=[m1.r, m2.r], writes=[m2.r])
                t8 = sm8.next()
                S.op("dve", lambda e: e.max(out=t8[:tn, :], in_=m2[:tn, :]), reads=[m2.r], writes=[t8.r])
                gm_ = sm8.next()
                S.op("dve", lambda e: e.tensor_scalar(out=gm_[:tn, :], in0=m2[:tn, :], scalar1=t8[:tn, CFG.TOPG - 1:CFG.TOPG], scalar2=None, op0=ALU.is_ge),
                     reads=[m2.r, t8.r], writes=[gm_.r])
                mk = sm.next()
                mk3 = mk[:tn, :].rearrange("p (g j) -> p g j", j=G)
                gmb = gm_[:tn, :].unsqueeze(2).broadcast_to([tn, CFG.NG, G])
                S.op("dve", lambda e: e.tensor_tensor(out=mk3, in0=bi3, in1=gmb, op=ALU.mult), reads=[bi.r, gm_.r], writes=[mk.r])
                pen = sm8.next()
                S.op("dve", lambda e: e.tensor_scalar(out=pen[:tn, :], in0=gm_[:tn, :], scalar1=100.0, scalar2=-100.0, op0=ALU.mult, op1=ALU.add),
                     reads=[gm_.r], writes=[pen.r])
                S.op("dve", lambda e: e.tensor_tensor(out=mk3, in0=mk3, in1=pen[:tn, :].unsqueeze(2).broadcast_to([tn, CFG.NG, G]), op=ALU.add),
                     reads=[mk.r, pen.r], writes=[mk.r])
                t8b = sm8.next()
                S.op("dve", lambda e: e.max(out=t8b[:tn, :], in_=mk[:tn, :]), reads=[mk.r], writes=[t8b.r])
                sel = sm.next()
                S.op("dve", lambda e: e.tensor_scalar(out=sel[:tn, :], in0=mk[:tn, :], scalar1=t8b[:tn, CFG.TOPK - 1:CFG.TOPK], scalar2=None, op0=ALU.is_ge),
                     reads=[mk.r, t8b.r], writes=[sel.r])
                S.op("dve", lambda e: e.tensor_tensor(out=sel[:tn, :], in0=sel[:tn, :], in1=sc[:tn, :], op=ALU.mult), reads=[sel.r, sc.r], writes=[sel.r])
                ws = sm8.next()
                S.op("dve", lambda e: e.tensor_reduce(out=ws[:tn, 0:1], in_=sel[:tn, :], axis=AX.X, op=ALU.add), reads=[sel.r], writes=[ws.r])
                S.op("dve", lambda e: e.reciprocal(out=ws[:tn, 1:2], in_=ws[:tn, 0:1]), reads=[ws.r], writes=[ws.r])
                S.op("dve", lambda e: e.tensor_scalar(out=gbf[:tn, :], in0=sel[:tn, :], scalar1=ws[:tn, 1:2], scalar2=float(CFG.RSCALE), op0=ALU.mult, op1=ALU.mult),
                     reads=[sel.r, ws.r], writes=[gbf.r])
                S.op("pe", lambda e: e.transpose(out=pbt[:NE, :tn], in_=gbf[:tn, :], identity=ident[:tn, :tn]), reads=[gbf.r, ident.r], writes=[pbt.r])
                S.op("dve", lambda e: e.tensor_copy(out=gT[:NE, tc * 128:tc * 128 + tn], in_=pbt[:NE, :tn]), reads=[pbt.r], writes=[gT.r])
            for p in range(NPAIR + 1):
                shared = (p == NPAIR)
                nm = 2 if shared else 3
                gps = [pb[0], pb[1], pb[2]]
                ups = [pb[3], pb[4], pb[5]]
                for which, wsrc, ssrc, pss in ((0, wg, sg, gps), (1, wu, su, ups)):
                    KH = max(1, KC // 2)
                    for k0 in range(0, KC, KH):
                        u = arena.next()
                        nk = min(KH, KC - k0)
                        ncol = HD if shared else 2 * HD
                        uv = u.t[:, 0:nk * ncol].rearrange("p (kc n) -> p kc n", n=ncol)
                        if prep:
                            S.dma("pool", u.t[:, 0:nk * ncol], (wsh[which, k0 // KH] if shared else wgu[p, which, k0 // KH])[:, 0:nk * ncol], writes=[u.r])
                        elif shared:
                            S.dma("pool", uv, ssrc.rearrange("(kc p) n -> p kc n", p=128)[:, k0:k0 + nk, :], writes=[u.r])
                        else:
                            for j in range(2):
                                S.dma("pool", uv[:, :, j * HD:(j + 1) * HD], wsrc[2 * p + j].rearrange("(kc p) n -> p kc n", p=128)[:, k0:k0 + nk, :], writes=[u.r])
                        for m in range(nm):
                            mw = min(128, ncol - m * 128)
                            for kk in range(nk):
                                kc = k0 + kk
                                S.op("pe", lambda e: e.matmul(pss[m][:mw, :nt], uv[:, kk, m * 128:m * 128 + mw], hT[:, kc, :nt], start=(kc == 0), stop=(kc == KC - 1)),
                                     reads=[u.r, hT.r], writes=[pss[m].r], inc=(kk == nk - 1))
                gbs = []
                if not shared:
                    for j in range(2):
                        gb = pb[6]
                        e_ = 2 * p + j
                        selT = ident[0:NE, e_:e_ + 1].broadcast_to([NE, 128])
                        S.op("pe", lambda e: e.matmul(gb[:, :nt], selT, gT[0:NE, :nt], start=True, stop=True), reads=[ident.r, gT.r], writes=[gb.r])
                        gs_ = f32t.next()
                        S.op("act", lambda e: e.activation(out=gs_[:, :nt], in_=gb[:, :nt], func=AF.Copy), reads=[gb.r], writes=[gs_.r])
                        gbs.append(gs_)
                for m in range(nm):
                    c = 3 * p + m
                    mw = 128 if (not shared or m == 0) else HD - 128
                    sgl = f32t.next()
                    S.op("act", lambda e: e.activation(out=sgl[:mw, :nt], in_=gps[m][:mw, :nt], func=AF.Silu), reads=[gps[m].r], writes=[sgl.r])
                    if shared:
                        S.op("dve", lambda e: e.tensor_tensor(out=hid[:mw, c, :nt], in0=sgl[:mw, :nt], in1=ups[m][:mw, :nt], op=ALU.mult),
                             reads=[sgl.r, ups[m].r], writes=[hid_rs[c]])
                        continue
                    S.op("dve", lambda e: e.tensor_tensor(out=sgl[:, :nt], in0=sgl[:, :nt], in1=ups[m][:, :nt], op=ALU.mult),
                         reads=[sgl.r, ups[m].r], writes=[sgl.r])
                    if m == 1:
                        for j in range(2):
                            S.op("dve", lambda e: e.tensor_tensor(out=hid[64 * j:64 * j + 64, c, :nt], in0=sgl[64 * j:64 * j + 64, :nt],
                                                                   in1=gbs[j][64 * j:64 * j + 64, :nt], op=ALU.mult),
                                 reads=[sgl.r, gbs[j].r], writes=[hid_rs[c]])
                    else:
                        g_ = gbs[0 if m == 0 else 1]
                        S.op("dve", lambda e: e.tensor_tensor(out=hid[:, c, :nt], in0=sgl[:, :nt], in1=g_[:, :nt], op=ALU.mult),
                             reads=[sgl.r, g_.r], writes=[hid_rs[c]])
            s1, s2 = pb[2], pb[3]
            NR = 3 * NPAIR
            H1 = (NCH + 1) // 2
            for mc in range(KC):
                yp = pb[mc % 2]
                for half in range(2):
                    c0 = half * H1
                    c1 = min(NCH, c0 + H1)
                    u = arena.next()
                    uv = u.t[:, 0:(c1 - c0) * 128].rearrange("p (c n) -> p c n", n=128)
                    rc1 = min(c1, NR)
                    if prep:
                        S.dma("pool", u.t[:, 0:(c1 - c0) * 128], wdp[mc, half][:, 0:(c1 - c0) * 128], writes=[u.r])
                        rc1, c1s = c0, NR + 9
                    else:
                        c1s = c1
                    if rc1 > c0:
                        S.dma("pool", uv[:, 0:rc1 - c0, :], wd.rearrange("(c p) n -> p c n", p=128)[:, c0:rc1, mc * 128:(mc + 1) * 128], writes=[u.r])
                    if c1s > NR and not prep:
                        S.dma("pool", uv[:, NR - c0, :], sd[0:128, mc * 128:(mc + 1) * 128], writes=[u.r])
                        S.dma("pool", uv[0:HD - 128, NR + 1 - c0, :], sd[128:HD, mc * 128:(mc + 1) * 128], writes=[u.r])
                    for c in range(c0, c1):
                        kr = 128 if c != NCH - 1 else HD - 128
                        S.op("pe", lambda e: e.matmul(yp[:, :nt], uv[:kr, c - c0, :], hid[:kr, c, :nt], start=(c == 0), stop=(c == NCH - 1)),
                             reads=[u.r, hid_rs[c]], writes=[yp.r], inc=(c == c1 - 1))
                xc = f32t.next()
                S.dma("sp", xc[:, :nt], x1_scr[mc * 128:(mc + 1) * 128, 0:nt], writes=[xc.r])
                S.op("act", lambda e: e.activation(out=xc[:, :nt], in_=xc[:, :nt], func=AF.Copy, scale=float(ALPHA)), reads=[xc.r], writes=[xc.r])
                rc = f32t.next()
                S.op("dve", lambda e: e.scalar_tensor_tensor(out=rc[:, :nt], in0=yp[:, :nt], scalar=col(cb + 3, mc), in1=xc[:, :nt],
                                                             op0=ALU.mult, op1=ALU.add), reads=[yp.r, xc.r, colt.r], writes=[rc.r])
                stats_acc(rc, nt, mc, s1, s2)
                S.dma("sp", r_scr[mc * 128:(mc + 1) * 128, 0:nt], rc[:, :nt], reads=[rc.r], prim=rc.r)
            ln_stats(nt, s1, s2)
            S.barrier()
            for mc in range(KC):
                rc = f32t.next()
                S.dma("sp", rc[:, :nt], r_scr[mc * 128:(mc + 1) * 128, 0:nt], writes=[rc.r])
                S.op("dve", lambda e: e.tensor_tensor(out=rc[:, :nt], in0=rc[:, :nt], in1=mean[:, :nt], op=ALU.subtract),
                     reads=[rc.r, mean.r], writes=[rc.r])
                S.op("pool", lambda e: e.tensor_tensor(out=rc[:, :nt], in0=rc[:, :nt], in1=rstd[:, :nt], op=ALU.mult),
                     reads=[rc.r, rstd.r], writes=[rc.r])
                x2c = f32t.next()
                S.op("act", lambda e: e.activation(out=x2c[:, :nt], in_=rc[:, :nt], func=AF.Identity, scale=col(10, mc), bias=col(11, mc)),
                     reads=[rc.r, colt.r], writes=[x2c.r])
                for xo in x2outs:
                    S.dma("sp", xo[mc * 128:(mc + 1) * 128, t0:t0 + nt], x2c[:, :nt], reads=[x2c.r], prim=x2c.r)
            S.barrier()
        if fz:
            S.phase_end()
        else:
            S.finish()
    return nc


def na_geometry(ROWS, KH=CFG.NA_KH):
    geo, keys, reps = [], {}, []
    for rp in range(ROWS // 2):
        s0 = min(max(2 * rp - KH // 2, 0), ROWS - KH)
        s1 = min(max(2 * rp + 1 - KH // 2, 0), ROWS - KH)
        cl, ch = s0 // 2, (s1 + KH - 1) // 2
        key = (s0 - 2 * rp, s1 - 2 * rp, cl - rp, ch - cl)
        if key not in keys:
            keys[key] = len(reps)
            reps.append(rp)
        geo.append((cl, ch - cl + 1, keys[key]))
    return geo, reps


def na_bias_tables(rpb, ROWS, W=CFG.GRID_W, KH=CFG.NA_KH, KW=CFG.NA_KW, NEG=-30000.0):
    geo, reps = na_geometry(ROWS, KH)
    H = rpb.shape[0]
    out = np.full((H, 128, len(reps), 5, 128), NEG, np.float32)
    kk = np.arange(128)
    qq = np.arange(128)
    for v, rp in enumerate(reps):
        cl, nb, _ = geo[rp]
        qrow = 2 * rp + qq // W
        qcol = qq % W
        rs = np.clip(qrow - KH // 2, 0, ROWS - KH)
        cs = np.clip(qcol - KW // 2, 0, W - KW)
        for ci in range(nb):
            krow = 2 * (cl + ci) + kk // W
            kcol = kk % W
            ok = ((krow[:, None] >= rs[None, :]) & (krow[:, None] < rs[None, :] + KH)
                  & (kcol[:, None] >= cs[None, :]) & (kcol[:, None] < cs[None, :] + KW))
            dr = np.clip(krow[:, None] - qrow[None, :] + KH - 1, 0, 2 * KH - 2)
            dc = np.clip(kcol[:, None] - qcol[None, :], 1 - KW, KW - 1) + KW - 1
            g = rpb[:, dr, dc]
            out[:, :, v, ci, :] = np.where(ok[None], g, np.float32(NEG))
    return out


def build_na(D=CFG.D, NHC=4, ROWS=128, W=CFG.GRID_W, CTXN=CFG.CTX, B=CFG.B, debug=False, fz=None):
    KC = D // 128
    L = ROWS * W
    TT = L + CTXN
    NCHK = TT // 128
    LCH = L // 128
    NCC = CTXN // 128
    geo, reps = na_geometry(ROWS)
    NV = len(reps)
    scale = float(128 ** -0.5)
    nc = fz.nc if fz else bass.Bass("TRN2", target_bir_lowering=False)
    dr = _mk_dr(nc, fz)
    xT = dr("xT", [B, D, TT], F32)
    cols = dr("cols", [128, 6 * KC], F32)
    wqkv = dr("wqkv", [D, 3 * NHC * 128], F32)
    bias = dr("bias", [NHC, 128, NV * 5 * 128], F32)
    oT = dr("oT", [B, NHC * 128, TT], BF16, "ExternalOutput")
    qk_scr = dr("qk_scr", [B, 2 * NHC, 128, TT], BF16, "ExternalOutput" if debug else "Internal")
    v_scr = dr("v_scr", [B, NHC, 128, NCHK, 128], BF16, "ExternalOutput" if debug else "Internal")
    tiles = [(t0, min(512, L - t0), 0) for t0 in range(0, L, 512)] + [(L, CTXN, 1)]
    with ExitStack() as es:
        S = fz.S if fz else Sched(nc, es)
        S.tes = es
        S.prefix = fz.pre if fz else ""
        pb = [T(S, [128, 512], F32, "pb%d" % i, psum=True) for i in range(4)]
        st2 = [T(S, [128, 1024], F32, "st%d" % i, psum=True) for i in range(2)]
        colt = T(S, [128, 6 * KC], F32, "colt")
        ones_bf = T(S, [128, 128], BF16, "ones_bf")
        if fz:
            fz.setup(S)
            for (blk, src) in fz.colsrc:
                S.dma("sp", colt[:, blk * KC:(blk + 1) * KC], src, writes=[colt.r])
        else:
            S.dma("sp", colt[:], cols, writes=[colt.r])
        S.op("dve", lambda e: e.memset(ones_bf[:], 1.0), writes=[ones_bf.r])
        for c in range(3):
            S.op("dve", lambda e, c=c: e.tensor_scalar_add(colt[:, (2 * c + 1) * KC:(2 * c + 2) * KC], colt[:, (2 * c + 1) * KC:(2 * c + 2) * KC], 1.0),
                 reads=[colt.r], writes=[colt.r])
        col = lambda idx, kc: colt[:, idx * KC + kc: idx * KC + kc + 1]
        with ExitStack() as es1:
            S.tes = es1
            Wt = T(S, [128, KC, 3 * NHC * 128], BF16, "Wt")
            hT = T(S, [128, KC, 512], BF16, "hT")
            xcs = Rot([T(S, [128, 512], F32, "xc%d" % i) for i in range(4)])
            obs = Rot([T(S, [128, 512], BF16, "ob%d" % i) for i in range(4)])
            for j in range(3):
                S.dma("pool", Wt[:, :, j * NHC * 128:(j + 1) * NHC * 128],
                      wqkv.rearrange("(kc p) n -> p kc n", p=128)[:, :, j * NHC * 128:(j + 1) * NHC * 128], writes=[Wt.r])
            for b in range(B):
                for (t0, nt, isctx) in tiles:
                    cnd = 2 if isctx else b
                    for kc in range(KC):
                        xc = xcs.next()
                        if fz:
                            fz.xload(S, b, kc, t0, nt, isctx, xc)
                        else:
                            S.dma("sp", xc[:, :nt], xT[b, kc * 128:(kc + 1) * 128, t0:t0 + nt], writes=[xc.r])
                        S.op("dve", lambda e: e.tensor_scalar(out=hT[:, kc, :nt], in0=xc[:, :nt], scalar1=col(2 * cnd + 1, kc), scalar2=col(2 * cnd, kc),
                                                              op0=ALU.mult, op1=ALU.add), reads=[xc.r, colt.r], writes=[hT.r])
                    for j in range(2 * NHC):
                        ps = pb[j % 2]
                        for kc in range(KC):
                            S.op("pe", lambda e: e.matmul(ps[:, :nt], Wt[:, kc, j * 128:(j + 1) * 128], hT[:, kc, :nt], start=(kc == 0), stop=(kc == KC - 1)),
                                 reads=[Wt.r, hT.r], writes=[ps.r], inc=(kc == KC - 1))
                        ob = obs.next()
                        S.op("act", lambda e: e.activation(out=ob[:, :nt], in_=ps[:, :nt], func=AF.Copy), reads=[ps.r], writes=[ob.r])
                        S.dma("sp", qk_scr[b, j, :, t0:t0 + nt], ob[:, :nt], reads=[ob.r], prim=ob.r)
                    for tc in range(nt // 128):
                        ps = pb[2 + tc % 2]
                        for kc in range(KC):
                            S.op("pe", lambda e: e.matmul(ps[:, :NHC * 128], hT[:, kc, tc * 128:(tc + 1) * 128], Wt[:, kc, 2 * NHC * 128:3 * NHC * 128],
                                                          start=(kc == 0), stop=(kc == KC - 1)), reads=[Wt.r, hT.r], writes=[ps.r], inc=(kc == KC - 1))
                        ob = obs.next()
                        S.op("act", lambda e: e.activation(out=ob[:, :NHC * 128], in_=ps[:, :NHC * 128], func=AF.Copy), reads=[ps.r], writes=[ob.r])
                        chk = (t0 + tc * 128) // 128
                        S.dma("sp", v_scr[b, :, :, chk, :].rearrange("h p n -> p h n"), ob[:, :NHC * 128].rearrange("p (h n) -> p h n", n=128),
                              reads=[ob.r], prim=ob.r)
            S.barrier()
        with ExitStack() as es2:
            S.tes = es2
            qTs = Rot([T(S, [128, TT], BF16, "qT%d" % i) for i in range(2)])
            kTs = Rot([T(S, [128, TT], BF16, "kT%d" % i) for i in range(2)])
            Vs = Rot([T(S, [128, NCHK, 128], BF16, "V%d" % i) for i in range(2)])
            oTs = Rot([T(S, [128, TT], BF16, "oTh%d" % i) for i in range(2)])
            bts = Rot([T(S, [128, NV * 5 * 128], F32, "bt%d" % i) for i in range(2)])
            tbs = Rot([T(S, [128, 640], F32, "tb%d" % i) for i in range(2)])
            pTs = Rot([T(S, [128, 1024], BF16, "pT%d" % i) for i in range(2)])
            recs = Rot([T(S, [128, 256], F32, "rec%d" % i) for i in range(2)])
            pos = Rot([pb[0], pb[1]])
            pds = Rot([pb[2], pb[3]])
            sts = Rot(st2)
            for b in range(B):
                for h in range(NHC):
                    qT, kT, V, oTh, bt = qTs.next(), kTs.next(), Vs.next(), oTs.next(), bts.next()
                    S.dma("sp", qT[:], qk_scr[b, h], writes=[qT.r])
                    S.dma("sp", kT[:], qk_scr[b, NHC + h], writes=[kT.r])
                    S.dma("sp", V[:], v_scr[b, h], writes=[V.r])
                    S.dma("sp", bt[:], bias[h], writes=[bt.r])
                    for rp in range(ROWS // 2):
                        cl, nb, var = geo[rp]
                        st, po, pd, tb, pT, rec = sts.next(), pos.next(), pds.next(), tbs.next(), pTs.next(), recs.next()
                        qsl = qT[:, rp * 128:(rp + 1) * 128]
                        for ci in range(nb):
                            S.op("pe", lambda e: e.matmul(st[:, ci * 128:(ci + 1) * 128], kT[:, (cl + ci) * 128:(cl + ci + 1) * 128], qsl, start=True, stop=True),
                                 reads=[kT.r, qT.r], writes=[st.r], inc=False)
                        for cj in range(NCC):
                            S.op("pe", lambda e: e.matmul(st[:, (5 + cj) * 128:(6 + cj) * 128], kT[:, L + cj * 128:L + (cj + 1) * 128], qsl, start=True, stop=True),
                                 reads=[kT.r, qT.r], writes=[st.r], inc=(cj == NCC - 1))
                        S.op("dve", lambda e: e.scalar_tensor_tensor(out=tb[:, :nb * 128], in0=st[:, :nb * 128], scalar=scale,
                                                                     in1=bt[:, var * 640:var * 640 + nb * 128], op0=ALU.mult, op1=ALU.add),
                             reads=[st.r, bt.r], writes=[tb.r])
                        S.op("act", lambda e: e.activation(out=pT[:, :nb * 128], in_=tb[:, :nb * 128], func=AF.Exp), reads=[tb.r], writes=[pT.r])
                        S.op("act", lambda e: e.activation(out=pT[:, 640:640 + CTXN], in_=st[:, 640:640 + CTXN], func=AF.Exp, scale=scale), reads=[st.r], writes=[pT.r])
                        slots = [(cl + ci, ci) for ci in range(nb)] + [(LCH + cj, 5 + cj) for cj in range(NCC)]
                        for i, (kchunk, slot) in enumerate(slots):
                            S.op("pe", lambda e: e.matmul(po[:, 0:128], V[:, kchunk, :], pT[:, slot * 128:(slot + 1) * 128], start=(i == 0), stop=(i == len(slots) - 1)),
                                 reads=[V.r, pT.r], writes=[po.r], inc=False)
                            S.op("pe", lambda e: e.matmul(pd[:, 0:128], ones_bf[:], pT[:, slot * 128:(slot + 1) * 128], start=(i == 0), stop=(i == len(slots) - 1)),
                                 reads=[ones_bf.r, pT.r], writes=[pd.r], inc=(i == len(slots) - 1))
                        S.op("dve", lambda e: e.reciprocal(out=rec[:, 0:128], in_=pd[:, 0:128]), reads=[pd.r], writes=[rec.r])
                        S.op("dve", lambda e: e.tensor_tensor(out=oTh[:, rp * 128:(rp + 1) * 128], in0=po[:, 0:128], in1=rec[:, 0:128], op=ALU.mult),
                             reads=[po.r, rec.r], writes=[oTh.r])
                    st, po, pd, pT, rec = sts.next(), pos.next(), pds.next(), pTs.next(), recs.next()
                    for cj in range(NCC):
                        S.op("pe", lambda e: e.matmul(st[:, cj * CTXN:(cj + 1) * CTXN], kT[:, L + cj * 128:L + (cj + 1) * 128], qT[:, L:L + CTXN], start=True, stop=True),
                             reads=[kT.r, qT.r], writes=[st.r], inc=(cj == NCC - 1))
                    S.op("act", lambda e: e.activation(out=pT[:, :NCC * CTXN], in_=st[:, :NCC * CTXN], func=AF.Exp, scale=scale), reads=[st.r], writes=[pT.r])
                    for cj in range(NCC):
                        S.op("pe", lambda e: e.matmul(po[:, 0:CTXN], V[:, LCH + cj, :], pT[:, cj * CTXN:(cj + 1) * CTXN], start=(cj == 0), stop=(cj == NCC - 1)),
                             reads=[V.r, pT.r], writes=[po.r], inc=False)
                        S.op("pe", lambda e: e.matmul(pd[:, 0:CTXN], ones_bf[:], pT[:, cj * CTXN:(cj + 1) * CTXN], start=(cj == 0), stop=(cj == NCC - 1)),
                             reads=[ones_bf.r, pT.r], writes=[pd.r], inc=(cj == NCC - 1))
                    S.op("dve", lambda e: e.reciprocal(out=rec[:, 0:CTXN], in_=pd[:, 0:CTXN]), reads=[pd.r], writes=[rec.r])
                    S.op("dve", lambda e: e.tensor_tensor(out=oTh[:, L:L + CTXN], in0=po[:, 0:CTXN], in1=rec[:, 0:CTXN], op=ALU.mult),
                         reads=[po.r, rec.r], writes=[oTh.r])
                    if fz:
                        fz.owrite_na(S, b, h, oTh)
                    else:
                        S.dma("sp", oT[b, h * 128:(h + 1) * 128, :], oTh[:], reads=[oTh.r], prim=oTh.r)
            S.barrier()
        S.tes = es
        if fz:
            S.phase_end()
        else:
            S.finish()
    return nc


def gla_consts(ROWS, W=CFG.GRID_W):
    L = ROWS * W
    nf = 64
    inv = (np.float32(10000.0) ** (-np.arange(nf, dtype=np.float32) / np.float32(nf))).astype(np.float32)
    pos = np.arange(L)
    p = np.arange(128)
    cos = np.zeros((128, 2, L), np.float32)
    sin = np.zeros((128, 2, L), np.float32)
    for ch, pp in enumerate((pos // W, pos % W)):
        ang = pp.astype(np.float32)[None, :] * inv[p % 64][:, None]
        cos[:, ch, :] = np.cos(ang)
        sin[:, ch, :] = np.sin(ang) * np.where(p < 64, -1.0, 1.0)[:, None].astype(np.float32)
    perm = np.zeros((128, 128), np.float32)
    perm[(p + 64) % 128, p] = 1.0
    si = np.arange(64)
    msk = np.zeros((64, 2, 64), np.float32)
    msk[:, 0, :] = (si[:, None] <= si[None, :])
    msk[:, 1, :] = (si[:, None] >= si[None, :])
    rmask = np.ones((128, 512), np.float32)
    rmask[:, ::64] = 0.0
    return cos, sin, perm, msk, rmask


def build_gla(D=CFG.D, ROWS=128, W=CFG.GRID_W, CTXN=CFG.CTX, B=CFG.B, dbg=0, fz=None):
    KC = D // 128
    L = ROWS * W
    TT = L + CTXN
    NCK = TT // 64
    DK, DV = 256, 512
    nc = fz.nc if fz else bass.Bass("TRN2", target_bir_lowering=False)
    dr = _mk_dr(nc, fz)
    xT = dr("xT", [B, D, TT], F32)
    cols = dr("cols", [128, 6 * KC], F32)
    win = dr("win", [D, 1536], F32)
    wa1 = dr("wa1", [D, 32], F32)
    wa2p = dr("wa2p", [32, 2, 256], F32)
    ba = dr("ba", [128, 4], F32)
    nw = dr("nw", [128, 4], F32)
    cos_d = dr("cos", [128, 2, L], F32)
    sin_d = dr("sin", [128, 2, L], F32)
    perm_d = dr("perm_in", [128, 128], F32)
    ident_d = dr("ident_in", [128, 128], F32)
    msk_d = dr("msk_in", [64, 2, 64], F32)
    rmask_d = dr("rmask_in", [128, 512], F32)
    oT = dr("oT", [B, DV, TT], BF16, "ExternalOutput")
    F_scr = dr("F_scr", [B, 2, 3, 2, 128, TT], BF16, "Internal")
    ksl_scr = dr("ksl_scr", [B, 2, TT, DK], BF16, "Internal")
    v_scr = dr("v_scr", [B, TT, DV], BF16, "Internal")
    g_scr = dr("g_scr", [B, 4, 128, TT], BF16, "Internal")
    of_scr = dr("of_scr", [B, TT, DV], F32, "Internal")
    tiles = [(t0, min(512, L - t0), 0) for t0 in range(0, L, 512)] + [(L, CTXN, 1)]
    with ExitStack() as es:
        S = fz.S if fz else Sched(nc, es)
        S.tes = es
        S.prefix = fz.pre if fz else ""
        pb = [T(S, [128, 512], F32, "pb%d" % i, psum=True) for i in range(6)]
        pbt = [T(S, [128, 1024], BF16, "pbt%d" % i, psum=True) for i in range(2)]
        colt = T(S, [128, 6 * KC], F32, "colt")
        ident = T(S, [128, 128], BF16, "ident")
        bat = T(S, [128, 4], F32, "bat")
        nwt = T(S, [128, 4], F32, "nwt")
        ebl = T(S, [128, B * 4, NCK], F32, "ebl")
        msk = T(S, [64, 2, 64], F32, "msk")
        if fz:
            fz.setup(S)
            for (blk, src) in fz.colsrc:
                S.dma("sp", colt[:, blk * KC:(blk + 1) * KC], src, writes=[colt.r])
        else:
            S.dma("sp", colt[:], cols, writes=[colt.r])
        S.dma("pool", ident[:], ident_d, writes=[ident.r])
        S.dma("sp", bat[:], ba, writes=[bat.r])
        S.dma("sp", nwt[:], nw, writes=[nwt.r])
        S.dma("sp", msk[:], msk_d, writes=[msk.r])
        S.op("dve", lambda e: e.tensor_scalar_mul(bat[:], bat[:], -1.0), reads=[bat.r], writes=[bat.r])
        for c in range(3):
            S.op("dve", lambda e, c=c: e.tensor_scalar_add(colt[:, (2 * c + 1) * KC:(2 * c + 2) * KC], colt[:, (2 * c + 1) * KC:(2 * c + 2) * KC], 1.0),
                 reads=[colt.r], writes=[colt.r])
        col = lambda idx, kc: colt[:, idx * KC + kc: idx * KC + kc + 1]
        with ExitStack() as es1:
            S.tes = es1
            Wt = T(S, [128, KC, 1536], BF16, "Wt")
            Wa = T(S, [128, KC, 32], BF16, "Wa")
            Wa2 = T(S, [32, 2, 256], BF16, "Wa2")
            perm = T(S, [128, 128], F32, "perm")
            rmask = T(S, [128, 512], F32, "rmask")
            hT = T(S, [128, KC, 512], BF16, "hT")
            a_bf = T(S, [32, 512], BF16, "a_bf")
            xcs = Rot([T(S, [128, 512], F32, "xc%d" % i) for i in range(2)])
            ft = Rot([T(S, [128, 512], F32, "ft%d" % i) for i in range(8)])
            qk32 = [T(S, [128, 512], F32, "qk32_%d" % i) for i in range(4)]
            cum = [T(S, [128, 512], F32, "cum%d" % i) for i in range(4)]
            cst = T(S, [128, 2, 512], F32, "cst")
            snt = T(S, [128, 2, 512], F32, "snt")
            obs = Rot([T(S, [128, 512], BF16, "ob%d" % i) for i in range(6)])
            kslS = Rot([T(S, [128, 4, 256], BF16, "kslS%d" % i) for i in range(2)])
            for j in range(3):
                S.dma("pool", Wt[:, :, j * 512:(j + 1) * 512], win.rearrange("(kc p) n -> p kc n", p=128)[:, :, j * 512:(j + 1) * 512], writes=[Wt.r])
            S.dma("pool", Wa[:], wa1.rearrange("(kc p) n -> p kc n", p=128), writes=[Wa.r])
            S.dma("pool", Wa2[:], wa2p, writes=[Wa2.r])
            S.dma("sp", perm[:], perm_d, writes=[perm.r])
            S.dma("sp", rmask[:], rmask_d, writes=[rmask.r])
            for b in range(B):
                for (t0, nt, isctx) in tiles:
                    cnd = 2 if isctx else b
                    nck = nt // 64
                    ck0 = t0 // 64
                    for kc in range(KC):
                        xc = xcs.next()
                        if fz:
                            fz.xload(S, b, kc, t0, nt, isctx, xc)
                        else:
                            S.dma("sp", xc[:, :nt], xT[b, kc * 128:(kc + 1) * 128, t0:t0 + nt], writes=[xc.r])
                        S.op("dve", lambda e: e.tensor_scalar(out=hT[:, kc, :nt], in0=xc[:, :nt], scalar1=col(2 * cnd + 1, kc), scalar2=col(2 * cnd, kc),
                                                              op0=ALU.mult, op1=ALU.add), reads=[xc.r, colt.r], writes=[hT.r])
                    if not isctx:
                        S.dma("sp", cst[:, :, :nt], cos_d[:, :, t0:t0 + nt], writes=[cst.r])
                        S.dma("sp", snt[:, :, :nt], sin_d[:, :, t0:t0 + nt], writes=[snt.r])

                    def proj(j, ps, n=nt):
                        for kc in range(KC):
                            S.op("pe", lambda e: e.matmul(ps[:, :n], Wt[:, kc, j * 128:(j + 1) * 128], hT[:, kc, :n], start=(kc == 0), stop=(kc == KC - 1)),
                                 reads=[Wt.r, hT.r], writes=[ps.r], inc=(kc == KC - 1))
                    for j in range(4):
                        ps = pb[j % 2]
                        proj(j, ps)
                        dst = qk32[j]
                        if isctx:
                            S.op("act", lambda e: e.activation(out=dst[:, :nt], in_=ps[:, :nt], func=AF.Copy, scale=(1.0 / 16.0 if j < 2 else 1.0)),
                                 reads=[ps.r], writes=[dst.r])
                        else:
                            raw = ft.next()
                            S.op("act", lambda e: e.activation(out=raw[:, :nt], in_=ps[:, :nt], func=AF.Copy, scale=(1.0 / 16.0 if j < 2 else 1.0)),
                                 reads=[ps.r], writes=[raw.r])
                            sw = pb[2]
                            S.op("pe", lambda e: e.matmul(sw[:, :nt], perm[:], raw[:, :nt], start=True, stop=True), reads=[perm.r, raw.r], writes=[sw.r])
                            t1 = ft.next()
                            S.op("dve", lambda e: e.tensor_tensor(out=t1[:, :nt], in0=sw[:, :nt], in1=snt[:, j % 2, :nt], op=ALU.mult), reads=[sw.r, snt.r], writes=[t1.r])
                            S.op("pool", lambda e: e.tensor_tensor(out=raw[:, :nt], in0=raw[:, :nt], in1=cst[:, j % 2, :nt], op=ALU.mult), reads=[raw.r, cst.r], writes=[raw.r])
                            S.op("pool", lambda e: e.tensor_tensor(out=dst[:, :nt], in0=raw[:, :nt], in1=t1[:, :nt], op=ALU.add), reads=[raw.r, t1.r], writes=[dst.r])
                    for j in range(4):
                        ps = pb[j % 2]
                        proj(8 + j, ps)
                        ob = obs.next()
                        S.op("act", lambda e: e.activation(out=ob[:, :nt], in_=ps[:, :nt], func=AF.Silu), reads=[ps.r], writes=[ob.r])
                        S.dma("sp", g_scr[b, j, :, t0:t0 + nt], ob[:, :nt], reads=[ob.r], prim=ob.r)
                    for tc in range(nt // 128):
                        ps = pb[tc % 2]
                        for kc in range(KC):
                            S.op("pe", lambda e: e.matmul(ps[:, :DV], hT[:, kc, tc * 128:(tc + 1) * 128], Wt[:, kc, 512:1024], start=(kc == 0), stop=(kc == KC - 1)),
                                 reads=[Wt.r, hT.r], writes=[ps.r], inc=(kc == KC - 1))
                        ob = obs.next()
                        S.op("act", lambda e: e.activation(out=ob[:, :DV], in_=ps[:, :DV], func=AF.Copy), reads=[ps.r], writes=[ob.r])
                        S.dma("sp", v_scr[b, t0 + tc * 128:t0 + (tc + 1) * 128, :], ob[:, :DV], reads=[ob.r], prim=ob.r)
                    pa = pb[3]
                    for kc in range(KC):
                        S.op("pe", lambda e: e.matmul(pa[:32, :nt], Wa[:, kc, :], hT[:, kc, :nt], start=(kc == 0), stop=(kc == KC - 1)),
                             reads=[Wa.r, hT.r], writes=[pa.r], inc=(kc == KC - 1))
                    S.op("act", lambda e: e.activation(out=a_bf[:, :nt], in_=pa[:32, :nt], func=AF.Copy), reads=[pa.r], writes=[a_bf.r])
                    for d in range(2):
                        for ch in range(2):
                            pz = pb[4 + ch]
                            S.op("pe", lambda e: e.matmul(pz[:, :nt], Wa2[:, d, ch * 128:(ch + 1) * 128], a_bf[:, :nt], start=True, stop=True),
                                 reads=[Wa2.r, a_bf.r], writes=[pz.r])
                            l_ = ft.next()
                            S.op("act", lambda e: e.activation(out=l_[:, :nt], in_=pz[:, :nt], func=AF.Exp, scale=-1.0, bias=bat[:, d * 2 + ch:d * 2 + ch + 1]),
                                 reads=[pz.r, bat.r], writes=[l_.r])
                            S.op("act", lambda e: e.activation(out=l_[:, :nt], in_=l_[:, :nt], func=AF.Ln, bias=1.0), reads=[l_.r], writes=[l_.r])
                            cm = cum[d * 2 + ch]
                            S.op("dve", lambda e: e.tensor_tensor_scan(out=cm[:, :nt], data0=rmask[:, :nt], data1=l_[:, :nt], initial=0.0, op0=ALU.mult, op1=ALU.add),
                                 reads=[rmask.r, l_.r], writes=[cm.r])
                            if d == 1:
                                c3 = cm[:, :nt].rearrange("p (c j) -> p c j", j=64)
                                l3 = l_[:, :nt].rearrange("p (c j) -> p c j", j=64)
                                S.op("pool", lambda e: e.tensor_tensor(out=l3, in0=l3, in1=c3[:, :, 63:64].broadcast_to([128, nck, 64]), op=ALU.add),
                                     reads=[l_.r, cm.r], writes=[l_.r])
                                S.op("pool", lambda e: e.tensor_tensor(out=cm[:, :nt], in0=l_[:, :nt], in1=cm[:, :nt], op=ALU.subtract), reads=[l_.r, cm.r], writes=[cm.r])
                    for d in range(2):
                        im, il = (32, 63) if d == 0 else (31, 0)
                        for ch in range(2):
                            cm = cum[d * 2 + ch]
                            c3 = cm[:, :nt].rearrange("p (c j) -> p c j", j=64)
                            qr, kr = qk32[ch], qk32[2 + ch]
                            dm = ft.next()
                            dm3 = dm[:, :nt].rearrange("p (c j) -> p c j", j=64)
                            S.op("dve", lambda e: e.tensor_tensor(out=dm3, in0=c3, in1=c3[:, :, im:im + 1].broadcast_to([128, nck, 64]), op=ALU.subtract),
                                 reads=[cm.r], writes=[dm.r])
                            specs = []
                            e1 = ft.next()
                            S.op("act", lambda e: e.activation(out=e1[:, :nt], in_=dm[:, :nt], func=AF.Exp, scale=-1.0 / 16.0), reads=[dm.r], writes=[e1.r])
                            specs.append((0, qr, e1))
                            e2 = ft.next()
                            S.op("act", lambda e: e.activation(out=e2[:, :nt], in_=dm[:, :nt], func=AF.Exp, scale=1.0 / 16.0), reads=[dm.r], writes=[e2.r])
                            specs.append((1, kr, e2))
                            e3 = ft.next()
                            S.op("act", lambda e: e.activation(out=e3[:, :nt], in_=cm[:, :nt], func=AF.Exp, scale=-1.0 / 16.0), reads=[cm.r], writes=[e3.r])
                            specs.append((2, qr, e3))
                            e33 = e3[:, :nt].rearrange("p (c j) -> p c j", j=64)
                            S.op("pool", lambda e: e.tensor_copy(out=ebl[:, b * 4 + d * 2 + ch, ck0:ck0 + nck], in_=e33[:, :, il]), reads=[e3.r], writes=[ebl.r])
                            for (which, src, ex) in specs:
                                ob = obs.next()
                                S.op("dve" if which != 1 else "pool", lambda e: e.tensor_tensor(out=ob[:, :nt], in0=src[:, :nt], in1=ex[:, :nt], op=ALU.mult),
                                     reads=[src.r, ex.r], writes=[ob.r])
                                S.dma("sp", F_scr[b, d, which, ch, :, t0:t0 + nt], ob[:, :nt], reads=[ob.r], prim=ob.r)
                            dl = ft.next()
                            dl3 = dl[:, :nt].rearrange("p (c j) -> p c j", j=64)
                            S.op("dve", lambda e: e.tensor_tensor(out=dl3, in0=c3, in1=c3[:, :, il:il + 1].broadcast_to([128, nck, 64]), op=ALU.subtract),
                                 reads=[cm.r], writes=[dl.r])
                            S.op("act", lambda e: e.activation(out=dl[:, :nt], in_=dl[:, :nt], func=AF.Exp, scale=1.0 / 16.0), reads=[dl.r], writes=[dl.r])
                            kb = obs.next()
                            S.op("dve", lambda e: e.tensor_tensor(out=kb[:, :nt], in0=kr[:, :nt], in1=dl[:, :nt], op=ALU.mult), reads=[kr.r, dl.r], writes=[kb.r])
                            if ch == 0:
                                ks_t = kslS.next()
                            pt = pbt[ch]
                            for tc in range(nt // 128):
                                S.op("pe", lambda e: e.transpose(out=pt[:, tc * 128:(tc + 1) * 128], in_=kb[:, tc * 128:(tc + 1) * 128], identity=ident[:]),
                                     reads=[kb.r, ident.r], writes=[pt.r], inc=(tc == nt // 128 - 1))
                            S.op("act", lambda e: e.activation(out=ks_t[:, :nt // 128, ch * 128:(ch + 1) * 128], in_=pt[:, :nt].rearrange("p (t n) -> p t n", n=128), func=AF.Copy),
                                 reads=[pt.r], writes=[ks_t.r])
                            if ch == 1:
                                S.dma("sp", ksl_scr[b, d, t0:t0 + nt, :].rearrange("(t p) n -> p t n", p=128), ks_t[:, :nt // 128, :], reads=[ks_t.r], prim=ks_t.r)
            S.barrier()
        with ExitStack() as es2:
            S.tes = es2
            if dbg == 1:
                B = 0
            St = T(S, [128, 2, DV], F32, "St")
            Sb = T(S, [128, 2, DV], BF16, "Sb")
            Fb = Rot([T(S, [128, 6, 512], BF16, "Fb%d" % i) for i in range(2)])
            kslb = Rot([T(S, [64, 8, DK], BF16, "kslb%d" % i) for i in range(2)])
            vb = Rot([T(S, [64, 8, DV], BF16, "vb%d" % i) for i in range(2)])
            ofb = Rot([T(S, [64, 8, DV], F32, "ofb%d" % i) for i in range(2)])
            gb = Rot([T(S, [128, 4, 512], BF16, "gb%d" % i) for i in range(2)])
            oTb = Rot([T(S, [128, 4, 512], BF16, "oTb%d" % i) for i in range(2)])
            atb = Rot([T(S, [64, 64], BF16, "atb%d" % i) for i in range(2)])
            osb = Rot([T(S, [64, DV], F32, "osb%d" % i) for i in range(2)])
            onb = Rot([T(S, [64, DV], BF16, "onb%d" % i) for i in range(2)])
            sq = T(S, [64, DV], F32, "sqj")
            msb = Rot([T(S, [64, 4], F32, "msb%d" % i) for i in range(2)])
            tmb = Rot([T(S, [128, 4, 64], F32, "tmb%d" % i) for i in range(2)])
            pat = Rot([pb[0], pb[1]])
            pos_ = Rot([pb[2], pb[3]])
            for b in range(B):
                for d in range(1 if dbg == 2 else 2):
                    S.op("dve", lambda e: e.memset(St[:], 0.0), writes=[St.r])
                    S.op("dve", lambda e: e.memset(Sb[:], 0.0), writes=[Sb.r])
                    lat = [t for t in tiles if not t[2]]
                    order = [tiles[-1]] + (lat if d == 0 else lat[::-1])
                    for (t0, nt, isctx) in order:
                        nck = nt // 64
                        F, ksl, v = Fb.next(), kslb.next(), vb.next()
                        for which in range(3):
                            for ch in range(2):
                                S.dma("sp", F[:, which * 2 + ch, :nt], F_scr[b, d, which, ch, :, t0:t0 + nt], writes=[F.r])
                        S.dma("sp", ksl[:, :nck, :], ksl_scr[b, d, t0:t0 + nt, :].rearrange("(c p) n -> p c n", p=64), writes=[ksl.r])
                        S.dma("sp", v[:, :nck, :], v_scr[b, t0:t0 + nt, :].rearrange("(c p) n -> p c n", p=64), writes=[v.r])
                        of = ofb.next()
                        if d == 1:
                            g, oTt = gb.next(), oTb.next()
                            S.dma("sp", of[:, :nck, :], of_scr[b, t0:t0 + nt, :].rearrange("(c p) n -> p c n", p=64), writes=[of.r])
                            for j in range(4):
                                S.dma("sp", g[:, j, :nt], g_scr[b, j, :, t0:t0 + nt], writes=[g.r])
                        for c in (range(nck) if d == 0 else range(nck - 1, -1, -1)):
                            cs = c * 64
                            gck = t0 // 64 + c
                            pa_ = pat.next()
                            for ch in range(2):
                                S.op("pe", lambda e: e.matmul(pa_[:64, :64], F[:, 2 + ch, cs:cs + 64], F[:, 0 + ch, cs:cs + 64], start=(ch == 0), stop=(ch == 1)),
                                     reads=[F.r], writes=[pa_.r], inc=(ch == 1))
                            at = atb.next()
                            S.op("dve", lambda e: e.tensor_tensor(out=at[:], in0=pa_[:64, :64], in1=msk[:, d, :], op=ALU.mult), reads=[pa_.r, msk.r], writes=[at.r])
                            po = pos_.next()
                            S.op("pe", lambda e: e.matmul(po[:64, :DV], at[:], v[:, c, :], start=True, stop=False), reads=[at.r, v.r], writes=[po.r], inc=False)
                            for ch in range(2):
                                S.op("pe", lambda e: e.matmul(po[:64, :DV], F[:, 4 + ch, cs:cs + 64], Sb[:, ch, :], start=False, stop=(ch == 1)),
                                     reads=[F.r, Sb.r], writes=[po.r], inc=(ch == 1))
                            for ch in range(2):
                                pd_ = pb[4 + ch]
                                S.op("pe", lambda e: e.matmul(pd_[:, :DV], ksl[:, c, ch * 128:(ch + 1) * 128], v[:, c, :], start=True, stop=True),
                                     reads=[ksl.r, v.r], writes=[pd_.r])
                                S.op("dve", lambda e: e.scalar_tensor_tensor(out=St[:, ch, :], in0=St[:, ch, :], scalar=ebl[:, b * 4 + d * 2 + ch, gck:gck + 1],
                                                                             in1=pd_[:, :DV], op0=ALU.mult, op1=ALU.add), reads=[St.r, ebl.r, pd_.r], writes=[St.r])
                                S.op("act", lambda e: e.activation(out=Sb[:, ch, :], in_=St[:, ch, :], func=AF.Copy), reads=[St.r], writes=[Sb.r])
                            if d == 0:
                                S.op("act", lambda e: e.activation(out=of[:, c, :], in_=po[:64, :DV], func=AF.Copy), reads=[po.r], writes=[of.r])
                            else:
                                os_, on_, ms = osb.next(), onb.next(), msb.next()
                                S.op("dve", lambda e: e.tensor_tensor(out=os_[:], in0=po[:64, :DV], in1=of[:, c, :], op=ALU.add), reads=[po.r, of.r], writes=[os_.r])
                                if dbg == 4:
                                    continue
                                S.op("pool", lambda e: e.tensor_tensor(out=sq[:], in0=os_[:], in1=os_[:], op=ALU.mult), reads=[os_.r], writes=[sq.r])
                                S.op("dve", lambda e: e.tensor_reduce(out=ms[:, 0:1], in_=sq[:], axis=AX.X, op=ALU.add), reads=[sq.r], writes=[ms.r])
                                S.op("dve", lambda e: e.tensor_scalar(out=ms[:, 1:2], in0=ms[:, 0:1], scalar1=1.0 / DV, scalar2=float(CFG.EPS), op0=ALU.mult, op1=ALU.add),
                                     reads=[ms.r], writes=[ms.r])
                                S.op("act", lambda e: e.activation(out=ms[:, 2:3], in_=ms[:, 1:2], func=AF.Sqrt), reads=[ms.r], writes=[ms.r])
                                S.op("dve", lambda e: e.reciprocal(out=ms[:, 3:4], in_=ms[:, 2:3]), reads=[ms.r], writes=[ms.r])
                                S.op("dve", lambda e: e.tensor_scalar_mul(on_[:], os_[:], ms[:, 3:4]), reads=[os_.r, ms.r], writes=[on_.r])
                                pt = pbt[(gck) % 2]
                                if dbg == 3:
                                    continue
                                for j in range(4):
                                    S.op("pe", lambda e: e.transpose(out=pt[:, j * 64:(j + 1) * 64], in_=on_[:, j * 128:(j + 1) * 128], identity=ident[:64, :64]),
                                         reads=[on_.r, ident.r], writes=[pt.r], inc=(j == 3))
                                tm = tmb.next()
                                for j in range(4):
                                    S.op("act", lambda e: e.activation(out=tm[:, j, :], in_=pt[:, j * 64:(j + 1) * 64], func=AF.Copy, scale=nwt[:, j:j + 1]),
                                         reads=[pt.r, nwt.r], writes=[tm.r])
                                S.op("dve", lambda e: e.tensor_tensor(out=oTt[:, :, cs:cs + 64], in0=tm[:], in1=g[:, :, cs:cs + 64], op=ALU.mult),
                                     reads=[tm.r, g.r], writes=[oTt.r])
                        if d == 0:
                            S.dma("sp", of_scr[b, t0:t0 + nt, :].rearrange("(c p) n -> p c n", p=64), of[:, :nck, :], reads=[of.r], prim=of.r)
                        elif fz:
                            fz.owrite_gla(S, b, t0, nt, isctx, oTt)
                        else:
                            S.dma("sp", oT[b].rearrange("(j p) t -> p j t", p=128)[:, :, t0:t0 + nt], oTt[:, :, :nt], reads=[oTt.r], prim=oTt.r)
                    S.barrier()
        S.tes = es
        if fz:
            S.phase_end()
        else:
            S.finish()
    return nc


def build_ada(D=CFG.D, R=CFG.ADA_R, NL=CFG.DEPTH, NCND=3, fz=None):
    KC = D // 128
    NJ = 6 * D // 128
    RC = R // 128
    HJ = NJ // 2
    nc = fz.nc if fz else bass.Bass("TRN2", target_bir_lowering=False)
    dr = _mk_dr(nc, fz)
    condT = dr("condT", [128, KC, NCND], F32)
    w1 = dr("w1", [NL, D, R], F32)
    w2 = dr("w2", [NL, R, 6 * D], F32)
    bT = dr("bT", [NL, 128, NJ], F32)
    mods = dr("mods", [NL, NCND, 128, NJ], F32, "ExternalOutput")
    with ExitStack() as es:
        S = fz.S if fz else Sched(nc, es)
        S.tes = es
        S.prefix = fz.pre if fz else ""
        ct = T(S, [128, KC, NCND], F32, "ct")
        sT = T(S, [128, KC, NCND], BF16, "sT")
        W1 = T(S, [128, KC, R], BF16, "W1")
        W2 = T(S, [128, RC, 6 * D], BF16, "W2")
        bt = T(S, [128, NJ], F32, "bt")
        ub = T(S, [128, RC, NCND], BF16, "ub")
        res = T(S, [128, NCND, NJ], F32, "res")
        pu = T(S, [128, 512], F32, "pu", psum=True)
        pm = [T(S, [128, 512], F32, "pm%d" % i, psum=True) for i in range(2)]
        S.dma("sp", ct[:], condT, writes=[ct.r])
        S.op("act", lambda e: e.activation(out=sT[:], in_=ct[:], func=AF.Silu), reads=[ct.r], writes=[sT.r])
        for l in range(NL):
            S.dma("pool", W1[:], w1[l].rearrange("(kc p) n -> p kc n", p=128), writes=[W1.r])
            for q in range(6):
                S.dma("pool", W2[:, :, q * D:(q + 1) * D], w2[l].rearrange("(kc p) n -> p kc n", p=128)[:, :, q * D:(q + 1) * D], writes=[W2.r])
            S.dma("sp", bt[:], bT[l], writes=[bt.r])
            for m in range(RC):
                for kc in range(KC):
                    S.op("pe", lambda e: e.matmul(pu[:, m * 8:m * 8 + NCND], W1[:, kc, m * 128:(m + 1) * 128], sT[:, kc, :], start=(kc == 0), stop=(kc == KC - 1)),
                         reads=[W1.r, sT.r], writes=[pu.r], inc=(kc == KC - 1))
            for m in range(RC):
                S.op("act", lambda e: e.activation(out=ub[:, m, :], in_=pu[:, m * 8:m * 8 + NCND], func=AF.Copy), reads=[pu.r], writes=[ub.r])
            for j in range(NJ):
                p_ = pm[j // HJ]
                o_ = (j % HJ) * NCND
                for m in range(RC):
                    S.op("pe", lambda e: e.matmul(p_[:, o_:o_ + NCND], W2[:, m, j * 128:(j + 1) * 128], ub[:, m, :], start=(m == 0), stop=(m == RC - 1)),
                         reads=[W2.r, ub.r], writes=[p_.r], inc=(m == RC - 1))
            for half in range(2):
                S.op("dve", lambda e: e.tensor_tensor(out=res[:, :, half * HJ:(half + 1) * HJ].rearrange("p c j -> p j c"),
                                                      in0=pm[half][:, :HJ * NCND].rearrange("p (j c) -> p j c", c=NCND),
                                                      in1=bt[:, half * HJ:(half + 1) * HJ].unsqueeze(2).broadcast_to([128, HJ, NCND]), op=ALU.add),
                     reads=[pm[half].r, bt.r], writes=[res.r])
            S.dma("sp", mods[l].rearrange("c p j -> p c j"), res[:], reads=[res.r], prim=res.r)
        if fz:
            S.phase_end()
        else:
            S.finish()
    return nc


def build_prep(NL=CFG.DEPTH, D=CFG.D, NE=CFG.NE, HD=CFG.HD):
    KC = D // 128
    KH = max(1, KC // 2)
    NKH = (KC + KH - 1) // KH
    NPAIR = NE // 2
    NPC = NPAIR // 8
    NCH = 3 * NPAIR + 2
    NR = 3 * NPAIR
    H1 = (NCH + 1) // 2
    MCC = KC // 8
    CW = MCC * 128
    nc = bass.Bass("TRN2", target_bir_lowering=False)
    dr = _mk_dr(nc, None)
    wg = dr("wg", [NL, 2 * NPC, D, HD], F32)
    wu = dr("wu", [NL, 2 * NPC, D, HD], F32)
    sg = dr("sg", [NL, D, HD], F32)
    su = dr("su", [NL, D, HD], F32)
    wd = dr("wd", [NL, NE * HD, CW], F32)
    sd = dr("sd", [NL, HD, CW], F32)
    wo = dr("wo", [NL, D, CW], F32)
    o_gu = dr("o_gu", [NL, NPC, 2, NKH, 128, KH * 2 * HD], BF16, "ExternalOutput")
    o_sh = dr("o_sh", [NL, 2, NKH, 128, KH * HD], BF16, "ExternalOutput")
    o_wd = dr("o_wd", [NL, MCC, 2, 128, H1 * 128], BF16, "ExternalOutput")
    o_wo = dr("o_wo", [NL, MCC, 128, KC * 128], BF16, "ExternalOutput")
    with ExitStack() as es:
        S = Sched(nc, es)
        USZ = max(KH * 2 * HD, H1 * 128, KC * 128)
        arena = Rot([T(S, [128, USZ], BF16, "ar%d" % i) for i in range(4)])
        zt = T(S, [128, 128], BF16, "zt")
        S.op("dve", lambda e: e.memset(zt[:], 0.0), writes=[zt.r])
        for l in range(NL):
            for which, wsrc, ssrc in ((0, wg, sg), (1, wu, su)):
                for kh in range(NKH):
                    k0 = kh * KH
                    nk = min(KH, KC - k0)
                    for p in range(NPC):
                        u = arena.next()
                        uv = u.t[:, 0:nk * 2 * HD].rearrange("p (kc n) -> p kc n", n=2 * HD)
                        for j in range(2):
                            S.dma("pool", uv[:, :, j * HD:(j + 1) * HD], wsrc[l, 2 * p + j].rearrange("(kc p) n -> p kc n", p=128)[:, k0:k0 + nk, :], writes=[u.r])
                        S.dma("sp", o_gu[l, p, which, kh][:, 0:nk * 2 * HD], u.t[:, 0:nk * 2 * HD], reads=[u.r], prim=u.r)
                    u = arena.next()
                    uv = u.t[:, 0:nk * HD].rearrange("p (kc n) -> p kc n", n=HD)
                    S.dma("pool", uv, ssrc[l].rearrange("(kc p) n -> p kc n", p=128)[:, k0:k0 + nk, :], writes=[u.r])
                    S.dma("sp", o_sh[l, which, kh][:, 0:nk * HD], u.t[:, 0:nk * HD], reads=[u.r], prim=u.r)
            for m4 in range(MCC):
                for half in range(2):
                    c0 = half * H1
                    c1 = min(NCH, c0 + H1)
                    u = arena.next()
                    uv = u.t[:, 0:(c1 - c0) * 128].rearrange("p (c n) -> p c n", n=128)
                    rc1 = min(c1, NR)
                    if rc1 > c0:
                        S.dma("pool", uv[:, 0:rc1 - c0, :], wd[l].rearrange("(c p) n -> p c n", p=128)[:, c0:rc1, m4 * 128:(m4 + 1) * 128], writes=[u.r])
                    if c1 > NR:
                        S.op("pool", lambda e: e.tensor_copy(out=uv[:, NR + 1 - c0, :], in_=zt[:]), reads=[zt.r], writes=[u.r])
                        S.dma("pool", uv[:, NR - c0, :], sd[l][0:128, m4 * 128:(m4 + 1) * 128], writes=[u.r])
                        S.dma("pool", uv[0:HD - 128, NR + 1 - c0, :], sd[l][128:HD, m4 * 128:(m4 + 1) * 128], writes=[u.r])
                    S.dma("sp", o_wd[l, m4, half][:, 0:(c1 - c0) * 128], u.t[:, 0:(c1 - c0) * 128], reads=[u.r], prim=u.r)
                u = arena.next()
                uv = u.t[:, 0:KC * 128].rearrange("p (kc n) -> p kc n", n=128)
                S.dma("pool", uv, wo[l].rearrange("(kc p) n -> p kc n", p=128)[:, :, m4 * 128:(m4 + 1) * 128], writes=[u.r])
                S.dma("sp", o_wo[l, m4], u.t[:, 0:KC * 128], reads=[u.r], prim=u.r)
        S.finish()
    return nc


def _run_prep(p, NLAY, NE):
    D = CFG.D
    KC = D // 128
    CW = (KC // 8) * 128
    NPC = NE // 2 // 8
    in_maps = []
    for k in range(8):
        wo = np.stack([(p["na_w_o"][l // 2] if l % 2 == 0 else p["gla_w_o"][l // 2])[:, k * CW:(k + 1) * CW] for l in range(NLAY)])
        in_maps.append(dict(
            wg=np.ascontiguousarray(p["moe_w_gate"][:NLAY, 2 * NPC * k:2 * NPC * (k + 1)]),
            wu=np.ascontiguousarray(p["moe_w_up"][:NLAY, 2 * NPC * k:2 * NPC * (k + 1)]),
            sg=np.ascontiguousarray(p["sh_w_gate"][:NLAY]), su=np.ascontiguousarray(p["sh_w_up"][:NLAY]),
            wd=np.ascontiguousarray(p["moe_w_down"][:NLAY, :NE, :, k * CW:(k + 1) * CW]).reshape(NLAY, NE * CFG.HD, CW),
            sd=np.ascontiguousarray(p["sh_w_down"][:NLAY, :, k * CW:(k + 1) * CW]), wo=np.ascontiguousarray(wo)))
    outs = _run(_prog(("prep", NLAY, NE), lambda: build_prep(NL=NLAY, NE=NE)), in_maps)
    res = []
    for l in range(NLAY):
        res.append(dict(wgu=np.ascontiguousarray(np.concatenate([o["o_gu"][l] for o in outs], axis=0)), wsh=np.ascontiguousarray(outs[0]["o_sh"][l]),
                        wdp=np.ascontiguousarray(np.concatenate([o["o_wd"][l] for o in outs], axis=0)),
                        wop=np.ascontiguousarray(np.concatenate([o["o_wo"][l] for o in outs], axis=0))))
    return res


def build_fused(L=CFG.SEQ, NLAY=CFG.DEPTH, NE=CFG.NE):
    D, KC, B, CT, HD = CFG.D, CFG.D // 128, CFG.B, CFG.CTX, CFG.HD
    ROWS = L // CFG.GRID_W
    LS, CS = L // 4, CT // 4
    TS, TT = LS + CS, L + CT
    NJ = 6 * KC
    nc = bass.Bass("TRN2", target_bir_lowering=False)
    din = lambda n, s, d: nc.dram_tensor(n, s, d, kind="ExternalInput").ap()
    xs = din("xs", [D, TS], F32)
    mask8_d = din("mask8", [128, 8], F32)
    ident_in = din("ident_in", [128, 128], F32)
    gconst = dict(cos=din("cos", [128, 2, L], F32), sin=din("sin", [128, 2, L], F32), perm_in=din("perm_in", [128, 128], F32),
                  msk_in=din("msk_in", [64, 2, 64], F32), rmask_in=din("rmask_in", [128, 512], F32))
    x_out = nc.dram_tensor("x_out", [D, TS], F32, kind="ExternalOutput").ap()
    xsh = nc.dram_tensor("xsh", [D, TS], F32).ap()
    DH = D // 2
    XGs = [nc.dram_tensor("XG%d" % i, [8 * DH, TS], F32, addr_space="Shared").ap() for i in range(2)]
    OSC = nc.dram_tensor("OSC", [8 * D, TS], BF16).ap()
    OSH = nc.dram_tensor("OSH", [D, TS], BF16).ap()
    modsD = nc.dram_tensor("modsD", [NLAY, 4, 128, NJ], F32).ap()
    r_scr = nc.dram_tensor("r_scr", [D, 512], F32).ap()
    x1_scr = nc.dram_tensor("x1_scr", [D, 512], F32).ap()
    ffn_tiles = [(t0, min(512, LS - t0), 0) for t0 in range(0, LS, 512)] + [(LS, CS, 1)]

    def segs_lat(b, t0, nt):
        out, t = [], t0
        while t < t0 + nt:
            r = t // LS
            ln = min(t0 + nt, (r + 1) * LS) - t
            out.append((b * 4 + r, t % LS, t - t0, ln))
            t += ln
        return out

    def segs_ctx(b, base):
        return [(b * 4 + qq, LS, base + qq * CS, CS) for qq in range(4)]

    class Hooks:
        pass
    hk = Hooks()

    def setup(S):
        hk.m8 = T(S, [128, 8], F32, "m8")
        S.dma("sp", hk.m8[:], mask8_d, writes=[hk.m8.r])
        hk.tmps = Rot([T(S, [128, 2048], BF16, "mt%d" % i) for i in range(2)])

    def xload(S, b, kc, t0, nt, isctx, xc):
        sg = segs_ctx(b, 0) if isctx else segs_lat(b, t0, nt)
        for (r, c0, off, ln) in sg:
            hf, kk = divmod(kc * 128, DH)
            S.dma("sp", xc[:, off:off + ln], XGs[hf][r * DH + kk:r * DH + kk + 128, c0:c0 + ln], writes=[xc.r])

    def owrite_na(S, b, h, oTh):
        sg = [(b * 4 + r, 0, r * LS, LS) for r in range(4)] + segs_ctx(b, L)
        for (r, c0, off, ln) in sg:
            for p0 in range(0, ln, 2048):
                pl = min(2048, ln - p0)
                for s_ in range(8):
                    tmp = hk.tmps.next()
                    S.op("pool", lambda e: e.tensor_scalar_mul(tmp[:, :pl], oTh[:, off + p0:off + p0 + pl], hk.m8[:, s_:s_ + 1]),
                         reads=[oTh.r, hk.m8.r], writes=[tmp.r])
                    row = r * D + s_ * 512 + h * 128
                    S.dma("sp", OSC[row:row + 128, c0 + p0:c0 + p0 + pl], tmp[:, :pl], reads=[tmp.r], prim=tmp.r)

    def owrite_gla(S, b, t0, nt, isctx, oTt):
        sg = segs_ctx(b, 0) if isctx else segs_lat(b, t0, nt)
        for (r, c0, off, ln) in sg:
            for s_ in range(8):
                tmp = hk.tmps.next()
                tv = tmp.t[:, 0:4 * ln].rearrange("p (j t) -> p j t", j=4)
                S.op("pool", lambda e: e.tensor_scalar_mul(tv, oTt[:, :, off:off + ln], hk.m8[:, s_:s_ + 1]), reads=[oTt.r, hk.m8.r], writes=[tmp.r])
                row = r * D + s_ * 512
                S.dma("sp", OSC[row:row + 512, c0:c0 + ln].rearrange("(j p) t -> p j t", p=128), tv, reads=[tmp.r], prim=tmp.r)

    with ExitStack() as es0:
        S = Sched(nc, es0)
        build_ada(NL=NLAY, NCND=4, fz=FZ(nc, S, "A_", io=dict(mods=modsD)))
        cp = Res("xcp")
        for i in range(8):
            S.dma("sp", xsh[i * D // 8:(i + 1) * D // 8, :], xs[i * D // 8:(i + 1) * D // 8, :], prim=cp)
        for i in range(2):
            S.cc("AllGather", ALU.bypass, xsh[i * DH:(i + 1) * DH, :], XGs[i])
        for l in range(NLAY):
            pre = "L%d_" % l
            mcolsrc = []
            for cnd in range(3):
                mcolsrc.append((2 * cnd, modsD[l, cnd, :, 0:KC]))
                mcolsrc.append((2 * cnd + 1, modsD[l, cnd, :, KC:2 * KC]))
            mio = dict(xT=None, cols=None, oT=None, ident_in=ident_in)
            if l % 2 == 0:
                build_na(ROWS=ROWS, fz=FZ(nc, S, pre + "m_", io=mio, setup=setup, xload=xload, owrite_na=owrite_na, colsrc=mcolsrc))
            else:
                mio.update(gconst)
                build_gla(ROWS=ROWS, fz=FZ(nc, S, pre + "m_", io=mio, setup=setup, xload=xload, owrite_gla=owrite_gla, colsrc=mcolsrc))
            S.cc("ReduceScatter", ALU.add, OSC, OSH)
            lncols = din(pre + "lncols", [128, 4 * KC], F32)
            fcolsrc = []
            for ci, cnd in enumerate((3, 2)):
                for i, ty in enumerate((2, 4, 3, 5)):
                    fcolsrc.append((4 * ci + i, modsD[l, cnd, :, ty * KC:(ty + 1) * KC]))
            for i in range(4):
                fcolsrc.append((8 + i, lncols[:, i * KC:(i + 1) * KC]))
            fio = dict(xT=xsh, oT=OSH, x2T=None, cols=None, r_scr=r_scr, x1_scr=x1_scr, ident_in=ident_in)
            last = (l == NLAY - 1)
            build_ffn(ffn_tiles, NE=NE, fz=FZ(nc, S, pre + "f_", io=fio, colsrc=fcolsrc, x2outs=[xsh] + ([x_out] if last else [])))
            if not last:
                for i in range(2):
                    S.cc("AllGather", ALU.bypass, xsh[i * DH:(i + 1) * DH, :], XGs[i])
        S.finish()
    return nc


_PROGS = {}


def _prog(name, fn):
    if name not in _PROGS:
        _PROGS[name] = fn()
    return _PROGS[name]


def _run(nc, in_maps):
    res = run_bass_kernel_spmd(nc, in_maps, core_ids=list(range(8)))
    return res.results


def _colset(v, KC):
    return np.ascontiguousarray(v.reshape(KC, 128).T)


_f32 = lambda a: np.ascontiguousarray(np.asarray(a, dtype=np.float32))


def _mixer_weights(l, k, p, ROWS):
    D = CFG.D
    j = l // 2
    if l % 2 == 0:
        wq = p["na_w_qkv"][j]
        wsel = np.ascontiguousarray(np.concatenate([wq[:, t * D + k * 512:t * D + (k + 1) * 512] for t in range(3)], axis=1))
        btab = na_bias_tables(_f32(p["na_rpb"][j][4 * k:4 * k + 4]), ROWS)
        return dict(wqkv=wsel, bias=np.ascontiguousarray(btab.reshape(4, 128, -1)))
    wi = p["gla_w_in"][j]
    win = np.ascontiguousarray(np.concatenate([wi[:, k * 256:(k + 1) * 256], wi[:, 2048 + k * 256:2048 + (k + 1) * 256],
                                               wi[:, 4096 + k * 512:4096 + (k + 1) * 512], wi[:, 8192 + k * 512:8192 + (k + 1) * 512]], axis=1))
    wa1 = np.ascontiguousarray(np.concatenate([p["gla_w_a1"][j][0], p["gla_w_a1"][j][1]], axis=1))
    w_a2, b_a = p["gla_w_a2"][j], p["gla_b_a"][j]
    wa2p = np.zeros((32, 2, 256), np.float32)
    wa2p[0:16, 0] = w_a2[0][:, k * 256:(k + 1) * 256]
    wa2p[16:32, 1] = w_a2[1][:, k * 256:(k + 1) * 256]
    ba = np.ascontiguousarray(np.stack([b_a[d][k * 256 + ch * 128:k * 256 + (ch + 1) * 128] for d in range(2) for ch in range(2)], axis=1))
    nwt = np.ascontiguousarray(p["gla_norm"][j].reshape(4, 128).T)
    return dict(win=win, wa1=wa1, wa2p=wa2p, ba=ba, nw=nwt)


def _ffn_weights(l, p, NE):
    D = CFG.D
    w_o = p["na_w_o"][l // 2] if l % 2 == 0 else p["gla_w_o"][l // 2]
    return dict(wo=w_o, wr=np.ascontiguousarray(p["moe_router"][l][:, :NE]), eb=np.ascontiguousarray(p["moe_bias"][l][:NE].reshape(1, NE)),
                wg=p["moe_w_gate"][l][:NE], wu=p["moe_w_up"][l][:NE], wd=np.ascontiguousarray(p["moe_w_down"][l][:NE]).reshape(NE * CFG.HD, D),
                sg=p["sh_w_gate"][l], su=p["sh_w_up"][l], sd=p["sh_w_down"][l])


def _lncols(l, p):
    KC = CFG.D // 128
    return np.ascontiguousarray(np.concatenate([_colset(p["ln_g"][l, 0], KC), _colset(p["ln_b"][l, 0], KC),
                                                _colset(p["ln_g"][l, 1], KC), _colset(p["ln_b"][l, 1], KC)], axis=1))


def _stage_x(p):
    x, ctx = p["x"], p["ctx"]
    B, L, D = x.shape
    CT = ctx.shape[1]
    xT = np.empty((B, D, L + CT), np.float32)
    for b in range(B):
        xT[b, :, :L] = x[b].T
        xT[b, :, L:] = ctx[b].T
    return xT


def kernel_fused(p, NLAY=CFG.DEPTH, NE=CFG.NE):
    D, KC = CFG.D, CFG.D // 128
    B, L, _ = p["x"].shape
    CT = p["ctx"].shape[1]
    ROWS = L // CFG.GRID_W
    LS, CS = L // 4, CT // 4
    xT = _stage_x(p)
    eye = np.eye(128, dtype=np.float32)
    cos, sin, perm, msk, rmask = gla_consts(ROWS)
    bT = np.ascontiguousarray(p["ada_b"][:NLAY].reshape(NLAY, 6 * KC, 128).transpose(0, 2, 1))
    w1, w2 = np.ascontiguousarray(p["ada_w1"][:NLAY]), np.ascontiguousarray(p["ada_w2"][:NLAY])
    in_maps = []
    for k in range(8):
        b, q = k // 4, k % 4
        conds = np.stack([p["c"][0], p["c"][1], p["c_ctx"], p["c"][b]], axis=0)
        m = dict(xs=np.ascontiguousarray(np.concatenate([xT[b][:, q * LS:(q + 1) * LS], xT[b][:, L + q * CS:L + (q + 1) * CS]], axis=1)),
                 mask8=np.ascontiguousarray(np.broadcast_to((np.arange(8) == k).astype(np.float32)[None, :], (128, 8))),
                 ident_in=eye, cos=cos, sin=sin, perm_in=perm, msk_in=msk, rmask_in=rmask,
                 A_condT=np.ascontiguousarray(conds.reshape(4, KC, 128).transpose(2, 1, 0)), A_w1=w1, A_w2=w2, A_bT=bT)
        for l in range(NLAY):
            for n, v in _mixer_weights(l, k, p, ROWS).items():
                m["L%d_m_%s" % (l, n)] = v
            for n, v in _ffn_weights(l, p, NE).items():
                m["L%d_f_%s" % (l, n)] = v
            m["L%d_lncols" % l] = _lncols(l, p)
        in_maps.append(m)
    outs = _run(_prog(("fused", L, NLAY, NE), lambda: build_fused(L, NLAY, NE)), in_maps)
    out = np.empty((B, L, D), np.float32)
    for k in range(8):
        b, q = k // 4, k % 4
        out[b, q * LS:(q + 1) * LS, :] = outs[k]["x_out"][:, :LS].T
    return out


def kernel_unfused(p, NLAY=CFG.DEPTH, NE=CFG.NE):
    D, KC = CFG.D, CFG.D // 128
    B, L, _ = p["x"].shape
    CT = p["ctx"].shape[1]
    ROWS = L // CFG.GRID_W
    LS, CS = L // 4, CT // 4
    eye = np.eye(128, dtype=np.float32)
    conds = np.stack([p["c"][0], p["c"][1], p["c_ctx"]], axis=0)
    condT = np.ascontiguousarray(conds.reshape(3, KC, 128).transpose(2, 1, 0))
    bT = np.ascontiguousarray(p["ada_b"][:NLAY].reshape(NLAY, 6 * KC, 128).transpose(0, 2, 1))
    ada_in = dict(condT=condT, w1=np.ascontiguousarray(p["ada_w1"][:NLAY]), w2=np.ascontiguousarray(p["ada_w2"][:NLAY]), bT=bT)
    mods = _run(_prog(("ada", NLAY), lambda: build_ada(NL=NLAY)), [ada_in] * 8)[0]["mods"]
    xT = _stage_x(p)
    pw = _run_prep(p, NLAY, NE)
    ffn_tiles = [(t0, min(512, LS - t0), 0) for t0 in range(0, LS, 512)] + [(LS, CS, 1)]
    cos, sin, perm, msk, rmask = gla_consts(ROWS)
    for l in range(NLAY):
        mcols = np.zeros((128, 6 * KC), np.float32)
        for cnd in range(3):
            mcols[:, (2 * cnd) * KC:(2 * cnd + 1) * KC] = mods[l][cnd][:, 0:KC]
            mcols[:, (2 * cnd + 1) * KC:(2 * cnd + 2) * KC] = mods[l][cnd][:, KC:2 * KC]
        in_maps = []
        for k in range(8):
            m = dict(xT=xT, cols=mcols)
            m.update(_mixer_weights(l, k, p, ROWS))
            if l % 2 == 1:
                m.update(cos=cos, sin=sin, perm_in=perm, ident_in=eye, msk_in=msk, rmask_in=rmask)
            in_maps.append(m)
        if l % 2 == 0:
            outs = _run(_prog(("na", ROWS), lambda: build_na(ROWS=ROWS)), in_maps)
        else:
            outs = _run(_prog(("gla", ROWS), lambda: build_gla(ROWS=ROWS)), in_maps)
        oT = np.concatenate([o["oT"] for o in outs], axis=1)
        del outs, in_maps
        fw = dict(wr=np.ascontiguousarray(p["moe_router"][l][:, :NE]), eb=np.ascontiguousarray(p["moe_bias"][l][:NE].reshape(1, NE)))
        fw.update(pw[l])
        lnc = _lncols(l, p)
        in_maps = []
        for k in range(8):
            b, q = k // 4, k % 4
            sl = lambda a: np.ascontiguousarray(np.concatenate([a[b][:, q * LS:(q + 1) * LS], a[b][:, L + q * CS:L + (q + 1) * CS]], axis=1))
            fc = np.zeros((128, 12 * KC), np.float32)
            for ci, cnd in enumerate((b, 2)):
                for i, ty in enumerate((2, 4, 3, 5)):
                    fc[:, (4 * ci + i) * KC:(4 * ci + i + 1) * KC] = mods[l][cnd][:, ty * KC:(ty + 1) * KC]
            fc[:, 8 * KC:12 * KC] = lnc
            m = dict(xT=sl(xT), oT=sl(oT), cols=fc, ident_in=eye)
            m.update(fw)
            in_maps.append(m)
        outs = _run(_prog(("ffn", LS, NE), lambda: build_ffn(ffn_tiles, NE=NE, prep=True)), in_maps)
        del in_maps
        for k in range(8):
            b, q = k // 4, k % 4
            x2 = outs[k]["x2T"]
            xT[b, :, q * LS:(q + 1) * LS] = x2[:, :LS]
            xT[b, :, L + q * CS:L + (q + 1) * CS] = x2[:, LS:]
        del outs
    return np.ascontiguousarray(xT[:, :, :L].transpose(0, 2, 1))


def kernel(x, c, ctx, c_ctx, ada_w1, ada_w2, ada_b, ln_g, ln_b, na_w_qkv, na_w_o, na_rpb,
           gla_w_in, gla_w_a1, gla_w_a2, gla_b_a, gla_norm, gla_w_o, moe_router, moe_bias,
           moe_w_gate, moe_w_up, moe_w_down, sh_w_gate, sh_w_up, sh_w_down):
    p = dict(x=x, c=c, ctx=ctx, c_ctx=c_ctx, ada_w1=ada_w1, ada_w2=ada_w2, ada_b=ada_b, ln_g=ln_g, ln_b=ln_b, na_w_qkv=na_w_qkv,
             na_w_o=na_w_o, na_rpb=na_rpb, gla_w_in=gla_w_in, gla_w_a1=gla_w_a1, gla_w_a2=gla_w_a2, gla_b_a=gla_b_a, gla_norm=gla_norm,
             gla_w_o=gla_w_o, moe_router=moe_router, moe_bias=moe_bias, moe_w_gate=moe_w_gate, moe_w_up=moe_w_up, moe_w_down=moe_w_down,
             sh_w_gate=sh_w_gate, sh_w_up=sh_w_up, sh_w_down=sh_w_down)
    p = {k: np.asarray(v, dtype=np.float32) for k, v in p.items()}
    return kernel_unfused(p)
```
